# Optimizing a Trainium2 kernel written in Bass

```python
import jax, jax.numpy as jnp
from jax import lax
import numpy as np

D_MODEL = 1024
BATCH = 4
SEQ = 8192
DEPTH = 1

MIX_WIDTH = D_MODEL
HEAD_DIM = 64
N_HEADS = (MIX_WIDTH // 2) // HEAD_DIM
N_KV_HEADS = 2
ATTN_WIDTH = N_HEADS * HEAD_DIM
KV_WIDTH = N_KV_HEADS * HEAD_DIM
CONV_CH = MIX_WIDTH - ATTN_WIDTH
CONV_GROUPS = CONV_CH // HEAD_DIM
CONV_W = 31
IN_WIDTH = ATTN_WIDTH + 2 * KV_WIDTH + 2 * CONV_CH
Q_BLOCK = 128
GRID_W = 64
ROPE_THETA = 10000.0
ROPE_AXIS_DIM = HEAD_DIM // 2
N_KEYS = 128
N_EXPERTS = N_KEYS * N_KEYS
PEER_HEADS = 8
PEER_DQ = 256
PEER_TOPK = 16
TOKEN_CHUNK = 128
EPS = 1e-6

kernel_name = "hybrid_conv_gqa_peer_encoder"


def _rmsnorm(x, g):
    xf = x.astype(jnp.float32)
    y = xf * lax.rsqrt(jnp.mean(xf * xf, axis=-1, keepdims=True) + EPS)
    return (y * g.astype(jnp.float32)).astype(x.dtype)


def _layernorm(x, g, b):
    xf = x.astype(jnp.float32)
    mu = jnp.mean(xf, axis=-1, keepdims=True)
    var = jnp.mean(jnp.square(xf - mu), axis=-1, keepdims=True)
    y = (xf - mu) * lax.rsqrt(var + EPS)
    return (y * g.astype(jnp.float32) + b.astype(jnp.float32)).astype(x.dtype)


def _rope_rotate(x, ang):
    p = ang.shape[-1]
    cos = jnp.cos(ang)[:, None, :]
    sin = jnp.sin(ang)[:, None, :]
    xf = x.astype(jnp.float32)
    x1, x2 = xf[..., :p], xf[..., p:]
    return jnp.concatenate([x1 * cos - x2 * sin, x2 * cos + x1 * sin], axis=-1)


def _axial_rope(x, ang_row, ang_col):
    r = _rope_rotate(x[..., :ROPE_AXIS_DIM], ang_row)
    c = _rope_rotate(x[..., ROPE_AXIS_DIM:], ang_col)
    return jnp.concatenate([r, c], axis=-1).astype(x.dtype)


def _gqa_blocked(q, k, v):
    b, s, hkv, g, hd = q.shape
    nb = s // Q_BLOCK
    qb = q.reshape(b, nb, Q_BLOCK, hkv, g, hd).transpose(1, 0, 2, 3, 4, 5)

    def one_block(qblk):
        sc = jnp.einsum('bqkgd,bskd->bkgqs', qblk, k).astype(jnp.float32)
        p = jax.nn.softmax(sc, axis=-1).astype(v.dtype)
        return jnp.einsum('bkgqs,bskd->bqkgd', p, v)

    ob = lax.map(one_block, qb)
    return ob.transpose(1, 0, 2, 3, 4, 5).reshape(b, s, hkv * g * hd)


def _conformer_conv(a, gate, conv_dw, conv_b, conv_ln_g, conv_ln_b):
    h = a * jax.nn.sigmoid(gate)
    pad = (CONV_W - 1) // 2
    h = lax.conv_general_dilated(
        h, conv_dw.astype(h.dtype), window_strides=(1,), padding=((pad, pad),),
        dimension_numbers=('NWC', 'WIO', 'NWC'), feature_group_count=CONV_CH)
    h = h + conv_b.astype(h.dtype)
    h = _layernorm(h, conv_ln_g, conv_ln_b)
    return jax.nn.silu(h)


def _peer(xn, peer_wq, peer_keys, peer_u, peer_v):
    t, d = xn.shape
    xc_all = xn.reshape(t // TOKEN_CHUNK, TOKEN_CHUNK, d)
    half = PEER_DQ // 2

    def chunk(xc):
        q = (xc @ peer_wq).reshape(TOKEN_CHUNK, PEER_HEADS, PEER_DQ)
        s1 = jnp.einsum('chd,nd->chn', q[..., :half], peer_keys[0]).astype(jnp.float32)
        s2 = jnp.einsum('chd,nd->chn', q[..., half:], peer_keys[1]).astype(jnp.float32)
        v1, i1 = lax.top_k(s1, PEER_TOPK)
        v2, i2 = lax.top_k(s2, PEER_TOPK)
        cand_s = (v1[..., :, None] + v2[..., None, :]).reshape(TOKEN_CHUNK, PEER_HEADS, -1)
        cand_i = (i1[..., :, None] * N_KEYS + i2[..., None, :]).reshape(TOKEN_CHUNK, PEER_HEADS, -1)
        top_s, pos = lax.top_k(cand_s, PEER_TOPK)
        e = jnp.take_along_axis(cand_i, pos, axis=-1)
        g = jax.nn.softmax(top_s, axis=-1).astype(xc.dtype)
        u = peer_u[e]
        h = jax.nn.gelu(jnp.einsum('cd,chkd->chk', xc, u))
        return jnp.einsum('chk,chkd->cd', g * h, peer_v[e])

    return lax.map(chunk, xc_all).reshape(t, d)


def setup_inputs(seed: int = 0) -> dict:
    key = jax.random.key(seed)
    ks = jax.random.split(key, 16)
    f32 = jnp.float32
    nrm = lambda k, shp, sc: jax.random.normal(k, shp, f32) * sc
    return {
        "x": nrm(ks[0], (BATCH, SEQ, D_MODEL), 1.0),
        "norm1_g": 1.0 + nrm(ks[1], (D_MODEL,), 0.02),
        "w_in": nrm(ks[2], (D_MODEL, IN_WIDTH), D_MODEL ** -0.5),
        "q_norm_g": 1.0 + nrm(ks[3], (HEAD_DIM,), 0.02),
        "k_norm_g": 1.0 + nrm(ks[4], (HEAD_DIM,), 0.02),
        "conv_dw": nrm(ks[5], (CONV_W, 1, CONV_CH), CONV_W ** -0.5),
        "conv_b": nrm(ks[6], (CONV_CH,), 0.01),
        "conv_ln_g": 1.0 + nrm(ks[7], (CONV_CH,), 0.02),
        "conv_ln_b": nrm(ks[8], (CONV_CH,), 0.01),
        "w_out": nrm(ks[9], (MIX_WIDTH, D_MODEL), MIX_WIDTH ** -0.5),
        "norm2_g": 1.0 + nrm(ks[10], (D_MODEL,), 0.02),
        "peer_wq": nrm(ks[11], (D_MODEL, PEER_HEADS * PEER_DQ), D_MODEL ** -0.5),
        "peer_keys": nrm(ks[12], (2, N_KEYS, PEER_DQ // 2), (PEER_DQ // 2) ** -0.5),
        "peer_u": nrm(ks[13], (N_EXPERTS, D_MODEL), D_MODEL ** -0.5),
        "peer_v": nrm(ks[14], (N_EXPERTS, D_MODEL), 0.3),
        "final_g": 1.0 + nrm(ks[15], (D_MODEL,), 0.02),
    }


def reference(x, norm1_g, w_in, q_norm_g, k_norm_g, conv_dw, conv_b, conv_ln_g,
              conv_ln_b, w_out, norm2_g, peer_wq, peer_keys, peer_u, peer_v, final_g):
    b, s, d = x.shape
    rows = s // GRID_W
    row = jnp.repeat(jnp.arange(rows), GRID_W).astype(jnp.float32)
    col = jnp.tile(jnp.arange(GRID_W), rows).astype(jnp.float32)
    n_pairs = ROPE_AXIS_DIM // 2
    inv_freq = ROPE_THETA ** (-jnp.arange(n_pairs, dtype=jnp.float32) / n_pairs)
    ang_row = row[:, None] * inv_freq[None, :]
    ang_col = col[:, None] * inv_freq[None, :]
    group = N_HEADS // N_KV_HEADS

    for _ in range(DEPTH):
        h = _rmsnorm(x, norm1_g)
        p = h @ w_in
        o0 = ATTN_WIDTH
        o1 = o0 + KV_WIDTH
        o2 = o1 + KV_WIDTH
        o3 = o2 + CONV_CH
        q = p[..., :o0].reshape(b, s, N_HEADS, HEAD_DIM)
        k = p[..., o0:o1].reshape(b, s, N_KV_HEADS, HEAD_DIM)
        v = p[..., o1:o2].reshape(b, s, N_KV_HEADS, HEAD_DIM)
        q = _axial_rope(_rmsnorm(q, q_norm_g), ang_row, ang_col)
        k = _axial_rope(_rmsnorm(k, k_norm_g), ang_row, ang_col)
        q = (q * (HEAD_DIM ** -0.5)).reshape(b, s, N_KV_HEADS, group, HEAD_DIM)
        attn_out = _gqa_blocked(q, k, v)
        conv_out = _conformer_conv(p[..., o2:o3], p[..., o3:], conv_dw, conv_b,
                                   conv_ln_g, conv_ln_b)
        mixed = jnp.concatenate([attn_out, conv_out], axis=-1)
        x = x + mixed @ w_out
        hn = _rmsnorm(x, norm2_g).reshape(b * s, d)
        x = x + _peer(hn, peer_wq, peer_keys, peer_u, peer_v).reshape(b, s, d)

    return _rmsnorm(x, final_g)
```

```python
import bisect
from contextlib import ExitStack
import numpy as np
import concourse.bass as bass
import concourse.mybir as mybir
from concourse.bass_utils import run_bass_kernel_spmd

F32 = mybir.dt.float32
F32R = mybir.dt.float32r
BF16 = mybir.dt.bfloat16
U32 = mybir.dt.uint32
I32 = mybir.dt.int32
ALU = mybir.AluOpType
AF = mybir.ActivationFunctionType
AX = mybir.AxisListType

SEM_LIMIT = 30000


class _Cut(Exception):
    pass


class Buf:
    def __init__(self, name, excl=False):
        self.name = name
        self.last_w = None
        self.readers = []
        self.excl = excl


class DSem:
    def __init__(self, prog, name):
        self.prog = prog
        self.name = name
        self.sem = prog.nc.alloc_semaphore(name=name)
        self.total = 0
        self.group_ends = []
        self.open = False

    def need(self, v):
        i = bisect.bisect_left(self.group_ends, v)
        if i < len(self.group_ends):
            return self.group_ends[i]
        self.group_ends.append(self.total)
        self.open = False
        return self.total


class Prog:
    ENG = ("pe", "dve", "act", "pool", "sp")

    def __init__(self, nc):
        self.nc = nc
        self.handles = {"pe": nc.tensor, "dve": nc.vector, "act": nc.scalar,
                        "pool": nc.gpsimd, "sp": nc.sync}
        self.lists = {e: [] for e in self.ENG}
        self.cnt = {e: 0 for e in self.ENG}
        self.gen = {e: 0 for e in self.ENG}
        self.sems = {e: [nc.alloc_semaphore(name=f"s_{e}_0")] for e in self.ENG}
        self.waited = {}
        self.same_engine_sync = {"pe": False, "dve": True, "act": True,
                                 "pool": True, "sp": False}
        self.dsems = {}
        self.old_dsems = []
        self.n_inst = 0

    def _wait(self, eng, ev):
        kind, key, val = ev
        if kind == "e":
            e2, g = key
            if e2 == eng and not self.same_engine_sync[eng]:
                return
            sem = self.sems[e2][g]
            wkey = (eng, "e", e2, g)
            need = val
        else:
            ds = key
            need = ds.need(val)
            sem = ds.sem
            wkey = (eng, "d", ds.name)
        if self.waited.get(wkey, 0) >= need:
            return
        self.waited[wkey] = need
        self.lists[eng].append(("wait", sem, need))

    def _deps(self, eng, reads, writes):
        for b in reads:
            if b.last_w is not None:
                self._wait(eng, b.last_w)
            if b.excl:
                for ev in b.readers:
                    if ev[0] == "e" and ev[1][0] != eng:
                        self._wait(eng, ev)
        for b in writes:
            if b.last_w is not None:
                self._wait(eng, b.last_w)
            for ev in b.readers:
                self._wait(eng, ev)

    def _commit(self, ev, reads, writes):
        for b in writes:
            b.last_w = ev
            b.readers = []
        for b in reads:
            if b not in writes:
                b.readers.append(ev)
                if len(b.readers) > 64:
                    b.readers = b.readers[-64:]

    def op(self, eng, fn, reads=(), writes=()):
        reads = list(reads)
        writes = list(writes)
        self._deps(eng, reads, writes)
        if self.cnt[eng] >= SEM_LIMIT:
            self.gen[eng] += 1
            self.cnt[eng] = 0
            self.sems[eng].append(self.nc.alloc_semaphore(name=f"s_{eng}_{self.gen[eng]}"))
        self.cnt[eng] += 1
        g = self.gen[eng]
        self.lists[eng].append(("op", fn, self.sems[eng][g]))
        ev = ("e", (eng, g), self.cnt[eng])
        self._commit(ev, reads, writes)
        self.n_inst += 1
        return ev

    def dsem(self, name):
        if name not in self.dsems:
            self.dsems[name] = DSem(self, "d_" + name)
        return self.dsems[name]

    def dma(self, queue, fn, dsem, reads=(), writes=()):
        ds = self.dsem(dsem) if isinstance(dsem, str) else dsem
        if ds.total >= SEM_LIMIT and not ds.open and isinstance(dsem, str):
            self._wait(queue, ("d", ds, ds.total))
            self._dgen = getattr(self, "_dgen", 0) + 1
            ds = DSem(self, f"d_{dsem}_{self._dgen}")
            self.dsems[dsem] = ds
            self.old_dsems.append(ds)
        reads = list(reads)
        writes = list(writes)
        self._deps(queue, reads, writes)
        if (not ds.open) and ds.total > 0:
            self._wait(queue, ("d", ds, ds.total))
        ds.total += 16
        ds.open = True
        self.lists[queue].append(("dma", fn, ds.sem))
        ev = ("d", ds, ds.total)
        self._commit(ev, reads, writes)
        self.n_inst += 1
        return ev

    def wait_all(self, eng, bufs):
        for b in bufs:
            if b.last_w is not None:
                self._wait(eng, b.last_w)

    def emit(self):
        nc = self.nc
        with nc.Block() as block:
            def mk(ename):
                items = self.lists[ename]

                def body(h):
                    for it in items:
                        if it[0] == "wait":
                            h.wait_ge(it[1], it[2])
                        elif it[0] == "op":
                            it[1](h).then_inc(it[2], 1)
                        else:
                            it[1](h).then_inc(it[2], 16)
                return body
            block.tensor(mk("pe"))
            block.vector(mk("dve"))
            block.scalar(mk("act"))
            block.gpsimd(mk("pool"))
            block.sync(mk("sp"))


D_MODEL = 1024
SEQ = 8192
NOWN = 4096
HD = 64
CONV_W = 31
IN_W = 1792
EPS = 1e-6
NT_OWN = NOWN // 128
NT_ALL = SEQ // 128
TWO_PI = 2.0 * np.pi


def build_program(stop_after="D", debug=False, grp_list=None, pool_eng="pool", cut=None, ntile_d=None):
    nc = bass.Bass("TRN2", target_bir_lowering=False)
    P = Prog(nc)
    dbg = {}

    def din(name, shape, dt=F32):
        return nc.dram_tensor(name, list(shape), dt, kind="ExternalInput").ap()

    x_own = din("x_own", [NOWN, D_MODEL])
    x_oth = din("x_oth", [NOWN, D_MODEL])
    x_halo = din("x_halo", [32, D_MODEL])
    rowcol = din("rowcol", [128, NT_ALL, 2])
    norm1_g = din("norm1_g", [D_MODEL])
    w_in = din("w_in", [D_MODEL, IN_W])
    q_norm_g = din("q_norm_g", [HD])
    k_norm_g = din("k_norm_g", [HD])
    conv_dw = din("conv_dw", [CONV_W, 512])
    conv_b = din("conv_b", [512])
    conv_ln_g = din("conv_ln_g", [512])
    conv_ln_b = din("conv_ln_b", [512])
    w_out = din("w_out", [D_MODEL, D_MODEL])
    norm2_g = din("norm2_g", [D_MODEL])
    peer_wq = din("peer_wq", [D_MODEL, 2048])
    peer_keys = din("peer_keys", [2, 128, 128])
    peer_u = din("peer_u", [16384, D_MODEL])
    peer_v = din("peer_v", [16384, D_MODEL])
    final_g = din("final_g", [D_MODEL])
    out_d = nc.dram_tensor("out", [NOWN, D_MODEL], F32, kind="ExternalOutput").ap()
    x2_d = nc.dram_tensor("x2_scratch", [NOWN, D_MODEL], F32, kind="Internal").ap()

    def dout(name, shape, dt=F32):
        dbg[name] = nc.dram_tensor(name, list(shape), dt, kind="ExternalOutput").ap()
        return dbg[name]

    _n = [0]
    cst = ExitStack()
    pers = ExitStack()
    scope = [ExitStack()]

    def sb(shape, dt=F32, name=None, persistent=False):
        _n[0] += 1
        nm = name or f"sb{_n[0]}"
        if persistent == "c":
            return cst.enter_context(nc.sbuf_tensor(nm, list(shape), dt, side="right"))
        if persistent:
            return pers.enter_context(nc.sbuf_tensor(nm, list(shape), dt, side="right"))
        return scope[0].enter_context(nc.sbuf_tensor(nm, list(shape), dt, side="left"))

    def new_scope():
        barrier()
        scope[0].close()
        scope[0] = ExitStack()

    def mm(out, lhsT, rhs, start, stop, R, W):
        P.op("pe", lambda h: h.matmul(out, lhsT=lhsT, rhs=rhs, start=start, stop=stop), R, W)

    def tr(out, in_, ident, R, W):
        P.op("pe", lambda h: h.transpose(out=out, in_=in_, identity=ident), R, W)

    def act(out, in_, func, R, W, bias=None, scale=None, accum=None):
        kw = {}
        if bias is not None:
            kw["bias"] = bias
        if scale is not None:
            kw["scale"] = scale
        if accum is not None:
            kw["accum_out"] = accum
        P.op("act", lambda h: h.activation(out=out, in_=in_, func=func, **kw), R, W)

    def acopy(out, in_, R, W):
        P.op("act", lambda h: h.copy(out=out, in_=in_), R, W)

    def tt(eng, out, in0, in1, op, R, W):
        P.op(eng, lambda h: h.tensor_tensor(out=out, in0=in0, in1=in1, op=op), R, W)

    def ts(eng, out, in0, s1, s2, op0, op1, R, W):
        if op1 is None:
            P.op(eng, lambda h: h.tensor_scalar(out=out, in0=in0, scalar1=s1, scalar2=None, op0=op0), R, W)
        else:
            P.op(eng, lambda h: h.tensor_scalar(out=out, in0=in0, scalar1=s1, scalar2=s2, op0=op0, op1=op1), R, W)

    def stt(out, in0, scalar, in1, op0, op1, R, W):
        P.op("dve", lambda h: h.scalar_tensor_tensor(out=out, in0=in0, scalar=scalar, in1=in1, op0=op0, op1=op1), R, W)

    def vcopy(eng, out, in_, R, W):
        P.op(eng, lambda h: h.tensor_copy(out=out, in_=in_), R, W)

    def recip(out, in_, R, W):
        P.op("dve", lambda h: h.reciprocal(out=out, in_=in_), R, W)

    def reduce_add(out, in_, R, W):
        P.op("dve", lambda h: h.tensor_reduce(out=out, in_=in_, axis=AX.X, op=ALU.add), R, W)

    def memset(eng, ap, val, W):
        P.op(eng, lambda h: h.memset(ap, val), [], W)

    def dma(q, out, in_, ds, R, W, slow=False):
        if slow:
            P.dma(q, lambda h: h.dma_start(out=out, in_=in_, allow_slow_non_contiguous=True), ds, R, W)
        else:
            P.dma(q, lambda h: h.dma_start(out=out, in_=in_), ds, R, W)

    def barrier():
        evs = []
        for e in P.ENG:
            if P.cnt[e] > 0:
                evs.append(("e", (e, P.gen[e]), P.cnt[e]))
        for ds in list(P.dsems.values()):
            if ds.total > 0:
                evs.append(("d", ds, ds.total))
        for e in P.ENG:
            for ev in evs:
                if ev[0] == "e" and ev[1][0] == e:
                    continue
                P._wait(e, ev)

    PSD = [nc.alloc_psum_tensor(f"pd{i}", [128, 1024], F32) for i in range(4)]
    PS = [PSD[i // 2][:, (i % 2) * 512:(i % 2 + 1) * 512] for i in range(8)]
    PSB = [Buf(f"bank{i}", excl=True) for i in range(8)]

    def psbf(i):
        return PS[i].bitcast(BF16)

    CONST = Buf("const")
    ident_f = sb([128, 128], F32, "ident_f", persistent="c")
    ident_b = sb([128, 128], BF16, "ident_b", persistent="c")
    iot = sb([128, 128], F32, "iot", persistent="c")
    P.op("pool", lambda h: h.iota(iot[:], pattern=[[1, 128]], base=0, channel_multiplier=-1,
                                  allow_small_or_imprecise_dtypes=True), [], [CONST])
    ts("dve", ident_f[:], iot[:], 0.0, None, ALU.is_equal, None, [CONST], [CONST])
    vcopy("dve", ident_b[:], ident_f[:], [CONST], [CONST])
    ones_f = sb([128, 128], F32, "ones_f", persistent="c")
    memset("dve", ones_f[:], 1.0 / 512.0, [CONST])
    iota16 = sb([128, 16], F32, "iota16", persistent="c")
    P.op("pool", lambda h: h.iota(iota16[:], pattern=[[1, 16]], base=0, channel_multiplier=0,
                                  allow_small_or_imprecise_dtypes=True), [], [CONST])

    g1 = sb([128, 8], F32, "g1", persistent="c")
    dma("sp", g1[:], norm1_g.rearrange("(c p) -> p c", p=128), "c0", [], [CONST], slow=True)
    cb = sb([128, 4], F32, "cb", persistent="c")
    lg = sb([128, 4], F32, "lg", persistent="c")
    lb = sb([128, 4], F32, "lb", persistent="c")
    dma("sp", cb[:], conv_b.rearrange("(c p) -> p c", p=128), "c0", [], [CONST], slow=True)
    dma("sp", lg[:], conv_ln_g.rearrange("(c p) -> p c", p=128), "c0", [], [CONST], slow=True)
    dma("sp", lb[:], conv_ln_b.rearrange("(c p) -> p c", p=128), "c0", [], [CONST], slow=True)
    G10 = sb([128, 10, 64], F32, "G10", persistent="c")
    dma("sp", G10[:, 0, :], q_norm_g.partition_broadcast(128), "c0", [], [CONST])
    dma("sp", G10[:, 8, :], k_norm_g.partition_broadcast(128), "c0", [], [CONST])
    ts("dve", G10[:, 0, :], G10[:, 0, :], HD ** -0.5, None, ALU.mult, None, [CONST], [CONST])
    for j in range(1, 8):
        vcopy("dve", G10[:, j, :], G10[:, 0, :], [CONST], [CONST])
    vcopy("dve", G10[:, 9, :], G10[:, 8, :], [CONST], [CONST])
    KT = sb([128, SEQ], BF16, "KT", persistent=True)
    VA = sb([128, NT_ALL, 2, 128], BF16, "VA", persistent=True)
    QT = sb([128, 4, NOWN], BF16, "QT", persistent=True)
    htstack = ExitStack()
    HT = htstack.enter_context(nc.sbuf_tensor("HT", [128, 4, NOWN + 32], BF16, side="left"))

    rc_t = sb([128, NT_ALL, 2], F32, "rc_t")
    dma("sp", rc_t[:], rowcol, "c0", [], [CONST])
    invf = sb([128, 16], F32, "invf")
    act(invf[:], iota16[:], AF.Exp, [CONST], [CONST], scale=-float(np.log(10000.0)) / 16.0)
    NTAB = NT_ALL * 32
    cos_t = sb([128, NT_ALL, 2, 16], F32, "cos_t")
    sin_t = sb([128, NT_ALL, 2, 16], F32, "sin_t")
    w1b = sb([128, 8, IN_W], BF16, "w1b")
    W1B = Buf("w1b")
    with nc.sbuf_tensor("rr_k", [128, NTAB], I32, side="left") as rr_k, \
            nc.sbuf_tensor("rr_f", [128, NTAB], F32, side="left") as rr_f, \
            nc.sbuf_tensor("rr_a", [128, NTAB], F32, side="left") as rr_a, \
            nc.sbuf_tensor("ang", [128, NT_ALL, 2, 16], F32, side="left") as ang:
        tt("dve", ang[:], rc_t[:].unsqueeze(3).to_broadcast([128, NT_ALL, 2, 16]),
           invf[:].unsqueeze(1).unsqueeze(1).to_broadcast([128, NT_ALL, 2, 16]), ALU.mult, [CONST], [CONST])
        angf = ang[:].rearrange("p a b c -> p (a b c)")
        TMPB = Buf("ropetmp")
        for tab, shift in ((sin_t, 0.0), (cos_t, np.pi / 2)):
            tf = tab[:].rearrange("p a b c -> p (a b c)")
            ts("dve", rr_a[:], angf, float(shift), None, ALU.add, None, [CONST], [TMPB])
            ts("dve", rr_k[:], rr_a[:], 1.0 / TWO_PI, 0.5, ALU.mult, ALU.add, [TMPB], [TMPB])
            vcopy("dve", rr_f[:], rr_k[:], [TMPB], [TMPB])
            stt(rr_f[:], rr_f[:], -TWO_PI, rr_a[:], ALU.mult, ALU.add, [TMPB], [TMPB])
            ts("dve", rr_a[:], rr_f[:], -float(np.pi), TWO_PI, ALU.is_lt, ALU.mult, [TMPB], [TMPB])
            tt("dve", rr_f[:], rr_f[:], rr_a[:], ALU.add, [TMPB], [TMPB])
            ts("dve", rr_a[:], rr_f[:], float(np.pi), TWO_PI, ALU.is_gt, ALU.mult, [TMPB], [TMPB])
            tt("dve", rr_a[:], rr_f[:], rr_a[:], ALU.subtract, [TMPB], [TMPB])
            ts("dve", rr_a[:], rr_a[:], float(np.pi), -float(np.pi), ALU.min, ALU.max, [TMPB], [TMPB])
            act(tf, rr_a[:], AF.Sin, [TMPB], [CONST])
        barrier()
    with nc.sbuf_tensor("stg0", [128, IN_W], F32, side="left") as stg0, \
            nc.sbuf_tensor("stg1", [128, IN_W], F32, side="left") as stg1:
        stg = [stg0, stg1]
        STG = [Buf("stg0"), Buf("stg1")]
        for dc in range(8):
            s = dc % 2
            dma("sp", stg[s][:], w_in[dc * 128:(dc + 1) * 128, :], f"stg{s}", [], [STG[s]])
            ts("dve", w1b[:, dc, 0:512].rearrange("p (j g d) -> p j g d", j=4, g=2),
               stg[s][:, 0:512].rearrange("p (g j d) -> p j g d", g=2, j=4),
               g1[:, dc:dc + 1], None, ALU.mult, None, [STG[s], CONST], [W1B])
            ts("pool", w1b[:, dc, 512:IN_W], stg[s][:, 512:IN_W], g1[:, dc:dc + 1], None, ALU.mult, None,
               [STG[s], CONST], [W1B])
        barrier()

    if stop_after == "0":
        o = dout("d_cos", [128, NT_ALL, 2, 16], F32)
        dma("sp", o, cos_t[:], "dbg", [], [])
        o = dout("d_sin", [128, NT_ALL, 2, 16], F32)
        dma("sp", o, sin_t[:], "dbg", [], [])
        o = dout("d_w1b", [128, 8, IN_W], BF16)
        dma("sp", o, w1b[:], "dbg", [], [])
        o = dout("d_G10", [128, 10, 64], F32)
        dma("sp", o, G10[:], "dbg", [], [])
        barrier()
        P.emit()
        return nc, dbg
    KTB = [Buf(f"kt{i}") for i in range(NT_ALL)]
    VAB = [Buf(f"va{i}") for i in range(NT_ALL)]
    QTB = [Buf(f"qt{i}") for i in range(NT_OWN)]
    HTB = [Buf(f"ht{i}") for i in range(NT_OWN // 4 + 1)]
    VINIT = Buf("vinit")
    memset("pool", VA[:, :, :, 64:128].rearrange("p a b c -> p (a b) c"), 1.0, [VINIT])
    for b in VAB:
        b.last_w = VINIT.last_w
    memset("pool", HT[:, :, NOWN + 30:NOWN + 32], 0.0, [HTB[NT_OWN // 4]])

    xin = [sb([128, D_MODEL], F32, f"xin{i}") for i in range(2)]
    XIN = [Buf(f"xin{i}") for i in range(2)]
    junk = sb([128, D_MODEL], BF16, "junk")
    JUNK = Buf("junk")
    xs = [sb([128, D_MODEL], BF16, f"xs{i}") for i in range(2)]
    XS = [Buf(f"xs{i}") for i in range(2)]
    xT = [sb([128, 8, 512], BF16, f"xT{i}") for i in range(2)]
    XT = [Buf(f"xT{i}") for i in range(2)]
    st = sb([128, 8], F32, "st")
    ST = Buf("st")
    sq = sb([128, 640], F32, "sq")
    SQ = Buf("sq")
    ssq = sb([128, 10], F32, "ssq")
    qn = sb([128, 10, 64], F32, "qn")
    QN = Buf("qn")
    t1 = sb([128, 10, 64], F32, "t1")
    t2 = sb([128, 10, 64], F32, "t2")
    T12 = Buf("t12")
    qr = sb([128, 640], BF16, "qr")
    QR = Buf("qr")
    sg = sb([128, 512], F32, "sg")
    SG = Buf("sg")

    def rms_rows(xt_ap, XB, np_, out_bf, OB):
        act(junk[0:np_, :], xt_ap, AF.Square, [XB], [JUNK, ST], accum=st[0:np_, 0:1])
        act(st[0:np_, 1:2], st[0:np_, 0:1], AF.Sqrt, [ST], [ST], bias=EPS, scale=1.0 / D_MODEL)
        recip(st[0:np_, 2:3], st[0:np_, 1:2], [ST], [ST])
        ts("dve", out_bf, xt_ap, st[0:np_, 2:3], None, ALU.mult, None, [XB, ST], [OB])

    try:
        n_groups = NT_ALL // 4
        tile_ctr = 0
        for grp in (grp_list if grp_list is not None else range(n_groups + 1)):
            halo = grp == n_groups
            own = grp < NT_OWN // 4
            gs = grp % 2
            nsub = 1 if halo else 4
            for sub in range(nsub):
                ti = grp * 4 + sub
                s = tile_ctr % 2
                tile_ctr += 1
                np_ = 32 if halo else 128
                if halo:
                    src = x_halo[:, :]
                elif own:
                    src = x_own[ti * 128:(ti + 1) * 128, :]
                else:
                    src = x_oth[(ti - NT_OWN) * 128:(ti - NT_OWN + 1) * 128, :]
                dma("sp", xin[s][0:np_, :], src, f"xin{s}", [], [XIN[s]])
                rms_rows(xin[s][0:np_, :], XIN[s], np_, xs[s][0:np_, :], XS[s])
                if cut == 1:
                    raise _Cut()
                pb = ti % 2
                for dc in range(8):
                    tr(psbf(pb)[:, dc * 128:dc * 128 + np_], xs[s][0:np_, dc * 128:(dc + 1) * 128],
                       ident_b[0:np_, 0:np_], [XS[s], CONST], [PSB[pb]])
                acopy(xT[gs][:, :, sub * 128:sub * 128 + np_],
                      psbf(pb).rearrange("p (c t) -> p c t", c=8)[:, :, 0:np_], [PSB[pb]], [XT[gs]])
                if cut == 2:
                    raise _Cut()
                if halo:
                    continue
                c0 = 0 if own else 512
                ncol = 768 - c0
                for (a, b) in (((0, 512), (512, 768)) if own else ((512, 768),)):
                    bank = 2 if a == 0 else 3
                    for dc in range(8):
                        mm(PS[bank][:, 0:b - a], xT[gs][:, dc, sub * 128:(sub + 1) * 128], w1b[:, dc, a:b],
                           dc == 0, dc == 7, [XT[gs], W1B], [PSB[bank]])
                if cut == 3:
                    raise _Cut()
                h0 = 0 if own else 8
                nh = 10 - h0
                if own:
                    act(sq[:, 0:512], PS[2][:, 0:512], AF.Square, [PSB[2]], [SQ])
                act(sq[:, 512:640], PS[3][:, 0:128], AF.Square, [PSB[3]], [SQ])
                reduce_add(ssq[:, h0:10], sq[:, h0 * 64:640].rearrange("p (h d) -> p h d", d=64), [SQ], [ST])
                act(ssq[:, h0:10], ssq[:, h0:10], AF.Sqrt, [ST], [ST], bias=EPS, scale=1.0 / HD)
                recip(ssq[:, h0:10], ssq[:, h0:10], [ST], [ST])
                if own:
                    tt("dve", qn[:, 0:8, :], PS[2][:, 0:512].rearrange("p (h d) -> p h d", d=64),
                       ssq[:, 0:8].unsqueeze(2).to_broadcast([128, 8, 64]), ALU.mult, [PSB[2], ST], [QN])
                tt("dve", qn[:, 8:10, :], PS[3][:, 0:128].rearrange("p (h d) -> p h d", d=64),
                   ssq[:, 8:10].unsqueeze(2).to_broadcast([128, 2, 64]), ALU.mult, [PSB[3], ST], [QN])
                if cut == 4:
                    raise _Cut()
                acopy(VA[:, ti, :, 0:64], PS[3][:, 128:256].rearrange("p (g d) -> p g d", g=2), [PSB[3]], [VAB[ti]])
                tt(pool_eng, qn[:, h0:10, :], qn[:, h0:10, :], G10[:, h0:10, :], ALU.mult, [QN, CONST], [QN])
                if cut == 5:
                    raise _Cut()
                for rc in range(2):
                    qv = qn[:, h0:10, rc * 32:(rc + 1) * 32].rearrange("p h (a d) -> p h a d", a=2)
                    cosb = cos_t[:, ti, rc, :].unsqueeze(1).unsqueeze(1).to_broadcast([128, nh, 2, 16])
                    sinb = sin_t[:, ti, rc, :].unsqueeze(1).to_broadcast([128, nh, 16])
                    t1v = t1[:, h0:10, rc * 32:(rc + 1) * 32].rearrange("p h (a d) -> p h a d", a=2)
                    t2v = t2[:, h0:10, rc * 32:(rc + 1) * 32].rearrange("p h (a d) -> p h a d", a=2)
                    tt("dve", t1v, qv, cosb, ALU.mult, [QN, CONST], [T12])
                    tt(pool_eng, t2v[:, :, 0, :], qv[:, :, 1, :], sinb, ALU.mult, [QN, CONST], [T12])
                    tt(pool_eng, t2v[:, :, 1, :], qv[:, :, 0, :], sinb, ALU.mult, [QN, CONST], [T12])
                    qrv = qr[:, h0 * 64:640].rearrange("p (h c) -> p h c", c=64)[:, :, rc * 32:(rc + 1) * 32] \
                        .rearrange("p h (a d) -> p h a d", a=2)
                    tt("dve", qrv[:, :, 0, :], t1v[:, :, 0, :], t2v[:, :, 0, :], ALU.subtract, [T12], [QR])
                    tt("dve", qrv[:, :, 1, :], t1v[:, :, 1, :], t2v[:, :, 1, :], ALU.add, [T12], [QR])
                    if cut == 6:
                        raise _Cut()
                if own:
                    for j in range(4):
                        tr(psbf(4)[:, j * 128:(j + 1) * 128], qr[:, j * 128:(j + 1) * 128], ident_b[:],
                           [QR, CONST], [PSB[4]])
                tr(psbf(4)[:, 512:640], qr[:, 512:640], ident_b[:], [QR, CONST], [PSB[4]])
                if cut == 71:
                    raise _Cut()
                if own:
                    acopy(QT[:, :, ti * 128:(ti + 1) * 128], psbf(4)[:, 0:512].rearrange("p (j t) -> p j t", j=4),
                          [PSB[4]], [QTB[ti]])
                if cut == 72:
                    raise _Cut()
                acopy(KT[:, ti * 128:(ti + 1) * 128], psbf(4)[:, 512:640], [PSB[4]], [KTB[ti]])
                if cut == 7:
                    raise _Cut()
            if own or halo:
                ntok = 32 if halo else 512
                for c in range(4):
                    for part, bank in ((0, 5), (1, 6)):
                        col = 768 + part * 512 + c * 128
                        for dc in range(8):
                            mm(PS[bank][:, 0:ntok], w1b[:, dc, col:col + 128], xT[gs][:, dc, 0:ntok],
                               dc == 0, dc == 7, [XT[gs], W1B], [PSB[bank]])
                    act(sg[:, 0:ntok], PS[6][:, 0:ntok], AF.Sigmoid, [PSB[6]], [SG])
                    if halo:
                        tt("dve", HT[:, c, 0:15], PS[5][:, 0:15], sg[:, 0:15], ALU.mult, [PSB[5], SG], [HTB[0]])
                        tt("dve", HT[:, c, NOWN + 15:NOWN + 30], PS[5][:, 16:31], sg[:, 16:31], ALU.mult,
                           [PSB[5], SG], [HTB[NT_OWN // 4]])
                    else:
                        tt("dve", HT[:, c, 15 + grp * 512:15 + (grp + 1) * 512], PS[5][:, :], sg[:, :], ALU.mult,
                           [PSB[5], SG], [HTB[grp]])
    except _Cut:
        pass
    barrier()

    if debug:
        o = dout("d_KT", [128, SEQ], BF16)
        dma("sp", o, KT[:], "dbg", [], [])
        o = dout("d_QT", [128, 4, NOWN], BF16)
        dma("sp", o, QT[:], "dbg", [], [])
        o = dout("d_VA", [128, NT_ALL, 2, 128], BF16)
        dma("sp", o, VA[:], "dbg", [], [])
        o = dout("d_HT", [128, 4, NOWN + 32], BF16)
        dma("sp", o, HT[:], "dbg", [], [])
        barrier()
    if stop_after == "A":
        P.emit()
        return nc, dbg

    new_scope()
    CT = sb([128, 4, NOWN], BF16, "CT", persistent=True)
    CTB = [Buf(f"ct{i}") for i in range(8)]
    dg = sb([128, 4, CONV_W, 128], BF16, "dg")
    DG = Buf("dg")
    cw = sb([128, 4, CONV_W], F32, "cw")
    cwr = sb([CONV_W, 512], F32, "cwr")
    CW = Buf("cw")
    dma("sp", cwr[:], conv_dw, "cw", [], [CW])
    for c in range(4):
        tr(PS[0][:, c * 32:c * 32 + CONV_W], cwr[:, c * 128:(c + 1) * 128], ident_f[0:CONV_W, 0:CONV_W],
           [CW, CONST], [PSB[0]])
    vcopy("dve", cw[:], PS[0][:, 0:128].rearrange("p (c j) -> p c j", c=4)[:, :, 0:CONV_W], [PSB[0]], [CW])
    for c in range(4):
        for j in range(CONV_W):
            eng = "dve" if (j % 2 == 0) else "pool"
            ts(eng, dg[:, c, j, :], ident_b[:], cw[:, c, j:j + 1], None, ALU.mult, None, [CW, CONST], [DG])
    ybuf = sb([128, 4, 512], F32, "ybuf")
    ysq = sb([128, 4, 512], F32, "ysq")
    YB = [Buf(f"yb{c}") for c in range(4)]
    YS = [Buf(f"ys{c}") for c in range(4)]
    m2 = sb([128, 512], F32, "m2")
    M2 = Buf("m2")
    rstd_c = sb([128, 512], F32, "rstd_c")
    RSC = Buf("rstd_c")
    tmpc = sb([128, 512], F32, "tmpc")
    TMPC = Buf("tmpc")
    for q in range(8):
        hreads = [HTB[q]] + ([HTB[q + 1]] if q + 1 <= 8 else []) + ([HTB[q - 1]] if q > 0 else [])
        for c in range(4):
            bank = c % 2
            for j in range(CONV_W):
                mm(PS[bank][:, :], dg[:, c, j, :], HT[:, c, q * 512 + j:q * 512 + j + 512],
                   j == 0, j == CONV_W - 1, hreads + [DG], [PSB[bank]])
            act(ybuf[:, c, :], PS[bank][:, :], AF.Identity, [PSB[bank], CONST], [YB[c]], bias=cb[:, c:c + 1])
            act(ysq[:, c, :], ybuf[:, c, :], AF.Square, [YB[c]], [YS[c]])
        for c in range(4):
            mm(PS[2][:, :], ones_f[:], ybuf[:, c, :], c == 0, c == 3, [YB[c], CONST], [PSB[2]])
        for c in range(4):
            mm(PS[3][:, :], ones_f[:], ysq[:, c, :], c == 0, c == 3, [YS[c], CONST], [PSB[3]])
        act(m2[:], PS[2][:, :], AF.Square, [PSB[2]], [M2])
        tt("dve", m2[:], PS[3][:, :], m2[:], ALU.subtract, [PSB[3], M2], [M2])
        act(m2[:], m2[:], AF.Sqrt, [M2], [M2], bias=EPS, scale=1.0)
        recip(rstd_c[:], m2[:], [M2], [RSC])
        for c in range(4):
            tt("dve", tmpc[:], ybuf[:, c, :], PS[2][:, :], ALU.subtract, [YB[c], PSB[2]], [TMPC])
            tt("dve", tmpc[:], tmpc[:], rstd_c[:], ALU.mult, [TMPC, RSC], [TMPC])
            act(CT[:, c, q * 512:(q + 1) * 512], tmpc[:], AF.Silu, [TMPC, CONST], [CTB[q]],
                bias=lb[:, c:c + 1], scale=lg[:, c:c + 1])
    barrier()
    if debug:
        o = dout("d_CT", [128, 4, NOWN], BF16)
        dma("sp", o, CT[:], "dbg", [], [])
        barrier()
    if stop_after == "B":
        P.emit()
        return nc, dbg

    new_scope()
    htstack.close()
    wob_a = sb([64, 8, D_MODEL], BF16, "wob_a")
    wob_c = sb([128, 4, D_MODEL], BF16, "wob_c")
    WOB = Buf("wob")
    xr = [sb([128, D_MODEL], F32, f"xr{i}") for i in range(2)]
    XR = [Buf(f"xr{i}") for i in range(2)]
    for hh in range(8):
        s = hh % 2
        dma("sp", xr[s][0:64, :], w_out[hh * 64:(hh + 1) * 64, :], f"xr{s}", [], [XR[s]])
        vcopy("dve", wob_a[:, hh, :], xr[s][0:64, :], [XR[s]], [WOB])
    for c in range(4):
        s = c % 2
        dma("sp", xr[s][:, :], w_out[512 + c * 128:512 + (c + 1) * 128, :], f"xr{s}", [], [XR[s]])
        vcopy("dve", wob_c[:, c, :], xr[s][:, :], [XR[s]], [WOB])
    pT = [sb([128, 512], BF16, f"pT{i}") for i in range(3)]
    PT = [Buf(f"pT{i}") for i in range(3)]
    den = sb([64, 512], F32, "den")
    DEN = Buf("den")
    OTn = sb([64, 8, 512], BF16, "OTn")
    OTB = [Buf(f"otn{h}") for h in range(8)]
    x2t = [sb([128, D_MODEL], F32, f"x2t{i}") for i in range(2)]
    X2T = [Buf(f"x2t{i}") for i in range(2)]
    X2D = [Buf(f"x2d{i}") for i in range(NT_OWN)]
    kk = 0
    for qt in range(8):
        qreads = [QTB[qt * 4 + i] for i in range(4)]
        for g in range(2):
            for j in range(4):
                h = g * 4 + j
                obank = 3 + (h % 2)
                for kt in range(NT_ALL):
                    sbank = kk % 3
                    slot = kk % 3
                    kk += 1
                    mm(PS[sbank][:, :], KT[g * 64:(g + 1) * 64, kt * 128:(kt + 1) * 128],
                       QT[g * 64:(g + 1) * 64, j, qt * 512:(qt + 1) * 512], True, True,
                       [KTB[kt]] + qreads, [PSB[sbank]])
                    act(pT[slot][:], PS[sbank][:, :], AF.Exp, [PSB[sbank]], [PT[slot]])
                    mm(PS[obank][:, :], VA[:, kt, g, :], pT[slot][:], kt == 0, kt == NT_ALL - 1,
                       [VAB[kt], PT[slot]], [PSB[obank]])
                acopy(den[:], PS[obank][64:128, :], [PSB[obank]], [DEN])
                recip(den[:], den[:], [DEN], [DEN])
                tt("dve", OTn[:, h, :], PS[obank][0:64, :], den[:], ALU.mult, [PSB[obank], DEN], [OTB[h]])
        for sub in range(4):
            ti = qt * 4 + sub
            s = ti % 2
            tok = ti * 128
            dma("sp", xr[s][:], x_own[tok:tok + 128, :], f"xr{s}", [], [XR[s]])
            for half in range(2):
                bank = 5 + half
                n0 = half * 512
                for h in range(8):
                    mm(PS[bank][:, :], OTn[:, h, sub * 128:(sub + 1) * 128], wob_a[:, h, n0:n0 + 512],
                       h == 0, False, [OTB[h], WOB], [PSB[bank]])
                for c in range(4):
                    mm(PS[bank][:, :], CT[:, c, tok:tok + 128], wob_c[:, c, n0:n0 + 512],
                       False, c == 3, [CTB[qt], WOB], [PSB[bank]])
                tt("dve", x2t[s][:, n0:n0 + 512], PS[bank][:, :], xr[s][:, n0:n0 + 512], ALU.add,
                   [PSB[bank], XR[s]], [X2T[s]])
            dma("sp", x2_d[tok:tok + 128, :], x2t[s][:], f"x2o{s}", [X2T[s]], [X2D[ti]])
    barrier()
    if debug:
        o = dout("d_x2", [NOWN, D_MODEL], F32)
        for ti in range(NT_OWN):
            s = ti % 2
            dma("sp", xr[s][:], x2_d[ti * 128:(ti + 1) * 128, :], f"xr{s}", [X2D[ti]], [XR[s]])
            dma("sp", o[ti * 128:(ti + 1) * 128, :], xr[s][:], f"x2o{s}", [XR[s]], [])
        barrier()
    if stop_after == "C":
        P.emit()
        return nc, dbg

    new_scope()
    pers.close()
    wqb = sb([128, 8, 2048], BF16, "wqb")
    WQB = Buf("wqb")
    keysT = sb([128, 2, 128], BF16, "keysT")
    g2bc = sb([128, D_MODEL], F32, "g2bc")
    gfbc = sb([128, D_MODEL], F32, "gfbc")
    sel = sb([128, 128, 128], BF16, "sel")
    x2s = [sb([128, D_MODEL], F32, f"x2s{i}") for i in range(2)]
    X2S = [Buf(f"x2s{i}") for i in range(2)]
    dma("sp", g2bc[:], norm2_g.partition_broadcast(128), "c0", [], [CONST])
    dma("sp", gfbc[:], final_g.partition_broadcast(128), "c0", [], [CONST])
    vcopy("pool", sel[:], ident_b[:].unsqueeze(2).to_broadcast([128, 128, 128]), [CONST], [CONST])
    for hf in range(2):
        dma("sp", x2s[hf][:, 0:128], peer_keys[hf, :, :], f"x2s{hf}", [], [X2S[hf]])
        tr(PS[6][:, hf * 128:(hf + 1) * 128], x2s[hf][:, 0:128], ident_f[:], [X2S[hf], CONST], [PSB[6]])
    acopy(keysT[:], PS[6][:, 0:256].rearrange("p (a n) -> p a n", a=2), [PSB[6]], [CONST])
    for dc in range(8):
        for hf in range(2):
            s = (dc * 2 + hf) % 2
            dma("sp", x2s[s][:], peer_wq[dc * 128:(dc + 1) * 128, hf * 1024:(hf + 1) * 1024], f"x2s{s}",
                [], [X2S[s]])
            if hf == 0:
                vcopy("dve", wqb[:, dc, 0:1024], x2s[s][:], [X2S[s]], [WQB])
            else:
                acopy(wqb[:, dc, 1024:2048], x2s[s][:], [X2S[s]], [WQB])

    junk = sb([128, D_MODEL], BF16, "junkD")
    JUNK = Buf("junkD")
    st = sb([128, 8], F32, "stD")
    ST = Buf("stD")
    hn = sb([128, D_MODEL], F32, "hn")
    HN = Buf("hn")
    hnb = sb([128, D_MODEL], BF16, "hnb")
    HNB = Buf("hnb")
    hnT = sb([128, 8, 128], BF16, "hnT")
    HNT = Buf("hnT")
    qTs = sb([128, 16, 128], BF16, "qTs")
    QTS = Buf("qTs")
    S = sb([128, 16, 128], F32, "S")
    SB_ = Buf("S")
    wk = sb([128, 256], F32, "wk")
    tv = sb([128, 16, 16], F32, "tv")
    tiu = sb([128, 16, 16], U32, "tiu")
    tif = sb([128, 16, 16], F32, "tif")
    TK = Buf("topk1")
    cand = sb([128, 8, 16, 16], F32, "cand")
    CAND = Buf("cand")
    tops = sb([128, 8, 16], F32, "tops")
    posu = sb([128, 8, 16], U32, "posu")
    piu = sb([128, 8, 16], U32, "piu")
    pju = sb([128, 8, 16], U32, "pju")
    pif = sb([128, 8, 16], F32, "pif")
    pjf = sb([128, 8, 16], F32, "pjf")
    TK2 = Buf("topk2")
    oh = sb([128, 8, 16, 16], F32, "oh")
    OH = Buf("oh")
    i1s = sb([128, 8, 16], F32, "i1s")
    i2s = sb([128, 8, 16], F32, "i2s")
    ef = sb([128, 128], F32, "ef")
    gw = sb([128, 8, 16], F32, "gw")
    ssum = sb([128, 8], F32, "ssum")
    EG = Buf("eg")
    eTu = sb([128, 128], U32, "eTu")
    ET = Buf("eTu")
    gT = sb([128, 128], F32, "gT")
    GT = Buf("gT")
    hT = sb([128, 128], F32, "hT")
    HTD = Buf("hT")
    gl = sb([128, 128], F32, "gl")
    gl2 = sb([128, 128], F32, "gl2")
    ghT = sb([128, 128], F32, "ghT")
    GH = Buf("ghT")
    gsl = [sb([128, D_MODEL], F32, f"gs{i}") for i in range(4)]
    GS = [Buf(f"gs{i}") for i in range(4)]
    oT = sb([128, 8, 128], F32, "oT")
    OT = Buf("oT")
    yb = sb([128, D_MODEL], F32, "yb")
    YBD = Buf("yb")
    yo = [sb([128, D_MODEL], F32, f"yo{i}") for i in range(2)]
    YO = [Buf(f"yo{i}") for i in range(2)]
    psO = PSD[2][:, :].rearrange("p (c t) -> p c t", c=8)
    gcnt = 0
    GELU_C = 2.0 * 0.7978845608028654
    n_tiles_d = NT_OWN if ntile_d is None else ntile_d
    for ti in range(n_tiles_d):
        tok = ti * 128
        s = ti % 2
        dma("sp", x2s[s][:], x2_d[tok:tok + 128, :], f"x2s{s}", [X2D[ti]], [X2S[s]])
        act(junk[:], x2s[s][:], AF.Square, [X2S[s]], [JUNK, ST], accum=st[:, 0:1])
        act(st[:, 1:2], st[:, 0:1], AF.Sqrt, [ST], [ST], bias=EPS, scale=1.0 / D_MODEL)
        recip(st[:, 2:3], st[:, 1:2], [ST], [ST])
        stt(hn[:], x2s[s][:], st[:, 2:3], g2bc[:], ALU.mult, ALU.mult, [X2S[s], ST, CONST], [HN])
        acopy(hnb[:], hn[:], [HN], [HNB])
        for dc in range(8):
            tr(psbf(6)[:, dc * 128:(dc + 1) * 128], hnb[:, dc * 128:(dc + 1) * 128], ident_b[:],
               [HNB, CONST], [PSB[6]])
        acopy(hnT[:], psbf(6).rearrange("p (c t) -> p c t", c=8), [PSB[6]], [HNT])
        for jj in range(16):
            b = jj // 4
            for dc in range(8):
                mm(PS[b][:, (jj % 4) * 128:(jj % 4 + 1) * 128], wqb[:, dc, jj * 128:(jj + 1) * 128],
                   hnT[:, dc, :], dc == 0, dc == 7, [WQB, HNT], [PSB[b]])
        for b in range(4):
            acopy(qTs[:, 4 * b:4 * b + 4, :], PS[b].rearrange("p (a t) -> p a t", a=4), [PSB[b]], [QTS])
        for jj in range(16):
            b = jj // 4
            mm(PS[b][:, (jj % 4) * 128:(jj % 4 + 1) * 128], qTs[:, jj, :], keysT[:, jj % 2, :], True, True,
               [QTS, CONST], [PSB[b]])
        for b in range(4):
            acopy(S[:, 4 * b:4 * b + 4, :], PS[b].rearrange("p (a t) -> p a t", a=4), [PSB[b]], [SB_])

        def top16(src_ap, n, tv_ap, ti_ap, R, W):
            P.op("dve", lambda h: h.max(out=tv_ap[:, 0:8], in_=src_ap), R, W)
            P.op("dve", lambda h: h.max_index(out=ti_ap[:, 0:8], in_max=tv_ap[:, 0:8], in_values=src_ap), R, W)
            P.op("dve", lambda h: h.match_replace(out=wk[:, 0:n], in_to_replace=tv_ap[:, 0:8], in_values=src_ap,
                                                  imm_value=-1e30), R, W)
            P.op("dve", lambda h: h.max(out=tv_ap[:, 8:16], in_=wk[:, 0:n]), R, W)
            P.op("dve", lambda h: h.max_index(out=ti_ap[:, 8:16], in_max=tv_ap[:, 8:16], in_values=wk[:, 0:n]), R, W)

        for jj in range(16):
            top16(S[:, jj, :], 128, tv[:, jj, :], tiu[:, jj, :], [SB_], [TK])
        vcopy("dve", tif[:], tiu[:], [TK], [TK])
        tvv = tv[:].rearrange("p (h a) k -> p h a k", a=2)
        tifv = tif[:].rearrange("p (h a) k -> p h a k", a=2)
        tt("dve", cand[:], tvv[:, :, 0, :].unsqueeze(3).to_broadcast([128, 8, 16, 16]),
           tvv[:, :, 1, :].unsqueeze(2).to_broadcast([128, 8, 16, 16]), ALU.add, [TK], [CAND])
        for hh in range(8):
            top16(cand[:, hh, :, :].rearrange("p a b -> p (a b)"), 256, tops[:, hh, :], posu[:, hh, :],
                  [CAND], [TK2])
        ts("dve", piu[:], posu[:], 4, None, ALU.logical_shift_right, None, [TK2], [TK2])
        ts("dve", pju[:], posu[:], 15, None, ALU.bitwise_and, None, [TK2], [TK2])
        vcopy("dve", pif[:], piu[:], [TK2], [TK2])
        vcopy("dve", pjf[:], pju[:], [TK2], [TK2])
        io16 = iota16[:].unsqueeze(1).unsqueeze(1).to_broadcast([128, 8, 16, 16])
        for (pf, col, dst) in ((pif, 0, i1s), (pjf, 1, i2s)):
            tt("dve", oh[:], io16, pf[:].unsqueeze(3).to_broadcast([128, 8, 16, 16]), ALU.is_equal,
               [TK2, CONST], [OH])
            tt("dve", oh[:], oh[:], tifv[:, :, col, :].unsqueeze(2).to_broadcast([128, 8, 16, 16]), ALU.mult,
               [OH, TK], [OH])
            reduce_add(dst[:], oh[:], [OH], [EG])
        stt(ef[:], i1s[:].rearrange("p h k -> p (h k)"), 128.0, i2s[:].rearrange("p h k -> p (h k)"),
            ALU.mult, ALU.add, [EG], [EG])
        tt("dve", gw[:], tops[:], tops[:, :, 0:1].to_broadcast([128, 8, 16]), ALU.subtract, [TK2], [EG])
        act(gw[:], gw[:], AF.Exp, [EG], [EG])
        reduce_add(ssum[:], gw[:], [EG], [EG])
        recip(ssum[:], ssum[:], [EG], [EG])
        tt("dve", gw[:], gw[:], ssum[:].unsqueeze(2).to_broadcast([128, 8, 16]), ALU.mult, [EG], [EG])
        tr(PS[6][:, 0:128], ef[:], ident_f[:], [EG, CONST], [PSB[6]])
        tr(PS[6][:, 128:256], gw[:].rearrange("p h k -> p (h k)"), ident_f[:], [EG, CONST], [PSB[6]])
        vcopy("dve", eTu[:], PS[6][:, 0:128], [PSB[6]], [ET])
        acopy(gT[:], PS[6][:, 128:256], [PSB[6]], [GT])
        if debug and ti == 0:
            o = dout("d_e", [128, 128], U32)
            dma("sp", o, eTu[:], "dbg", [ET], [])
            o = dout("d_g", [128, 128], F32)
            dma("sp", o, gT[:], "dbg", [GT], [])
        for t in range(128):
            sl = gcnt % 4
            pb = (gcnt % 2) * 2
            gcnt += 1
            P.dma("pool", (lambda o, ia: (lambda h: h.indirect_dma_start(
                out=o, out_offset=None, in_=peer_u[:, :],
                in_offset=bass.IndirectOffsetOnAxis(ap=ia, axis=0))))(gsl[sl][:], eTu[:, t:t + 1]),
                f"g{sl}", [ET], [GS[sl]])
            mm(PS[pb][:, :], sel[:, t, :], hnb[:, 0:512], True, True, [HNB, CONST], [PSB[pb], PSB[pb + 1]])
            mm(PS[pb + 1][:, :], sel[:, t, :], hnb[:, 512:1024], True, True, [HNB, CONST], [PSB[pb], PSB[pb + 1]])
            W = [HTD] if t in (0, 127) else []
            P.op("dve", (lambda a, b_, c: (lambda h: h.scalar_tensor_tensor(
                out=junk[:], in0=a, scalar=1.0, in1=b_, op0=ALU.mult, op1=ALU.mult, accum_out=c)))(
                gsl[sl][:], PSD[pb // 2][:, :], hT[:, t:t + 1]), [GS[sl], PSB[pb], PSB[pb + 1]], W)
        act(gl[:], hT[:], AF.Square, [HTD], [GH])
        ts("dve", gl[:], gl[:], 0.044715, 1.0, ALU.mult, ALU.add, [GH], [GH])
        tt("dve", gl[:], gl[:], hT[:], ALU.mult, [GH, HTD], [GH])
        act(gl2[:], gl[:], AF.Sigmoid, [GH], [GH], scale=GELU_C)
        tt("dve", gl2[:], gl2[:], hT[:], ALU.mult, [GH, HTD], [GH])
        tt("dve", ghT[:], gl2[:], gT[:], ALU.mult, [GH, GT], [GH])
        for t in range(128):
            sl = gcnt % 4
            gcnt += 1
            P.dma("pool", (lambda o, ia: (lambda h: h.indirect_dma_start(
                out=o, out_offset=None, in_=peer_v[:, :],
                in_offset=bass.IndirectOffsetOnAxis(ap=ia, axis=0))))(gsl[sl][:], eTu[:, t:t + 1]),
                f"g{sl}", [ET], [GS[sl]])
            for c in range(8):
                mm(psO[:, c, t:t + 1], gsl[sl][:, c * 128:(c + 1) * 128], ghT[:, t:t + 1], True, True,
                   [GS[sl], GH], [PSB[4], PSB[5]])
        acopy(oT[:], psO, [PSB[4], PSB[5]], [OT])
        for c in range(8):
            tr(PSD[3][:, c * 128:(c + 1) * 128], oT[:, c, :], ident_f[:], [OT, CONST], [PSB[6], PSB[7]])
        tt("dve", yb[:], PSD[3][:, :], x2s[s][:], ALU.add, [PSB[6], PSB[7], X2S[s]], [YBD])
        act(junk[:], yb[:], AF.Square, [YBD], [JUNK, ST], accum=st[:, 4:5])
        act(st[:, 5:6], st[:, 4:5], AF.Sqrt, [ST], [ST], bias=EPS, scale=1.0 / D_MODEL)
        recip(st[:, 6:7], st[:, 5:6], [ST], [ST])
        stt(yo[s][:], yb[:], st[:, 6:7], gfbc[:], ALU.mult, ALU.mult, [YBD, ST, CONST], [YO[s]])
        dma("sp", out_d[tok:tok + 128, :], yo[s][:], f"yo{s}", [YO[s]], [])
    barrier()
    P.emit()
    return nc, dbg


def make_in_maps(inputs):
    x = np.ascontiguousarray(np.asarray(inputs["x"], dtype=np.float32))
    shared = {}
    for k in ("norm1_g", "w_in", "q_norm_g", "k_norm_g", "conv_b", "conv_ln_g", "conv_ln_b", "w_out",
              "norm2_g", "peer_wq", "peer_keys", "peer_u", "peer_v", "final_g"):
        shared[k] = np.ascontiguousarray(np.asarray(inputs[k], dtype=np.float32))
    shared["conv_dw"] = np.ascontiguousarray(np.asarray(inputs["conv_dw"], dtype=np.float32).reshape(CONV_W, 512))
    maps = []
    for c in range(8):
        b, hf = c // 2, c % 2
        own0 = hf * NOWN
        oth0 = (1 - hf) * NOWN
        m = dict(shared)
        m["x_own"] = x[b, own0:own0 + NOWN]
        m["x_oth"] = x[b, oth0:oth0 + NOWN]
        halo = np.zeros((32, D_MODEL), np.float32)
        if hf == 1:
            halo[0:15] = x[b, own0 - 15:own0]
        else:
            halo[16:31] = x[b, own0 + NOWN:own0 + NOWN + 15]
        m["x_halo"] = halo
        pos = np.concatenate([np.arange(own0, own0 + NOWN), np.arange(oth0, oth0 + NOWN)])
        pos = pos.reshape(NT_ALL, 128).T
        rc = np.stack([pos // 64, pos % 64], axis=-1).astype(np.float32)
        m["rowcol"] = np.ascontiguousarray(rc)
        maps.append(m)
    return maps


_NC_CACHE = {}


def kernel(**inputs):
    if "nc" not in _NC_CACHE:
        _NC_CACHE["nc"] = build_program("D", False)[0]
    nc = _NC_CACHE["nc"]
    maps = make_in_maps(inputs)
    res = run_bass_kernel_spmd(nc, maps, core_ids=list(range(8)))
    out = np.empty((4, SEQ, D_MODEL), np.float32)
    for c in range(8):
        b, hf = c // 2, c % 2
        out[b, hf * NOWN:(hf + 1) * NOWN] = res.results[c]["out"]
    return out
```

```python
import bisect
from contextlib import ExitStack
import numpy as np
import concourse.bass as bass
import concourse.mybir as mybir
from concourse.bass_utils import run_bass_kernel_spmd

F32 = mybir.dt.float32
F32R = mybir.dt.float32r
BF16 = mybir.dt.bfloat16
U32 = mybir.dt.uint32
I32 = mybir.dt.int32
ALU = mybir.AluOpType
AF = mybir.ActivationFunctionType
AX = mybir.AxisListType

SEM_LIMIT = 30000


class _Cut(Exception):
    pass


class Buf:
    def __init__(self, name, excl=False):
        self.name = name
        self.last_w = None
        self.readers = []
        self.excl = excl


class DSem:
    def __init__(self, prog, name):
        self.prog = prog
        self.name = name
        self.sem = prog.nc.alloc_semaphore(name=name)
        self.total = 0
        self.group_ends = []
        self.open = False

    def need(self, v):
        i = bisect.bisect_left(self.group_ends, v)
        if i < len(self.group_ends):
            return self.group_ends[i]
        self.group_ends.append(self.total)
        self.open = False
        return self.total


class Prog:
    ENG = ("pe", "dve", "act", "pool", "sp")

    def __init__(self, nc):
        self.nc = nc
        self.handles = {"pe": nc.tensor, "dve": nc.vector, "act": nc.scalar,
                        "pool": nc.gpsimd, "sp": nc.sync}
        self.lists = {e: [] for e in self.ENG}
        self.cnt = {e: 0 for e in self.ENG}
        self.gen = {e: 0 for e in self.ENG}
        self.sems = {e: [nc.alloc_semaphore(name=f"s_{e}_0")] for e in self.ENG}
        self.waited = {}
        self.same_engine_sync = {"pe": False, "dve": True, "act": True,
                                 "pool": True, "sp": False}
        self.dsems = {}
        self.old_dsems = []
        self.n_inst = 0

    def _wait(self, eng, ev):
        kind, key, val = ev
        if kind == "e":
            e2, g = key
            if e2 == eng and not self.same_engine_sync[eng]:
                return
            sem = self.sems[e2][g]
            wkey = (eng, "e", e2, g)
            need = val
        else:
            ds = key
            need = ds.need(val)
            sem = ds.sem
            wkey = (eng, "d", ds.name)
        if self.waited.get(wkey, 0) >= need:
            return
        self.waited[wkey] = need
        self.lists[eng].append(("wait", sem, need))

    def _deps(self, eng, reads, writes):
        for b in reads:
            if b.last_w is not None:
                self._wait(eng, b.last_w)
            if b.excl:
                for ev in b.readers:
                    if ev[0] == "e" and ev[1][0] != eng:
                        self._wait(eng, ev)
        for b in writes:
            if b.last_w is not None:
                self._wait(eng, b.last_w)
            for ev in b.readers:
                self._wait(eng, ev)

    def _commit(self, ev, reads, writes):
        for b in writes:
            b.last_w = ev
            b.readers = []
        for b in reads:
            if b not in writes:
                b.readers.append(ev)
                if len(b.readers) > 64:
                    b.readers = b.readers[-64:]

    def op(self, eng, fn, reads=(), writes=()):
        reads = list(reads)
        writes = list(writes)
        self._deps(eng, reads, writes)
        if self.cnt[eng] >= SEM_LIMIT:
            self.gen[eng] += 1
            self.cnt[eng] = 0
            self.sems[eng].append(self.nc.alloc_semaphore(name=f"s_{eng}_{self.gen[eng]}"))
        self.cnt[eng] += 1
        g = self.gen[eng]
        self.lists[eng].append(("op", fn, self.sems[eng][g]))
        ev = ("e", (eng, g), self.cnt[eng])
        self._commit(ev, reads, writes)
        self.n_inst += 1
        return ev

    def dsem(self, name):
        if name not in self.dsems:
            self.dsems[name] = DSem(self, "d_" + name)
        return self.dsems[name]

    def dma(self, queue, fn, dsem, reads=(), writes=()):
        ds = self.dsem(dsem) if isinstance(dsem, str) else dsem
        if ds.total >= SEM_LIMIT and not ds.open and isinstance(dsem, str):
            self._wait(queue, ("d", ds, ds.total))
            self._dgen = getattr(self, "_dgen", 0) + 1
            ds = DSem(self, f"d_{dsem}_{self._dgen}")
            self.dsems[dsem] = ds
            self.old_dsems.append(ds)
        reads = list(reads)
        writes = list(writes)
        self._deps(queue, reads, writes)
        if (not ds.open) and ds.total > 0:
            self._wait(queue, ("d", ds, ds.total))
        ds.total += 16
        ds.open = True
        self.lists[queue].append(("dma", fn, ds.sem))
        ev = ("d", ds, ds.total)
        self._commit(ev, reads, writes)
        self.n_inst += 1
        return ev

    def wait_all(self, eng, bufs):
        for b in bufs:
            if b.last_w is not None:
                self._wait(eng, b.last_w)

    def emit(self):
        nc = self.nc
        with nc.Block() as block:
            def mk(ename):
                items = self.lists[ename]

                def body(h):
                    for it in items:
                        if it[0] == "wait":
                            h.wait_ge(it[1], it[2])
                        elif it[0] == "op":
                            it[1](h).then_inc(it[2], 1)
                        else:
                            it[1](h).then_inc(it[2], 16)
                return body
            block.tensor(mk("pe"))
            block.vector(mk("dve"))
            block.scalar(mk("act"))
            block.gpsimd(mk("pool"))
            block.sync(mk("sp"))


D_MODEL = 1024
SEQ = 8192
NOWN = 4096
HD = 64
CONV_W = 31
IN_W = 1792
EPS = 1e-6
NT_OWN = NOWN // 128
NT_ALL = SEQ // 128
TWO_PI = 2.0 * np.pi


def build_program(stop_after="D", debug=False, grp_list=None, pool_eng="pool", cut=None, ntile_d=None):
    nc = bass.Bass("TRN2", target_bir_lowering=False)
    P = Prog(nc)
    dbg = {}

    def din(name, shape, dt=F32):
        return nc.dram_tensor(name, list(shape), dt, kind="ExternalInput").ap()

    x_own = din("x_own", [NOWN, D_MODEL])
    x_oth = din("x_oth", [NOWN, D_MODEL])
    x_halo = din("x_halo", [32, D_MODEL])
    rowcol = din("rowcol", [128, NT_ALL, 2])
    norm1_g = din("norm1_g", [D_MODEL])
    w_in = din("w_in", [D_MODEL, IN_W])
    q_norm_g = din("q_norm_g", [HD])
    k_norm_g = din("k_norm_g", [HD])
    conv_dw = din("conv_dw", [CONV_W, 512])
    conv_b = din("conv_b", [512])
    conv_ln_g = din("conv_ln_g", [512])
    conv_ln_b = din("conv_ln_b", [512])
    w_out = din("w_out", [D_MODEL, D_MODEL])
    norm2_g = din("norm2_g", [D_MODEL])
    peer_wq = din("peer_wq", [D_MODEL, 2048])
    peer_keys = din("peer_keys", [2, 128, 128])
    peer_u = din("peer_u", [16384, D_MODEL])
    peer_v = din("peer_v", [16384, D_MODEL])
    final_g = din("final_g", [D_MODEL])
    out_d = nc.dram_tensor("out", [NOWN, D_MODEL], F32, kind="ExternalOutput").ap()
    x2_d = nc.dram_tensor("x2_scratch", [NOWN, D_MODEL], F32, kind="Internal").ap()
    uv_d = nc.dram_tensor("uv_scratch", [16384, 2 * D_MODEL], BF16, kind="Internal").ap()

    def dout(name, shape, dt=F32):
        dbg[name] = nc.dram_tensor(name, list(shape), dt, kind="ExternalOutput").ap()
        return dbg[name]

    _n = [0]
    cst = ExitStack()
    pers = ExitStack()
    scope = [ExitStack()]

    def sb(shape, dt=F32, name=None, persistent=False):
        _n[0] += 1
        nm = name or f"sb{_n[0]}"
        if persistent == "c":
            return cst.enter_context(nc.sbuf_tensor(nm, list(shape), dt, side="right"))
        if persistent:
            return pers.enter_context(nc.sbuf_tensor(nm, list(shape), dt, side="right"))
        return scope[0].enter_context(nc.sbuf_tensor(nm, list(shape), dt, side="left"))

    def new_scope():
        barrier()
        scope[0].close()
        scope[0] = ExitStack()

    def mm(out, lhsT, rhs, start, stop, R, W):
        P.op("pe", lambda h: h.matmul(out, lhsT=lhsT, rhs=rhs, start=start, stop=stop), R, W)

    def tr(out, in_, ident, R, W):
        P.op("pe", lambda h: h.transpose(out=out, in_=in_, identity=ident), R, W)

    def act(out, in_, func, R, W, bias=None, scale=None, accum=None):
        kw = {}
        if bias is not None:
            kw["bias"] = bias
        if scale is not None:
            kw["scale"] = scale
        if accum is not None:
            kw["accum_out"] = accum
        P.op("act", lambda h: h.activation(out=out, in_=in_, func=func, **kw), R, W)

    def acopy(out, in_, R, W):
        P.op("act", lambda h: h.copy(out=out, in_=in_), R, W)

    def tt(eng, out, in0, in1, op, R, W):
        P.op(eng, lambda h: h.tensor_tensor(out=out, in0=in0, in1=in1, op=op), R, W)

    def ts(eng, out, in0, s1, s2, op0, op1, R, W):
        if op1 is None:
            P.op(eng, lambda h: h.tensor_scalar(out=out, in0=in0, scalar1=s1, scalar2=None, op0=op0), R, W)
        else:
            P.op(eng, lambda h: h.tensor_scalar(out=out, in0=in0, scalar1=s1, scalar2=s2, op0=op0, op1=op1), R, W)

    def stt(out, in0, scalar, in1, op0, op1, R, W):
        P.op("dve", lambda h: h.scalar_tensor_tensor(out=out, in0=in0, scalar=scalar, in1=in1, op0=op0, op1=op1), R, W)

    def vcopy(eng, out, in_, R, W):
        P.op(eng, lambda h: h.tensor_copy(out=out, in_=in_), R, W)

    def recip(out, in_, R, W):
        P.op("dve", lambda h: h.reciprocal(out=out, in_=in_), R, W)

    def reduce_add(out, in_, R, W):
        P.op("dve", lambda h: h.tensor_reduce(out=out, in_=in_, axis=AX.X, op=ALU.add), R, W)

    def memset(eng, ap, val, W):
        P.op(eng, lambda h: h.memset(ap, val), [], W)

    def dma(q, out, in_, ds, R, W, slow=False):
        if slow:
            P.dma(q, lambda h: h.dma_start(out=out, in_=in_, allow_slow_non_contiguous=True), ds, R, W)
        else:
            P.dma(q, lambda h: h.dma_start(out=out, in_=in_), ds, R, W)

    def barrier():
        evs = []
        for e in P.ENG:
            if P.cnt[e] > 0:
                evs.append(("e", (e, P.gen[e]), P.cnt[e]))
        for ds in list(P.dsems.values()):
            if ds.total > 0:
                evs.append(("d", ds, ds.total))
        for e in P.ENG:
            for ev in evs:
                if ev[0] == "e" and ev[1][0] == e:
                    continue
                P._wait(e, ev)

    PSD = [nc.alloc_psum_tensor(f"pd{i}", [128, 1024], F32) for i in range(4)]
    PS = [PSD[i // 2][:, (i % 2) * 512:(i % 2 + 1) * 512] for i in range(8)]
    PSB = [Buf(f"bank{i}", excl=True) for i in range(8)]

    def psbf(i):
        return PS[i].bitcast(BF16)

    CONST = Buf("const")
    ident_f = sb([128, 128], F32, "ident_f", persistent="c")
    ident_b = sb([128, 128], BF16, "ident_b", persistent="c")
    iot = sb([128, 128], F32, "iot", persistent="c")
    P.op("pool", lambda h: h.iota(iot[:], pattern=[[1, 128]], base=0, channel_multiplier=-1,
                                  allow_small_or_imprecise_dtypes=True), [], [CONST])
    ts("dve", ident_f[:], iot[:], 0.0, None, ALU.is_equal, None, [CONST], [CONST])
    vcopy("dve", ident_b[:], ident_f[:], [CONST], [CONST])
    ones_f = sb([128, 128], F32, "ones_f", persistent="c")
    memset("dve", ones_f[:], 1.0 / 512.0, [CONST])
    iota16 = sb([128, 16], F32, "iota16", persistent="c")
    P.op("pool", lambda h: h.iota(iota16[:], pattern=[[1, 16]], base=0, channel_multiplier=0,
                                  allow_small_or_imprecise_dtypes=True), [], [CONST])

    g1 = sb([128, 8], F32, "g1", persistent="c")
    dma("sp", g1[:], norm1_g.rearrange("(c p) -> p c", p=128), "c0", [], [CONST], slow=True)
    cb = sb([128, 4], F32, "cb", persistent="c")
    lg = sb([128, 4], F32, "lg", persistent="c")
    lb = sb([128, 4], F32, "lb", persistent="c")
    dma("sp", cb[:], conv_b.rearrange("(c p) -> p c", p=128), "c0", [], [CONST], slow=True)
    dma("sp", lg[:], conv_ln_g.rearrange("(c p) -> p c", p=128), "c0", [], [CONST], slow=True)
    dma("sp", lb[:], conv_ln_b.rearrange("(c p) -> p c", p=128), "c0", [], [CONST], slow=True)
    G10 = sb([128, 10, 64], F32, "G10", persistent="c")
    dma("sp", G10[:, 0, :], q_norm_g.partition_broadcast(128), "c0", [], [CONST])
    dma("sp", G10[:, 8, :], k_norm_g.partition_broadcast(128), "c0", [], [CONST])
    ts("dve", G10[:, 0, :], G10[:, 0, :], HD ** -0.5, None, ALU.mult, None, [CONST], [CONST])
    for j in range(1, 8):
        vcopy("dve", G10[:, j, :], G10[:, 0, :], [CONST], [CONST])
    vcopy("dve", G10[:, 9, :], G10[:, 8, :], [CONST], [CONST])
    KT = sb([128, SEQ], BF16, "KT", persistent=True)
    VA = sb([128, NT_ALL, 2, 128], BF16, "VA", persistent=True)
    QT = sb([128, 4, NOWN], BF16, "QT", persistent=True)
    htstack = ExitStack()
    HT = htstack.enter_context(nc.sbuf_tensor("HT", [128, 4, NOWN + 32], BF16, side="left"))

    rc_t = sb([128, NT_ALL, 2], F32, "rc_t")
    dma("sp", rc_t[:], rowcol, "c0", [], [CONST])
    invf = sb([128, 16], F32, "invf")
    act(invf[:], iota16[:], AF.Exp, [CONST], [CONST], scale=-float(np.log(10000.0)) / 16.0)
    NTAB = NT_ALL * 32
    cos_t = sb([128, NT_ALL, 2, 16], F32, "cos_t")
    sin_t = sb([128, NT_ALL, 2, 16], F32, "sin_t")
    w1b = sb([128, 8, IN_W], BF16, "w1b")
    W1B = Buf("w1b")
    with nc.sbuf_tensor("rr_k", [128, NTAB], I32, side="left") as rr_k, \
            nc.sbuf_tensor("rr_f", [128, NTAB], F32, side="left") as rr_f, \
            nc.sbuf_tensor("rr_a", [128, NTAB], F32, side="left") as rr_a, \
            nc.sbuf_tensor("ang", [128, NT_ALL, 2, 16], F32, side="left") as ang:
        tt("dve", ang[:], rc_t[:].unsqueeze(3).to_broadcast([128, NT_ALL, 2, 16]),
           invf[:].unsqueeze(1).unsqueeze(1).to_broadcast([128, NT_ALL, 2, 16]), ALU.mult, [CONST], [CONST])
        angf = ang[:].rearrange("p a b c -> p (a b c)")
        TMPB = Buf("ropetmp")
        for tab, shift in ((sin_t, 0.0), (cos_t, np.pi / 2)):
            tf = tab[:].rearrange("p a b c -> p (a b c)")
            ts("dve", rr_a[:], angf, float(shift), None, ALU.add, None, [CONST], [TMPB])
            ts("dve", rr_k[:], rr_a[:], 1.0 / TWO_PI, 0.5, ALU.mult, ALU.add, [TMPB], [TMPB])
            vcopy("dve", rr_f[:], rr_k[:], [TMPB], [TMPB])
            stt(rr_f[:], rr_f[:], -TWO_PI, rr_a[:], ALU.mult, ALU.add, [TMPB], [TMPB])
            ts("dve", rr_a[:], rr_f[:], -float(np.pi), TWO_PI, ALU.is_lt, ALU.mult, [TMPB], [TMPB])
            tt("dve", rr_f[:], rr_f[:], rr_a[:], ALU.add, [TMPB], [TMPB])
            ts("dve", rr_a[:], rr_f[:], float(np.pi), TWO_PI, ALU.is_gt, ALU.mult, [TMPB], [TMPB])
            tt("dve", rr_a[:], rr_f[:], rr_a[:], ALU.subtract, [TMPB], [TMPB])
            ts("dve", rr_a[:], rr_a[:], float(np.pi), -float(np.pi), ALU.min, ALU.max, [TMPB], [TMPB])
            act(tf, rr_a[:], AF.Sin, [TMPB], [CONST])
        barrier()
    with nc.sbuf_tensor("stg0", [128, IN_W], F32, side="left") as stg0, \
            nc.sbuf_tensor("stg1", [128, IN_W], F32, side="left") as stg1:
        stg = [stg0, stg1]
        STG = [Buf("stg0"), Buf("stg1")]
        for dc in range(8):
            s = dc % 2
            dma("sp", stg[s][:], w_in[dc * 128:(dc + 1) * 128, :], f"stg{s}", [], [STG[s]])
            ts("dve", w1b[:, dc, 0:512].rearrange("p (j g d) -> p j g d", j=4, g=2),
               stg[s][:, 0:512].rearrange("p (g j d) -> p j g d", g=2, j=4),
               g1[:, dc:dc + 1], None, ALU.mult, None, [STG[s], CONST], [W1B])
            ts("pool", w1b[:, dc, 512:IN_W], stg[s][:, 512:IN_W], g1[:, dc:dc + 1], None, ALU.mult, None,
               [STG[s], CONST], [W1B])
        barrier()

    if stop_after == "0":
        o = dout("d_cos", [128, NT_ALL, 2, 16], F32)
        dma("sp", o, cos_t[:], "dbg", [], [])
        o = dout("d_sin", [128, NT_ALL, 2, 16], F32)
        dma("sp", o, sin_t[:], "dbg", [], [])
        o = dout("d_w1b", [128, 8, IN_W], BF16)
        dma("sp", o, w1b[:], "dbg", [], [])
        o = dout("d_G10", [128, 10, 64], F32)
        dma("sp", o, G10[:], "dbg", [], [])
        barrier()
        P.emit()
        return nc, dbg
    KTB = [Buf(f"kt{i}") for i in range(NT_ALL)]
    VAB = [Buf(f"va{i}") for i in range(NT_ALL)]
    QTB = [Buf(f"qt{i}") for i in range(NT_OWN)]
    HTB = [Buf(f"ht{i}") for i in range(NT_OWN // 4 + 1)]
    VINIT = Buf("vinit")
    memset("pool", VA[:, :, :, 64:128].rearrange("p a b c -> p (a b) c"), 1.0, [VINIT])
    for b in VAB:
        b.last_w = VINIT.last_w
    memset("pool", HT[:, :, NOWN + 30:NOWN + 32], 0.0, [HTB[NT_OWN // 4]])

    xin = [sb([128, D_MODEL], F32, f"xin{i}") for i in range(2)]
    XIN = [Buf(f"xin{i}") for i in range(2)]
    junk = sb([128, D_MODEL], BF16, "junk")
    JUNK = Buf("junk")
    xs = [sb([128, D_MODEL], BF16, f"xs{i}") for i in range(2)]
    XS = [Buf(f"xs{i}") for i in range(2)]
    xT = [sb([128, 8, 512], BF16, f"xT{i}") for i in range(2)]
    XT = [Buf(f"xT{i}") for i in range(2)]
    st = sb([128, 8], F32, "st")
    ST = Buf("st")
    sq = sb([128, 640], F32, "sq")
    SQ = Buf("sq")
    ssq = sb([128, 10], F32, "ssq")
    qn = sb([128, 10, 64], F32, "qn")
    QN = Buf("qn")
    t1 = sb([128, 10, 64], F32, "t1")
    t2 = sb([128, 10, 64], F32, "t2")
    T12 = Buf("t12")
    qr = sb([128, 640], BF16, "qr")
    QR = Buf("qr")
    sg = sb([128, 512], F32, "sg")
    SG = Buf("sg")

    def rms_rows(xt_ap, XB, np_, out_bf, OB):
        act(junk[0:np_, :], xt_ap, AF.Square, [XB], [JUNK, ST], accum=st[0:np_, 0:1])
        act(st[0:np_, 1:2], st[0:np_, 0:1], AF.Sqrt, [ST], [ST], bias=EPS, scale=1.0 / D_MODEL)
        recip(st[0:np_, 2:3], st[0:np_, 1:2], [ST], [ST])
        ts("dve", out_bf, xt_ap, st[0:np_, 2:3], None, ALU.mult, None, [XB, ST], [OB])

    try:
        n_groups = NT_ALL // 4
        tile_ctr = 0
        for grp in (grp_list if grp_list is not None else range(n_groups + 1)):
            halo = grp == n_groups
            own = grp < NT_OWN // 4
            gs = grp % 2
            nsub = 1 if halo else 4
            for sub in range(nsub):
                ti = grp * 4 + sub
                s = tile_ctr % 2
                tile_ctr += 1
                np_ = 32 if halo else 128
                if halo:
                    src = x_halo[:, :]
                elif own:
                    src = x_own[ti * 128:(ti + 1) * 128, :]
                else:
                    src = x_oth[(ti - NT_OWN) * 128:(ti - NT_OWN + 1) * 128, :]
                dma("sp", xin[s][0:np_, :], src, f"xin{s}", [], [XIN[s]])
                rms_rows(xin[s][0:np_, :], XIN[s], np_, xs[s][0:np_, :], XS[s])
                if cut == 1:
                    raise _Cut()
                pb = ti % 2
                for dc in range(8):
                    tr(psbf(pb)[:, dc * 128:dc * 128 + np_], xs[s][0:np_, dc * 128:(dc + 1) * 128],
                       ident_b[0:np_, 0:np_], [XS[s], CONST], [PSB[pb]])
                acopy(xT[gs][:, :, sub * 128:sub * 128 + np_],
                      psbf(pb).rearrange("p (c t) -> p c t", c=8)[:, :, 0:np_], [PSB[pb]], [XT[gs]])
                if cut == 2:
                    raise _Cut()
                if halo:
                    continue
                c0 = 0 if own else 512
                ncol = 768 - c0
                for (a, b) in (((0, 512), (512, 768)) if own else ((512, 768),)):
                    bank = 2 if a == 0 else 3
                    for dc in range(8):
                        mm(PS[bank][:, 0:b - a], xT[gs][:, dc, sub * 128:(sub + 1) * 128], w1b[:, dc, a:b],
                           dc == 0, dc == 7, [XT[gs], W1B], [PSB[bank]])
                if cut == 3:
                    raise _Cut()
                h0 = 0 if own else 8
                nh = 10 - h0
                if own:
                    act(sq[:, 0:512], PS[2][:, 0:512], AF.Square, [PSB[2]], [SQ])
                act(sq[:, 512:640], PS[3][:, 0:128], AF.Square, [PSB[3]], [SQ])
                reduce_add(ssq[:, h0:10], sq[:, h0 * 64:640].rearrange("p (h d) -> p h d", d=64), [SQ], [ST])
                act(ssq[:, h0:10], ssq[:, h0:10], AF.Sqrt, [ST], [ST], bias=EPS, scale=1.0 / HD)
                recip(ssq[:, h0:10], ssq[:, h0:10], [ST], [ST])
                if own:
                    tt("dve", qn[:, 0:8, :], PS[2][:, 0:512].rearrange("p (h d) -> p h d", d=64),
                       ssq[:, 0:8].unsqueeze(2).to_broadcast([128, 8, 64]), ALU.mult, [PSB[2], ST], [QN])
                tt("dve", qn[:, 8:10, :], PS[3][:, 0:128].rearrange("p (h d) -> p h d", d=64),
                   ssq[:, 8:10].unsqueeze(2).to_broadcast([128, 2, 64]), ALU.mult, [PSB[3], ST], [QN])
                if cut == 4:
                    raise _Cut()
                acopy(VA[:, ti, :, 0:64], PS[3][:, 128:256].rearrange("p (g d) -> p g d", g=2), [PSB[3]], [VAB[ti]])
                tt(pool_eng, qn[:, h0:10, :], qn[:, h0:10, :], G10[:, h0:10, :], ALU.mult, [QN, CONST], [QN])
                if cut == 5:
                    raise _Cut()
                for rc in range(2):
                    qv = qn[:, h0:10, rc * 32:(rc + 1) * 32].rearrange("p h (a d) -> p h a d", a=2)
                    cosb = cos_t[:, ti, rc, :].unsqueeze(1).unsqueeze(1).to_broadcast([128, nh, 2, 16])
                    sinb = sin_t[:, ti, rc, :].unsqueeze(1).to_broadcast([128, nh, 16])
                    t1v = t1[:, h0:10, rc * 32:(rc + 1) * 32].rearrange("p h (a d) -> p h a d", a=2)
                    t2v = t2[:, h0:10, rc * 32:(rc + 1) * 32].rearrange("p h (a d) -> p h a d", a=2)
                    tt("dve", t1v, qv, cosb, ALU.mult, [QN, CONST], [T12])
                    tt(pool_eng, t2v[:, :, 0, :], qv[:, :, 1, :], sinb, ALU.mult, [QN, CONST], [T12])
                    tt(pool_eng, t2v[:, :, 1, :], qv[:, :, 0, :], sinb, ALU.mult, [QN, CONST], [T12])
                    qrv = qr[:, h0 * 64:640].rearrange("p (h c) -> p h c", c=64)[:, :, rc * 32:(rc + 1) * 32] \
                        .rearrange("p h (a d) -> p h a d", a=2)
                    tt("dve", qrv[:, :, 0, :], t1v[:, :, 0, :], t2v[:, :, 0, :], ALU.subtract, [T12], [QR])
                    tt("dve", qrv[:, :, 1, :], t1v[:, :, 1, :], t2v[:, :, 1, :], ALU.add, [T12], [QR])
                    if cut == 6:
                        raise _Cut()
                if own:
                    for j in range(4):
                        tr(psbf(4)[:, j * 128:(j + 1) * 128], qr[:, j * 128:(j + 1) * 128], ident_b[:],
                           [QR, CONST], [PSB[4]])
                tr(psbf(4)[:, 512:640], qr[:, 512:640], ident_b[:], [QR, CONST], [PSB[4]])
                if cut == 71:
                    raise _Cut()
                if own:
                    acopy(QT[:, :, ti * 128:(ti + 1) * 128], psbf(4)[:, 0:512].rearrange("p (j t) -> p j t", j=4),
                          [PSB[4]], [QTB[ti]])
                if cut == 72:
                    raise _Cut()
                acopy(KT[:, ti * 128:(ti + 1) * 128], psbf(4)[:, 512:640], [PSB[4]], [KTB[ti]])
                if cut == 7:
                    raise _Cut()
            if own or halo:
                ntok = 32 if halo else 512
                for c in range(4):
                    for part, bank in ((0, 5), (1, 6)):
                        col = 768 + part * 512 + c * 128
                        for dc in range(8):
                            mm(PS[bank][:, 0:ntok], w1b[:, dc, col:col + 128], xT[gs][:, dc, 0:ntok],
                               dc == 0, dc == 7, [XT[gs], W1B], [PSB[bank]])
                    act(sg[:, 0:ntok], PS[6][:, 0:ntok], AF.Sigmoid, [PSB[6]], [SG])
                    if halo:
                        tt("dve", HT[:, c, 0:15], PS[5][:, 0:15], sg[:, 0:15], ALU.mult, [PSB[5], SG], [HTB[0]])
                        tt("dve", HT[:, c, NOWN + 15:NOWN + 30], PS[5][:, 16:31], sg[:, 16:31], ALU.mult,
                           [PSB[5], SG], [HTB[NT_OWN // 4]])
                    else:
                        tt("dve", HT[:, c, 15 + grp * 512:15 + (grp + 1) * 512], PS[5][:, :], sg[:, :], ALU.mult,
                           [PSB[5], SG], [HTB[grp]])
    except _Cut:
        pass
    barrier()

    if debug:
        o = dout("d_KT", [128, SEQ], BF16)
        dma("sp", o, KT[:], "dbg", [], [])
        o = dout("d_QT", [128, 4, NOWN], BF16)
        dma("sp", o, QT[:], "dbg", [], [])
        o = dout("d_VA", [128, NT_ALL, 2, 128], BF16)
        dma("sp", o, VA[:], "dbg", [], [])
        o = dout("d_HT", [128, 4, NOWN + 32], BF16)
        dma("sp", o, HT[:], "dbg", [], [])
        barrier()
    if stop_after == "A":
        P.emit()
        return nc, dbg

    new_scope()
    CT = sb([128, 4, NOWN], BF16, "CT", persistent=True)
    CTB = [Buf(f"ct{i}") for i in range(8)]
    dg = sb([128, 4, CONV_W, 128], BF16, "dg")
    DG = Buf("dg")
    cw = sb([128, 4, CONV_W], F32, "cw")
    cwr = sb([CONV_W, 512], F32, "cwr")
    CW = Buf("cw")
    dma("sp", cwr[:], conv_dw, "cw", [], [CW])
    for c in range(4):
        tr(PS[0][:, c * 32:c * 32 + CONV_W], cwr[:, c * 128:(c + 1) * 128], ident_f[0:CONV_W, 0:CONV_W],
           [CW, CONST], [PSB[0]])
    vcopy("dve", cw[:], PS[0][:, 0:128].rearrange("p (c j) -> p c j", c=4)[:, :, 0:CONV_W], [PSB[0]], [CW])
    for c in range(4):
        for j in range(CONV_W):
            eng = "dve" if (j % 2 == 0) else "pool"
            ts(eng, dg[:, c, j, :], ident_b[:], cw[:, c, j:j + 1], None, ALU.mult, None, [CW, CONST], [DG])
    ybuf = sb([128, 4, 512], F32, "ybuf")
    ysq = sb([128, 4, 512], F32, "ysq")
    YB = [Buf(f"yb{c}") for c in range(4)]
    YS = [Buf(f"ys{c}") for c in range(4)]
    m2 = sb([128, 512], F32, "m2")
    M2 = Buf("m2")
    rstd_c = sb([128, 512], F32, "rstd_c")
    RSC = Buf("rstd_c")
    tmpc = sb([128, 512], F32, "tmpc")
    TMPC = Buf("tmpc")
    for q in range(8):
        hreads = [HTB[q]] + ([HTB[q + 1]] if q + 1 <= 8 else []) + ([HTB[q - 1]] if q > 0 else [])
        for c in range(4):
            bank = c % 2
            for j in range(CONV_W):
                mm(PS[bank][:, :], dg[:, c, j, :], HT[:, c, q * 512 + j:q * 512 + j + 512],
                   j == 0, j == CONV_W - 1, hreads + [DG], [PSB[bank]])
            act(ybuf[:, c, :], PS[bank][:, :], AF.Identity, [PSB[bank], CONST], [YB[c]], bias=cb[:, c:c + 1])
            act(ysq[:, c, :], ybuf[:, c, :], AF.Square, [YB[c]], [YS[c]])
        for c in range(4):
            mm(PS[2][:, :], ones_f[:], ybuf[:, c, :], c == 0, c == 3, [YB[c], CONST], [PSB[2]])
        for c in range(4):
            mm(PS[3][:, :], ones_f[:], ysq[:, c, :], c == 0, c == 3, [YS[c], CONST], [PSB[3]])
        act(m2[:], PS[2][:, :], AF.Square, [PSB[2]], [M2])
        tt("dve", m2[:], PS[3][:, :], m2[:], ALU.subtract, [PSB[3], M2], [M2])
        act(m2[:], m2[:], AF.Sqrt, [M2], [M2], bias=EPS, scale=1.0)
        recip(rstd_c[:], m2[:], [M2], [RSC])
        for c in range(4):
            tt("dve", tmpc[:], ybuf[:, c, :], PS[2][:, :], ALU.subtract, [YB[c], PSB[2]], [TMPC])
            tt("dve", tmpc[:], tmpc[:], rstd_c[:], ALU.mult, [TMPC, RSC], [TMPC])
            act(CT[:, c, q * 512:(q + 1) * 512], tmpc[:], AF.Silu, [TMPC, CONST], [CTB[q]],
                bias=lb[:, c:c + 1], scale=lg[:, c:c + 1])
    barrier()
    if debug:
        o = dout("d_CT", [128, 4, NOWN], BF16)
        dma("sp", o, CT[:], "dbg", [], [])
        barrier()
    if stop_after == "B":
        P.emit()
        return nc, dbg

    new_scope()
    htstack.close()
    wob_a = sb([64, 8, D_MODEL], BF16, "wob_a")
    wob_c = sb([128, 4, D_MODEL], BF16, "wob_c")
    WOB = Buf("wob")
    xr = [sb([128, D_MODEL], F32, f"xr{i}") for i in range(2)]
    XR = [Buf(f"xr{i}") for i in range(2)]
    for hh in range(8):
        s = hh % 2
        dma("sp", xr[s][0:64, :], w_out[hh * 64:(hh + 1) * 64, :], f"xr{s}", [], [XR[s]])
        vcopy("dve", wob_a[:, hh, :], xr[s][0:64, :], [XR[s]], [WOB])
    for c in range(4):
        s = c % 2
        dma("sp", xr[s][:, :], w_out[512 + c * 128:512 + (c + 1) * 128, :], f"xr{s}", [], [XR[s]])
        vcopy("dve", wob_c[:, c, :], xr[s][:, :], [XR[s]], [WOB])
    pT = [sb([128, 1024], BF16, f"pT{i}") for i in range(3)]
    PT = [Buf(f"pT{i}") for i in range(3)]
    den = sb([64, 512], F32, "den")
    DEN = Buf("den")
    OTn = sb([64, 8, 512], BF16, "OTn")
    OTB = [Buf(f"otn{h}") for h in range(8)]
    x2t = [sb([128, D_MODEL], F32, f"x2t{i}") for i in range(2)]
    X2T = [Buf(f"x2t{i}") for i in range(2)]
    X2D = [Buf(f"x2d{i}") for i in range(NT_OWN)]
    NKP = NT_ALL // 2
    its = [(qt, g, j, kp) for qt in range(8) for g in range(2) for j in range(4) for kp in range(NKP)]
    LOOK = 1

    QTz = [sb([128, 8, 512], BF16, f"QTz{i}") for i in range(2)]
    QTZ = [Buf(f"qtz{i}") for i in range(2)]
    for zz in range(2):
        memset("pool", QTz[zz][:].rearrange("p a b -> p (a b)"), 0.0, [QTZ[zz]])
    qtz_done = set()

    def fill_qtz(qt):
        if qt in qtz_done:
            return
        qtz_done.add(qt)
        for g in range(2):
            for j in range(4):
                vcopy("pool", QTz[qt % 2][g * 64:(g + 1) * 64, g * 4 + j, :],
                      QT[g * 64:(g + 1) * 64, j, qt * 512:(qt + 1) * 512],
                      [QTB[qt * 4 + ii] for ii in range(4)], [QTZ[qt % 2]])

    def issue_qk(i):
        qt, g, j, kp = its[i]
        fill_qtz(qt)
        sp = i % 2
        for u in range(2):
            kt = kp * 2 + u
            mm(PSD[sp][:, u * 512:(u + 1) * 512], KT[:, kt * 128:(kt + 1) * 128],
               QTz[qt % 2][:, g * 4 + j, :], True, True,
               [KTB[kt], QTZ[qt % 2]], [PSB[2 * sp], PSB[2 * sp + 1]])

    for i in range(LOOK):
        issue_qk(i)
    for i, (qt, g, j, kp) in enumerate(its):
        if i + LOOK < len(its):
            issue_qk(i + LOOK)
        h = g * 4 + j
        obank = 4 + (h % 2)
        sp = i % 2
        slot = i % 3
        act(pT[slot][:], PSD[sp][:, :], AF.Exp, [PSB[2 * sp], PSB[2 * sp + 1]], [PT[slot]])
        for u in range(2):
            kt = kp * 2 + u
            mm(PS[obank][:, :], VA[:, kt, g, :], pT[slot][:, u * 512:(u + 1) * 512], kt == 0, kt == NT_ALL - 1,
               [VAB[kt], PT[slot]], [PSB[obank]])
        if kp != NKP - 1:
            continue
        acopy(den[:], PS[obank][64:128, :], [PSB[obank]], [DEN])
        recip(den[:], den[:], [DEN], [DEN])
        tt("dve", OTn[:, h, :], PS[obank][0:64, :], den[:], ALU.mult, [PSB[obank], DEN], [OTB[h]])
        if h != 7:
            continue
        for sub in range(4):
            ti = qt * 4 + sub
            s = ti % 2
            tok = ti * 128
            dma("sp", xr[s][:], x_own[tok:tok + 128, :], f"xr{s}", [], [XR[s]])
            for half in range(2):
                bank = 6 + half
                n0 = half * 512
                for h in range(8):
                    mm(PS[bank][:, :], OTn[:, h, sub * 128:(sub + 1) * 128], wob_a[:, h, n0:n0 + 512],
                       h == 0, False, [OTB[h], WOB], [PSB[bank]])
                for c in range(4):
                    mm(PS[bank][:, :], CT[:, c, tok:tok + 128], wob_c[:, c, n0:n0 + 512],
                       False, c == 3, [CTB[qt], WOB], [PSB[bank]])
                tt("dve", x2t[s][:, n0:n0 + 512], PS[bank][:, :], xr[s][:, n0:n0 + 512], ALU.add,
                   [PSB[bank], XR[s]], [X2T[s]])
            dma("sp", x2_d[tok:tok + 128, :], x2t[s][:], f"x2o{s}", [X2T[s]], [X2D[ti]])
    barrier()
    if debug:
        o = dout("d_x2", [NOWN, D_MODEL], F32)
        for ti in range(NT_OWN):
            s = ti % 2
            dma("sp", xr[s][:], x2_d[ti * 128:(ti + 1) * 128, :], f"xr{s}", [X2D[ti]], [XR[s]])
            dma("sp", o[ti * 128:(ti + 1) * 128, :], xr[s][:], f"x2o{s}", [XR[s]], [])
        barrier()
    if stop_after == "C":
        P.emit()
        return nc, dbg

    new_scope()
    pers.close()
    wqb = sb([128, 8, 2048], BF16, "wqb")
    WQB = Buf("wqb")
    keysT = sb([128, 2, 128], BF16, "keysT")
    g2bc = sb([128, D_MODEL], F32, "g2bc")
    gfbc = sb([128, D_MODEL], F32, "gfbc")
    x2s = [sb([128, D_MODEL], F32, f"x2s{i}") for i in range(2)]
    X2S = [Buf(f"x2s{i}") for i in range(2)]
    dma("sp", g2bc[:], norm2_g.partition_broadcast(128), "c0", [], [CONST])
    dma("sp", gfbc[:], final_g.partition_broadcast(128), "c0", [], [CONST])
    for hf in range(2):
        dma("sp", x2s[hf][:, 0:128], peer_keys[hf, :, :], f"x2s{hf}", [], [X2S[hf]])
        tr(PS[6][:, hf * 128:(hf + 1) * 128], x2s[hf][:, 0:128], ident_f[:], [X2S[hf], CONST], [PSB[6]])
    acopy(keysT[:], PS[6][:, 0:256].rearrange("p (a n) -> p a n", a=2), [PSB[6]], [CONST])
    for dc in range(8):
        for hf in range(2):
            s = (dc * 2 + hf) % 2
            dma("sp", x2s[s][:], peer_wq[dc * 128:(dc + 1) * 128, hf * 1024:(hf + 1) * 1024], f"x2s{s}",
                [], [X2S[s]])
            if hf == 0:
                vcopy("dve", wqb[:, dc, 0:1024], x2s[s][:], [X2S[s]], [WQB])
            else:
                acopy(wqb[:, dc, 1024:2048], x2s[s][:], [X2S[s]], [WQB])

    junk = sb([128, D_MODEL], BF16, "junkD")
    JUNK = Buf("junkD")
    st = sb([128, 8], F32, "stD")
    ST = Buf("stD")
    hn = sb([128, D_MODEL], F32, "hn")
    HN = Buf("hn")
    hnb = sb([128, D_MODEL], BF16, "hnb")
    HNB = Buf("hnb")
    hnT = sb([128, 8, 128], BF16, "hnT")
    HNT = Buf("hnT")
    qTs = sb([128, 16, 128], BF16, "qTs")
    QTS = Buf("qTs")
    S = sb([128, 16, 128], F32, "S")
    SB_ = Buf("S")
    wk = sb([128, 256], F32, "wk")
    tv = sb([128, 16, 16], F32, "tv")
    tiu = sb([128, 16, 16], U32, "tiu")
    tif = sb([128, 16, 16], F32, "tif")
    TK = Buf("topk1")
    cand = sb([128, 8, 16, 16], F32, "cand")
    CAND = Buf("cand")
    tops = sb([128, 8, 16], F32, "tops")
    posu = sb([128, 8, 16], U32, "posu")
    piu = sb([128, 8, 16], U32, "piu")
    pju = sb([128, 8, 16], U32, "pju")
    pif = sb([128, 8, 16], F32, "pif")
    pjf = sb([128, 8, 16], F32, "pjf")
    TK2 = Buf("topk2")
    oh = sb([128, 8, 16, 16], F32, "oh")
    OH = Buf("oh")
    i1s = sb([128, 8, 16], F32, "i1s")
    i2s = sb([128, 8, 16], F32, "i2s")
    ef = sb([128, 128], F32, "ef")
    gw = sb([128, 8, 16], F32, "gw")
    ssum = sb([128, 8], F32, "ssum")
    EG = Buf("eg")
    eTu = sb([128, 128], U32, "eTu")
    ET = Buf("eTu")
    gT = sb([128, 128], F32, "gT")
    GT = Buf("gT")
    hT = sb([128, 128], F32, "hT")
    HTD = Buf("hT")
    gl = sb([128, 128], F32, "gl")
    gl2 = sb([128, 128], F32, "gl2")
    ghT = sb([128, 128], F32, "ghT")
    GH = Buf("ghT")
    GHB = [Buf("ghb0"), Buf("ghb1")]
    NSL = 16
    SUBB = 8
    gsl = [sb([128, 2 * D_MODEL], BF16, f"gs{i}") for i in range(NSL)]
    GS = [Buf(f"gs{i}") for i in range(NSL)]
    ghb = sb([128, 128], BF16, "ghb")
    UVD = Buf("uvd")
    for a in range(128):
        s = a % 2
        stgf = gsl[2 * s][:].bitcast(F32)
        stgv = gsl[2 * s + 1][:].bitcast(F32)
        stgb = gsl[4 + s]
        dma("sp", stgf, peer_u[a * 128:(a + 1) * 128, :], f"pu{s}", [], [GS[2 * s]])
        dma("sp", stgv, peer_v[a * 128:(a + 1) * 128, :], f"pv{s}", [], [GS[2 * s + 1]])
        vcopy("pool", stgb[:, 0:1024], stgf, [GS[2 * s]], [GS[4 + s]])
        acopy(stgb[:, 1024:2048], stgv, [GS[2 * s + 1]], [GS[4 + s]])
        dma("sp", uv_d[a * 128:(a + 1) * 128, :], stgb[:], f"po{s}", [GS[4 + s]], [UVD])
    barrier()
    oT = sb([128, 8, 128], F32, "oT")
    OT = Buf("oT")
    yb = sb([128, D_MODEL], F32, "yb")
    YBD = Buf("yb")
    yo = [sb([128, D_MODEL], F32, f"yo{i}") for i in range(2)]
    YO = [Buf(f"yo{i}") for i in range(2)]
    psO = PSD[2][:, :].rearrange("p (c t) -> p c t", c=8)
    gcnt = 0
    GELU_C = 2.0 * 0.7978845608028654
    n_tiles_d = NT_OWN if ntile_d is None else ntile_d
    for ti in range(n_tiles_d):
        tok = ti * 128
        s = ti % 2
        dma("sp", x2s[s][:], x2_d[tok:tok + 128, :], f"x2s{s}", [X2D[ti]], [X2S[s]])
        act(junk[:], x2s[s][:], AF.Square, [X2S[s]], [JUNK, ST], accum=st[:, 0:1])
        act(st[:, 1:2], st[:, 0:1], AF.Sqrt, [ST], [ST], bias=EPS, scale=1.0 / D_MODEL)
        recip(st[:, 2:3], st[:, 1:2], [ST], [ST])
        stt(hn[:], x2s[s][:], st[:, 2:3], g2bc[:], ALU.mult, ALU.mult, [X2S[s], ST, CONST], [HN])
        acopy(hnb[:], hn[:], [HN], [HNB])
        for dc in range(8):
            tr(psbf(6)[:, dc * 128:(dc + 1) * 128], hnb[:, dc * 128:(dc + 1) * 128], ident_b[:],
               [HNB, CONST], [PSB[6]])
        acopy(hnT[:], psbf(6).rearrange("p (c t) -> p c t", c=8), [PSB[6]], [HNT])
        for jj in range(16):
            b = jj // 4
            for dc in range(8):
                mm(PS[b][:, (jj % 4) * 128:(jj % 4 + 1) * 128], wqb[:, dc, jj * 128:(jj + 1) * 128],
                   hnT[:, dc, :], dc == 0, dc == 7, [WQB, HNT], [PSB[b]])
        for b in range(4):
            acopy(qTs[:, 4 * b:4 * b + 4, :], PS[b].rearrange("p (a t) -> p a t", a=4), [PSB[b]], [QTS])
        for jj in range(16):
            b = jj // 4
            mm(PS[b][:, (jj % 4) * 128:(jj % 4 + 1) * 128], qTs[:, jj, :], keysT[:, jj % 2, :], True, True,
               [QTS, CONST], [PSB[b]])
        for b in range(4):
            acopy(S[:, 4 * b:4 * b + 4, :], PS[b].rearrange("p (a t) -> p a t", a=4), [PSB[b]], [SB_])

        def top16(src_ap, n, tv_ap, ti_ap, R, W):
            P.op("dve", lambda h: h.max(out=tv_ap[:, 0:8], in_=src_ap), R, W)
            P.op("dve", lambda h: h.max_index(out=ti_ap[:, 0:8], in_max=tv_ap[:, 0:8], in_values=src_ap), R, W)
            P.op("dve", lambda h: h.match_replace(out=wk[:, 0:n], in_to_replace=tv_ap[:, 0:8], in_values=src_ap,
                                                  imm_value=-1e30), R, W)
            P.op("dve", lambda h: h.max(out=tv_ap[:, 8:16], in_=wk[:, 0:n]), R, W)
            P.op("dve", lambda h: h.max_index(out=ti_ap[:, 8:16], in_max=tv_ap[:, 8:16], in_values=wk[:, 0:n]), R, W)

        for jj in range(16):
            top16(S[:, jj, :], 128, tv[:, jj, :], tiu[:, jj, :], [SB_], [TK])
        vcopy("dve", tif[:], tiu[:], [TK], [TK])
        tvv = tv[:].rearrange("p (h a) k -> p h a k", a=2)
        tifv = tif[:].rearrange("p (h a) k -> p h a k", a=2)
        tt("dve", cand[:], tvv[:, :, 0, :].unsqueeze(3).to_broadcast([128, 8, 16, 16]),
           tvv[:, :, 1, :].unsqueeze(2).to_broadcast([128, 8, 16, 16]), ALU.add, [TK], [CAND])
        for hh in range(8):
            top16(cand[:, hh, :, :].rearrange("p a b -> p (a b)"), 256, tops[:, hh, :], posu[:, hh, :],
                  [CAND], [TK2])
        ts("dve", piu[:], posu[:], 4, None, ALU.logical_shift_right, None, [TK2], [TK2])
        ts("dve", pju[:], posu[:], 15, None, ALU.bitwise_and, None, [TK2], [TK2])
        vcopy("dve", pif[:], piu[:], [TK2], [TK2])
        vcopy("dve", pjf[:], pju[:], [TK2], [TK2])
        io16 = iota16[:].unsqueeze(1).unsqueeze(1).to_broadcast([128, 8, 16, 16])
        for (pf, col, dst) in ((pif, 0, i1s), (pjf, 1, i2s)):
            tt("dve", oh[:], io16, pf[:].unsqueeze(3).to_broadcast([128, 8, 16, 16]), ALU.is_equal,
               [TK2, CONST], [OH])
            tt("dve", oh[:], oh[:], tifv[:, :, col, :].unsqueeze(2).to_broadcast([128, 8, 16, 16]), ALU.mult,
               [OH, TK], [OH])
            reduce_add(dst[:], oh[:], [OH], [EG])
        stt(ef[:], i1s[:].rearrange("p h k -> p (h k)"), 128.0, i2s[:].rearrange("p h k -> p (h k)"),
            ALU.mult, ALU.add, [EG], [EG])
        tt("dve", gw[:], tops[:], tops[:, :, 0:1].to_broadcast([128, 8, 16]), ALU.subtract, [TK2], [EG])
        act(gw[:], gw[:], AF.Exp, [EG], [EG])
        reduce_add(ssum[:], gw[:], [EG], [EG])
        recip(ssum[:], ssum[:], [EG], [EG])
        tt("dve", gw[:], gw[:], ssum[:].unsqueeze(2).to_broadcast([128, 8, 16]), ALU.mult, [EG], [EG])
        tr(PS[6][:, 0:128], ef[:], ident_f[:], [EG, CONST], [PSB[6]])
        tr(PS[6][:, 128:256], gw[:].rearrange("p h k -> p (h k)"), ident_f[:], [EG, CONST], [PSB[6]])
        vcopy("dve", eTu[:], PS[6][:, 0:128], [PSB[6]], [ET])
        acopy(gT[:], PS[6][:, 128:256], [PSB[6]], [GT])
        if debug and ti == 0:
            o = dout("d_e", [128, 128], U32)
            dma("sp", o, eTu[:], "dbg", [ET], [])
            o = dout("d_g", [128, 128], F32)
            dma("sp", o, gT[:], "dbg", [GT], [])
        nsb = 128 // SUBB

        def stageA(k):
            nonlocal gcnt
            for t in range(k * SUBB, (k + 1) * SUBB):
                sl = (k % 2) * SUBB + (t % SUBB)
                pb = (gcnt % 2) * 2
                gcnt += 1
                P.dma("pool", (lambda o, ia: (lambda h: h.indirect_dma_start(
                    out=o, out_offset=None, in_=uv_d[:, :],
                    in_offset=bass.IndirectOffsetOnAxis(ap=ia, axis=0))))(gsl[sl][:], eTu[:, t:t + 1]),
                    f"g{sl}", [ET, UVD], [GS[sl]])
                lh = ident_b[:, t:t + 1].to_broadcast([128, 128])
                mm(PS[pb][:, :], lh, hnb[:, 0:512], True, True, [HNB, CONST], [PSB[pb], PSB[pb + 1]])
                mm(PS[pb + 1][:, :], lh, hnb[:, 512:1024], True, True, [HNB, CONST], [PSB[pb], PSB[pb + 1]])
                W = [HTD] if (t % SUBB) in (0, SUBB - 1) else []
                P.op("dve", (lambda a, b_, c: (lambda h: h.scalar_tensor_tensor(
                    out=junk[:], in0=a, scalar=1.0, in1=b_, op0=ALU.mult, op1=ALU.mult, accum_out=c)))(
                    gsl[sl][:, 0:1024], PSD[pb // 2][:, :], hT[:, t:t + 1]), [GS[sl], PSB[pb], PSB[pb + 1]], W)

        def stageG(k):
            c0, c1 = k * SUBB, (k + 1) * SUBB
            act(gl[:, c0:c1], hT[:, c0:c1], AF.Square, [HTD], [GH])
            ts("dve", gl[:, c0:c1], gl[:, c0:c1], 0.044715, 1.0, ALU.mult, ALU.add, [GH], [GH])
            tt("dve", gl[:, c0:c1], gl[:, c0:c1], hT[:, c0:c1], ALU.mult, [GH, HTD], [GH])
            act(gl2[:, c0:c1], gl[:, c0:c1], AF.Sigmoid, [GH], [GH], scale=GELU_C)
            tt("dve", gl2[:, c0:c1], gl2[:, c0:c1], hT[:, c0:c1], ALU.mult, [GH, HTD], [GH])
            tt("dve", ghb[:, c0:c1], gl2[:, c0:c1], gT[:, c0:c1], ALU.mult, [GH, GT], [GHB[k % 2]])

        def stageS(k):
            for t in range(k * SUBB, (k + 1) * SUBB):
                sl = (k % 2) * SUBB + (t % SUBB)
                for c in range(8):
                    mm(psO[:, c, t:t + 1], gsl[sl][:, D_MODEL + c * 128:D_MODEL + (c + 1) * 128], ghb[:, t:t + 1],
                       True, True, [GS[sl], GHB[k % 2]], [PSB[4], PSB[5]])

        stageA(0)
        stageG(0)
        for k in range(1, nsb):
            stageA(k)
            stageS(k - 1)
            stageG(k)
        stageS(nsb - 1)
        acopy(oT[:], psO, [PSB[4], PSB[5]], [OT])
        for c in range(8):
            tr(PSD[3][:, c * 128:(c + 1) * 128], oT[:, c, :], ident_f[:], [OT, CONST], [PSB[6], PSB[7]])
        tt("dve", yb[:], PSD[3][:, :], x2s[s][:], ALU.add, [PSB[6], PSB[7], X2S[s]], [YBD])
        act(junk[:], yb[:], AF.Square, [YBD], [JUNK, ST], accum=st[:, 4:5])
        act(st[:, 5:6], st[:, 4:5], AF.Sqrt, [ST], [ST], bias=EPS, scale=1.0 / D_MODEL)
        recip(st[:, 6:7], st[:, 5:6], [ST], [ST])
        stt(yo[s][:], yb[:], st[:, 6:7], gfbc[:], ALU.mult, ALU.mult, [YBD, ST, CONST], [YO[s]])
        dma("sp", out_d[tok:tok + 128, :], yo[s][:], f"yo{s}", [YO[s]], [])
    barrier()
    P.emit()
    return nc, dbg


def make_in_maps(inputs):
    x = np.ascontiguousarray(np.asarray(inputs["x"], dtype=np.float32))
    shared = {}
    for k in ("norm1_g", "w_in", "q_norm_g", "k_norm_g", "conv_b", "conv_ln_g", "conv_ln_b", "w_out",
              "norm2_g", "peer_wq", "peer_keys", "peer_u", "peer_v", "final_g"):
        shared[k] = np.ascontiguousarray(np.asarray(inputs[k], dtype=np.float32))
    shared["conv_dw"] = np.ascontiguousarray(np.asarray(inputs["conv_dw"], dtype=np.float32).reshape(CONV_W, 512))
    maps = []
    for c in range(8):
        b, hf = c // 2, c % 2
        own0 = hf * NOWN
        oth0 = (1 - hf) * NOWN
        m = dict(shared)
        m["x_own"] = x[b, own0:own0 + NOWN]
        m["x_oth"] = x[b, oth0:oth0 + NOWN]
        halo = np.zeros((32, D_MODEL), np.float32)
        if hf == 1:
            halo[0:15] = x[b, own0 - 15:own0]
        else:
            halo[16:31] = x[b, own0 + NOWN:own0 + NOWN + 15]
        m["x_halo"] = halo
        pos = np.concatenate([np.arange(own0, own0 + NOWN), np.arange(oth0, oth0 + NOWN)])
        pos = pos.reshape(NT_ALL, 128).T
        rc = np.stack([pos // 64, pos % 64], axis=-1).astype(np.float32)
        m["rowcol"] = np.ascontiguousarray(rc)
        maps.append(m)
    return maps


_NC_CACHE = {}


def kernel(**inputs):
    if "nc" not in _NC_CACHE:
        _NC_CACHE["nc"] = build_program("D", False)[0]
    nc = _NC_CACHE["nc"]
    maps = make_in_maps(inputs)
    res = run_bass_kernel_spmd(nc, maps, core_ids=list(range(8)))
    out = np.empty((4, SEQ, D_MODEL), np.float32)
    for c in range(8):
        b, hf = c // 2, c % 2
        out[b, hf * NOWN:(hf + 1) * NOWN] = res.results[c]["out"]
    return out
```

```python
import bisect
from contextlib import ExitStack
import numpy as np
import concourse.bass as bass
import concourse.mybir as mybir
from concourse.bass_utils import run_bass_kernel_spmd

F32 = mybir.dt.float32
F32R = mybir.dt.float32r
BF16 = mybir.dt.bfloat16
U32 = mybir.dt.uint32
I32 = mybir.dt.int32
ALU = mybir.AluOpType
AF = mybir.ActivationFunctionType
AX = mybir.AxisListType

SEM_LIMIT = 30000


class _Cut(Exception):
    pass


class Buf:
    def __init__(self, name, excl=False):
        self.name = name
        self.last_w = None
        self.readers = []
        self.excl = excl


class DSem:
    def __init__(self, prog, name):
        self.prog = prog
        self.name = name
        self.sem = prog.nc.alloc_semaphore(name=name)
        self.total = 0
        self.group_ends = []
        self.open = False

    def need(self, v):
        i = bisect.bisect_left(self.group_ends, v)
        if i < len(self.group_ends):
            return self.group_ends[i]
        self.group_ends.append(self.total)
        self.open = False
        return self.total


class Prog:
    ENG = ("pe", "dve", "act", "pool", "sp")

    def __init__(self, nc):
        self.nc = nc
        self.handles = {"pe": nc.tensor, "dve": nc.vector, "act": nc.scalar,
                        "pool": nc.gpsimd, "sp": nc.sync}
        self.lists = {e: [] for e in self.ENG}
        self.cnt = {e: 0 for e in self.ENG}
        self.gen = {e: 0 for e in self.ENG}
        self.sems = {e: [nc.alloc_semaphore(name=f"s_{e}_0")] for e in self.ENG}
        self.waited = {}
        self.same_engine_sync = {"pe": False, "dve": True, "act": True,
                                 "pool": True, "sp": False}
        self.dsems = {}
        self.old_dsems = []
        self.n_inst = 0

    def _wait(self, eng, ev):
        kind, key, val = ev
        if kind == "e":
            e2, g = key
            if e2 == eng and not self.same_engine_sync[eng]:
                return
            sem = self.sems[e2][g]
            wkey = (eng, "e", e2, g)
            need = val
        else:
            ds = key
            need = ds.need(val)
            sem = ds.sem
            wkey = (eng, "d", ds.name)
        if self.waited.get(wkey, 0) >= need:
            return
        self.waited[wkey] = need
        self.lists[eng].append(("wait", sem, need))

    def _deps(self, eng, reads, writes):
        for b in reads:
            if b.last_w is not None:
                self._wait(eng, b.last_w)
            if b.excl:
                for ev in b.readers:
                    if ev[0] == "e" and ev[1][0] != eng:
                        self._wait(eng, ev)
        for b in writes:
            if b.last_w is not None:
                self._wait(eng, b.last_w)
            for ev in b.readers:
                self._wait(eng, ev)

    def _commit(self, ev, reads, writes):
        for b in writes:
            b.last_w = ev
            b.readers = []
        for b in reads:
            if b not in writes:
                b.readers.append(ev)
                if len(b.readers) > 64:
                    b.readers = b.readers[-64:]

    def op(self, eng, fn, reads=(), writes=()):
        reads = list(reads)
        writes = list(writes)
        self._deps(eng, reads, writes)
        if self.cnt[eng] >= SEM_LIMIT:
            self.gen[eng] += 1
            self.cnt[eng] = 0
            self.sems[eng].append(self.nc.alloc_semaphore(name=f"s_{eng}_{self.gen[eng]}"))
        self.cnt[eng] += 1
        g = self.gen[eng]
        self.lists[eng].append(("op", fn, self.sems[eng][g]))
        ev = ("e", (eng, g), self.cnt[eng])
        self._commit(ev, reads, writes)
        self.n_inst += 1
        return ev

    def dsem(self, name):
        if name not in self.dsems:
            self.dsems[name] = DSem(self, "d_" + name)
        return self.dsems[name]

    def dma(self, queue, fn, dsem, reads=(), writes=()):
        ds = self.dsem(dsem) if isinstance(dsem, str) else dsem
        if ds.total >= SEM_LIMIT and not ds.open and isinstance(dsem, str):
            self._wait(queue, ("d", ds, ds.total))
            self._dgen = getattr(self, "_dgen", 0) + 1
            ds = DSem(self, f"d_{dsem}_{self._dgen}")
            self.dsems[dsem] = ds
            self.old_dsems.append(ds)
        reads = list(reads)
        writes = list(writes)
        self._deps(queue, reads, writes)
        if (not ds.open) and ds.total > 0:
            self._wait(queue, ("d", ds, ds.total))
        ds.total += 16
        ds.open = True
        self.lists[queue].append(("dma", fn, ds.sem))
        ev = ("d", ds, ds.total)
        self._commit(ev, reads, writes)
        self.n_inst += 1
        return ev

    def wait_all(self, eng, bufs):
        for b in bufs:
            if b.last_w is not None:
                self._wait(eng, b.last_w)

    def emit(self):
        nc = self.nc
        with nc.Block() as block:
            def mk(ename):
                items = self.lists[ename]

                def body(h):
                    for it in items:
                        if it[0] == "wait":
                            h.wait_ge(it[1], it[2])
                        elif it[0] == "op":
                            it[1](h).then_inc(it[2], 1)
                        else:
                            it[1](h).then_inc(it[2], 16)
                return body
            block.tensor(mk("pe"))
            block.vector(mk("dve"))
            block.scalar(mk("act"))
            block.gpsimd(mk("pool"))
            block.sync(mk("sp"))


D_MODEL = 1024
SEQ = 8192
NOWN = 4096
HD = 64
CONV_W = 31
IN_W = 1792
EPS = 1e-6
NT_OWN = NOWN // 128
NT_ALL = SEQ // 128
TWO_PI = 2.0 * np.pi


def build_program(stop_after="D", debug=False, grp_list=None, pool_eng="pool", cut=None, ntile_d=None):
    nc = bass.Bass("TRN2", target_bir_lowering=False)
    P = Prog(nc)
    dbg = {}

    def din(name, shape, dt=F32):
        return nc.dram_tensor(name, list(shape), dt, kind="ExternalInput").ap()

    x_own = din("x_own", [NOWN, D_MODEL])
    x_oth = din("x_oth", [NOWN, D_MODEL])
    x_halo = din("x_halo", [32, D_MODEL])
    rowcol = din("rowcol", [128, NT_ALL, 2])
    norm1_g = din("norm1_g", [D_MODEL])
    w_in = din("w_in", [D_MODEL, IN_W])
    q_norm_g = din("q_norm_g", [HD])
    k_norm_g = din("k_norm_g", [HD])
    conv_dw = din("conv_dw", [CONV_W, 512])
    conv_b = din("conv_b", [512])
    conv_ln_g = din("conv_ln_g", [512])
    conv_ln_b = din("conv_ln_b", [512])
    w_out = din("w_out", [D_MODEL, D_MODEL])
    norm2_g = din("norm2_g", [D_MODEL])
    peer_wq = din("peer_wq", [D_MODEL, 2048])
    peer_keys = din("peer_keys", [2, 128, 128])
    peer_u = din("peer_u", [16384, D_MODEL])
    peer_v = din("peer_v", [16384, D_MODEL])
    final_g = din("final_g", [D_MODEL])
    out_d = nc.dram_tensor("out", [NOWN, D_MODEL], F32, kind="ExternalOutput").ap()
    x2_d = nc.dram_tensor("x2_scratch", [NOWN, D_MODEL], F32, kind="Internal").ap()
    uv_d = nc.dram_tensor("uv_scratch", [16384, 2 * D_MODEL], BF16, kind="Internal").ap()

    def dout(name, shape, dt=F32):
        dbg[name] = nc.dram_tensor(name, list(shape), dt, kind="ExternalOutput").ap()
        return dbg[name]

    _n = [0]
    cst = ExitStack()
    pers = ExitStack()
    scope = [ExitStack()]

    def sb(shape, dt=F32, name=None, persistent=False):
        _n[0] += 1
        nm = name or f"sb{_n[0]}"
        if persistent == "c":
            return cst.enter_context(nc.sbuf_tensor(nm, list(shape), dt, side="right"))
        if persistent:
            return pers.enter_context(nc.sbuf_tensor(nm, list(shape), dt, side="right"))
        return scope[0].enter_context(nc.sbuf_tensor(nm, list(shape), dt, side="left"))

    def new_scope():
        barrier()
        scope[0].close()
        scope[0] = ExitStack()

    def mm(out, lhsT, rhs, start, stop, R, W):
        P.op("pe", lambda h: h.matmul(out, lhsT=lhsT, rhs=rhs, start=start, stop=stop), R, W)

    def tr(out, in_, ident, R, W):
        P.op("pe", lambda h: h.transpose(out=out, in_=in_, identity=ident), R, W)

    def act(out, in_, func, R, W, bias=None, scale=None, accum=None):
        kw = {}
        if bias is not None:
            kw["bias"] = bias
        if scale is not None:
            kw["scale"] = scale
        if accum is not None:
            kw["accum_out"] = accum
        P.op("act", lambda h: h.activation(out=out, in_=in_, func=func, **kw), R, W)

    def acopy(out, in_, R, W):
        P.op("act", lambda h: h.copy(out=out, in_=in_), R, W)

    def tt(eng, out, in0, in1, op, R, W):
        P.op(eng, lambda h: h.tensor_tensor(out=out, in0=in0, in1=in1, op=op), R, W)

    def ts(eng, out, in0, s1, s2, op0, op1, R, W):
        if op1 is None:
            P.op(eng, lambda h: h.tensor_scalar(out=out, in0=in0, scalar1=s1, scalar2=None, op0=op0), R, W)
        else:
            P.op(eng, lambda h: h.tensor_scalar(out=out, in0=in0, scalar1=s1, scalar2=s2, op0=op0, op1=op1), R, W)

    def stt(out, in0, scalar, in1, op0, op1, R, W):
        P.op("dve", lambda h: h.scalar_tensor_tensor(out=out, in0=in0, scalar=scalar, in1=in1, op0=op0, op1=op1), R, W)

    def vcopy(eng, out, in_, R, W):
        P.op(eng, lambda h: h.tensor_copy(out=out, in_=in_), R, W)

    def recip(out, in_, R, W):
        P.op("dve", lambda h: h.reciprocal(out=out, in_=in_), R, W)

    def reduce_add(out, in_, R, W):
        P.op("dve", lambda h: h.tensor_reduce(out=out, in_=in_, axis=AX.X, op=ALU.add), R, W)

    def memset(eng, ap, val, W):
        P.op(eng, lambda h: h.memset(ap, val), [], W)

    def dma(q, out, in_, ds, R, W, slow=False):
        if slow:
            P.dma(q, lambda h: h.dma_start(out=out, in_=in_, allow_slow_non_contiguous=True), ds, R, W)
        else:
            P.dma(q, lambda h: h.dma_start(out=out, in_=in_), ds, R, W)

    def barrier():
        evs = []
        for e in P.ENG:
            if P.cnt[e] > 0:
                evs.append(("e", (e, P.gen[e]), P.cnt[e]))
        for ds in list(P.dsems.values()):
            if ds.total > 0:
                evs.append(("d", ds, ds.total))
        for e in P.ENG:
            for ev in evs:
                if ev[0] == "e" and ev[1][0] == e:
                    continue
                P._wait(e, ev)

    PSD = [nc.alloc_psum_tensor(f"pd{i}", [128, 1024], F32) for i in range(4)]
    PS = [PSD[i // 2][:, (i % 2) * 512:(i % 2 + 1) * 512] for i in range(8)]
    PSB = [Buf(f"bank{i}", excl=True) for i in range(8)]

    def psbf(i):
        return PS[i].bitcast(BF16)

    CONST = Buf("const")
    ident_f = sb([128, 128], F32, "ident_f", persistent="c")
    ident_b = sb([128, 128], BF16, "ident_b", persistent="c")
    iot = sb([128, 128], F32, "iot", persistent="c")
    P.op("pool", lambda h: h.iota(iot[:], pattern=[[1, 128]], base=0, channel_multiplier=-1,
                                  allow_small_or_imprecise_dtypes=True), [], [CONST])
    ts("dve", ident_f[:], iot[:], 0.0, None, ALU.is_equal, None, [CONST], [CONST])
    vcopy("dve", ident_b[:], ident_f[:], [CONST], [CONST])
    ones_f = sb([128, 128], F32, "ones_f", persistent="c")
    memset("dve", ones_f[:], 1.0 / 512.0, [CONST])
    iota16 = sb([128, 16], F32, "iota16", persistent="c")
    epsc = sb([128, 1], F32, "epsc", persistent="c")
    memset("dve", epsc[:], EPS, [CONST])
    P.op("pool", lambda h: h.iota(iota16[:], pattern=[[1, 16]], base=0, channel_multiplier=0,
                                  allow_small_or_imprecise_dtypes=True), [], [CONST])

    g1 = sb([128, 8], F32, "g1", persistent="c")
    dma("sp", g1[:], norm1_g.rearrange("(c p) -> p c", p=128), "c0", [], [CONST], slow=True)
    cb = sb([128, 4], F32, "cb", persistent="c")
    lg = sb([128, 4], F32, "lg", persistent="c")
    lb = sb([128, 4], F32, "lb", persistent="c")
    dma("sp", cb[:], conv_b.rearrange("(c p) -> p c", p=128), "c0", [], [CONST], slow=True)
    dma("sp", lg[:], conv_ln_g.rearrange("(c p) -> p c", p=128), "c0", [], [CONST], slow=True)
    dma("sp", lb[:], conv_ln_b.rearrange("(c p) -> p c", p=128), "c0", [], [CONST], slow=True)
    G10 = sb([128, 10, 64], F32, "G10", persistent="c")
    dma("sp", G10[:, 0, :], q_norm_g.partition_broadcast(128), "c0", [], [CONST])
    dma("sp", G10[:, 8, :], k_norm_g.partition_broadcast(128), "c0", [], [CONST])
    ts("dve", G10[:, 0, :], G10[:, 0, :], HD ** -0.5, None, ALU.mult, None, [CONST], [CONST])
    for j in range(1, 8):
        vcopy("dve", G10[:, j, :], G10[:, 0, :], [CONST], [CONST])
    vcopy("dve", G10[:, 9, :], G10[:, 8, :], [CONST], [CONST])
    KT = sb([128, SEQ], BF16, "KT", persistent=True)
    VA = sb([128, NT_ALL, 2, 128], BF16, "VA", persistent=True)
    QT = sb([128, 4, NOWN], BF16, "QT", persistent=True)
    htstack = ExitStack()
    HT = htstack.enter_context(nc.sbuf_tensor("HT", [128, 4, NOWN + 32], BF16, side="left"))

    rc_t = sb([128, NT_ALL, 2], F32, "rc_t")
    dma("sp", rc_t[:], rowcol, "c0", [], [CONST])
    invf = sb([128, 16], F32, "invf")
    act(invf[:], iota16[:], AF.Exp, [CONST], [CONST], scale=-float(np.log(10000.0)) / 16.0)
    NTAB = NT_ALL * 32
    cos_t = sb([128, NT_ALL, 2, 16], F32, "cos_t")
    sin_t = sb([128, NT_ALL, 2, 16], F32, "sin_t")
    w1b = sb([128, 8, IN_W], BF16, "w1b")
    W1B = Buf("w1b")
    with nc.sbuf_tensor("rr_k", [128, NTAB], I32, side="left") as rr_k, \
            nc.sbuf_tensor("rr_f", [128, NTAB], F32, side="left") as rr_f, \
            nc.sbuf_tensor("rr_a", [128, NTAB], F32, side="left") as rr_a, \
            nc.sbuf_tensor("ang", [128, NT_ALL, 2, 16], F32, side="left") as ang:
        tt("dve", ang[:], rc_t[:].unsqueeze(3).to_broadcast([128, NT_ALL, 2, 16]),
           invf[:].unsqueeze(1).unsqueeze(1).to_broadcast([128, NT_ALL, 2, 16]), ALU.mult, [CONST], [CONST])
        angf = ang[:].rearrange("p a b c -> p (a b c)")
        TMPB = Buf("ropetmp")
        for tab, shift in ((sin_t, 0.0), (cos_t, np.pi / 2)):
            tf = tab[:].rearrange("p a b c -> p (a b c)")
            ts("dve", rr_a[:], angf, float(shift), None, ALU.add, None, [CONST], [TMPB])
            ts("dve", rr_k[:], rr_a[:], 1.0 / TWO_PI, 0.5, ALU.mult, ALU.add, [TMPB], [TMPB])
            vcopy("dve", rr_f[:], rr_k[:], [TMPB], [TMPB])
            stt(rr_f[:], rr_f[:], -TWO_PI, rr_a[:], ALU.mult, ALU.add, [TMPB], [TMPB])
            ts("dve", rr_a[:], rr_f[:], -float(np.pi), TWO_PI, ALU.is_lt, ALU.mult, [TMPB], [TMPB])
            tt("dve", rr_f[:], rr_f[:], rr_a[:], ALU.add, [TMPB], [TMPB])
            ts("dve", rr_a[:], rr_f[:], float(np.pi), TWO_PI, ALU.is_gt, ALU.mult, [TMPB], [TMPB])
            tt("dve", rr_a[:], rr_f[:], rr_a[:], ALU.subtract, [TMPB], [TMPB])
            ts("dve", rr_a[:], rr_a[:], float(np.pi), -float(np.pi), ALU.min, ALU.max, [TMPB], [TMPB])
            act(tf, rr_a[:], AF.Sin, [TMPB], [CONST])
        barrier()
    with nc.sbuf_tensor("stg0", [128, IN_W], F32, side="left") as stg0, \
            nc.sbuf_tensor("stg1", [128, IN_W], F32, side="left") as stg1:
        stg = [stg0, stg1]
        STG = [Buf("stg0"), Buf("stg1")]
        for dc in range(8):
            s = dc % 2
            dma("sp", stg[s][:], w_in[dc * 128:(dc + 1) * 128, :], f"stg{s}", [], [STG[s]])
            ts("dve", w1b[:, dc, 0:512].rearrange("p (j g d) -> p j g d", j=4, g=2),
               stg[s][:, 0:512].rearrange("p (g j d) -> p j g d", g=2, j=4),
               g1[:, dc:dc + 1], None, ALU.mult, None, [STG[s], CONST], [W1B])
            ts("pool", w1b[:, dc, 512:IN_W], stg[s][:, 512:IN_W], g1[:, dc:dc + 1], None, ALU.mult, None,
               [STG[s], CONST], [W1B])
        barrier()

    if stop_after == "0":
        o = dout("d_cos", [128, NT_ALL, 2, 16], F32)
        dma("sp", o, cos_t[:], "dbg", [], [])
        o = dout("d_sin", [128, NT_ALL, 2, 16], F32)
        dma("sp", o, sin_t[:], "dbg", [], [])
        o = dout("d_w1b", [128, 8, IN_W], BF16)
        dma("sp", o, w1b[:], "dbg", [], [])
        o = dout("d_G10", [128, 10, 64], F32)
        dma("sp", o, G10[:], "dbg", [], [])
        barrier()
        P.emit()
        return nc, dbg
    KTB = [Buf(f"kt{i}") for i in range(NT_ALL)]
    VAB = [Buf(f"va{i}") for i in range(NT_ALL)]
    QTB = [Buf(f"qt{i}") for i in range(NT_OWN)]
    HTB = [Buf(f"ht{i}") for i in range(NT_OWN // 4 + 1)]
    VINIT = Buf("vinit")
    memset("pool", VA[:, :, :, 64:128].rearrange("p a b c -> p (a b) c"), 1.0, [VINIT])
    for b in VAB:
        b.last_w = VINIT.last_w
    memset("pool", HT[:, :, NOWN + 30:NOWN + 32], 0.0, [HTB[NT_OWN // 4]])

    xin = [sb([128, D_MODEL], F32, f"xin{i}") for i in range(2)]
    XIN = [Buf(f"xin{i}") for i in range(2)]
    junk = sb([128, D_MODEL], BF16, "junk")
    JUNK = Buf("junk")
    xs = [sb([128, D_MODEL], BF16, f"xs{i}") for i in range(2)]
    XS = [Buf(f"xs{i}") for i in range(2)]
    xT = [sb([128, 8, 512], BF16, f"xT{i}") for i in range(2)]
    XT = [Buf(f"xT{i}") for i in range(2)]
    st = sb([128, 8], F32, "st")
    ST = Buf("st")
    sq = sb([128, 640], F32, "sq")
    SQ = Buf("sq")
    ssq = sb([128, 10], F32, "ssq")
    qn = sb([128, 10, 64], F32, "qn")
    QN = Buf("qn")
    t1 = sb([128, 10, 64], F32, "t1")
    t2 = sb([128, 10, 64], F32, "t2")
    T12 = Buf("t12")
    qr = sb([128, 640], BF16, "qr")
    QR = Buf("qr")
    sg = sb([128, 512], F32, "sg")
    SG = Buf("sg")

    def rms_rows(xt_ap, XB, np_, out_bf, OB):
        act(junk[0:np_, :], xt_ap, AF.Square, [XB], [JUNK, ST], accum=st[0:np_, 0:1])
        act(st[0:np_, 1:2], st[0:np_, 0:1], AF.Sqrt, [ST], [ST], bias=EPS, scale=1.0 / D_MODEL)
        recip(st[0:np_, 2:3], st[0:np_, 1:2], [ST], [ST])
        ts("dve", out_bf, xt_ap, st[0:np_, 2:3], None, ALU.mult, None, [XB, ST], [OB])

    try:
        n_groups = NT_ALL // 4
        tile_ctr = 0
        for grp in (grp_list if grp_list is not None else range(n_groups + 1)):
            halo = grp == n_groups
            own = grp < NT_OWN // 4
            gs = grp % 2
            nsub = 1 if halo else 4
            for sub in range(nsub):
                ti = grp * 4 + sub
                s = tile_ctr % 2
                tile_ctr += 1
                np_ = 32 if halo else 128
                if halo:
                    src = x_halo[:, :]
                elif own:
                    src = x_own[ti * 128:(ti + 1) * 128, :]
                else:
                    src = x_oth[(ti - NT_OWN) * 128:(ti - NT_OWN + 1) * 128, :]
                dma("sp", xin[s][0:np_, :], src, f"xin{s}", [], [XIN[s]])
                rms_rows(xin[s][0:np_, :], XIN[s], np_, xs[s][0:np_, :], XS[s])
                if cut == 1:
                    raise _Cut()
                pb = ti % 2
                for dc in range(8):
                    tr(psbf(pb)[:, dc * 128:dc * 128 + np_], xs[s][0:np_, dc * 128:(dc + 1) * 128],
                       ident_b[0:np_, 0:np_], [XS[s], CONST], [PSB[pb]])
                acopy(xT[gs][:, :, sub * 128:sub * 128 + np_],
                      psbf(pb).rearrange("p (c t) -> p c t", c=8)[:, :, 0:np_], [PSB[pb]], [XT[gs]])
                if cut == 2:
                    raise _Cut()
                if halo:
                    continue
                c0 = 0 if own else 512
                ncol = 768 - c0
                for (a, b) in (((0, 512), (512, 768)) if own else ((512, 768),)):
                    bank = 2 if a == 0 else 3
                    for dc in range(8):
                        mm(PS[bank][:, 0:b - a], xT[gs][:, dc, sub * 128:(sub + 1) * 128], w1b[:, dc, a:b],
                           dc == 0, dc == 7, [XT[gs], W1B], [PSB[bank]])
                if cut == 3:
                    raise _Cut()
                h0 = 0 if own else 8
                nh = 10 - h0
                if own:
                    act(sq[:, 0:512], PS[2][:, 0:512], AF.Square, [PSB[2]], [SQ])
                act(sq[:, 512:640], PS[3][:, 0:128], AF.Square, [PSB[3]], [SQ])
                reduce_add(ssq[:, h0:10], sq[:, h0 * 64:640].rearrange("p (h d) -> p h d", d=64), [SQ], [ST])
                act(ssq[:, h0:10], ssq[:, h0:10], AF.Sqrt, [ST], [ST], bias=EPS, scale=1.0 / HD)
                recip(ssq[:, h0:10], ssq[:, h0:10], [ST], [ST])
                if own:
                    tt("dve", qn[:, 0:8, :], PS[2][:, 0:512].rearrange("p (h d) -> p h d", d=64),
                       ssq[:, 0:8].unsqueeze(2).to_broadcast([128, 8, 64]), ALU.mult, [PSB[2], ST], [QN])
                tt("dve", qn[:, 8:10, :], PS[3][:, 0:128].rearrange("p (h d) -> p h d", d=64),
                   ssq[:, 8:10].unsqueeze(2).to_broadcast([128, 2, 64]), ALU.mult, [PSB[3], ST], [QN])
                if cut == 4:
                    raise _Cut()
                acopy(VA[:, ti, :, 0:64], PS[3][:, 128:256].rearrange("p (g d) -> p g d", g=2), [PSB[3]], [VAB[ti]])
                tt(pool_eng, qn[:, h0:10, :], qn[:, h0:10, :], G10[:, h0:10, :], ALU.mult, [QN, CONST], [QN])
                if cut == 5:
                    raise _Cut()
                for rc in range(2):
                    qv = qn[:, h0:10, rc * 32:(rc + 1) * 32].rearrange("p h (a d) -> p h a d", a=2)
                    cosb = cos_t[:, ti, rc, :].unsqueeze(1).unsqueeze(1).to_broadcast([128, nh, 2, 16])
                    sinb = sin_t[:, ti, rc, :].unsqueeze(1).to_broadcast([128, nh, 16])
                    t1v = t1[:, h0:10, rc * 32:(rc + 1) * 32].rearrange("p h (a d) -> p h a d", a=2)
                    t2v = t2[:, h0:10, rc * 32:(rc + 1) * 32].rearrange("p h (a d) -> p h a d", a=2)
                    tt("dve", t1v, qv, cosb, ALU.mult, [QN, CONST], [T12])
                    tt(pool_eng, t2v[:, :, 0, :], qv[:, :, 1, :], sinb, ALU.mult, [QN, CONST], [T12])
                    tt(pool_eng, t2v[:, :, 1, :], qv[:, :, 0, :], sinb, ALU.mult, [QN, CONST], [T12])
                    qrv = qr[:, h0 * 64:640].rearrange("p (h c) -> p h c", c=64)[:, :, rc * 32:(rc + 1) * 32] \
                        .rearrange("p h (a d) -> p h a d", a=2)
                    tt("dve", qrv[:, :, 0, :], t1v[:, :, 0, :], t2v[:, :, 0, :], ALU.subtract, [T12], [QR])
                    tt("dve", qrv[:, :, 1, :], t1v[:, :, 1, :], t2v[:, :, 1, :], ALU.add, [T12], [QR])
                    if cut == 6:
                        raise _Cut()
                if own:
                    for j in range(4):
                        tr(psbf(4)[:, j * 128:(j + 1) * 128], qr[:, j * 128:(j + 1) * 128], ident_b[:],
                           [QR, CONST], [PSB[4]])
                tr(psbf(4)[:, 512:640], qr[:, 512:640], ident_b[:], [QR, CONST], [PSB[4]])
                if cut == 71:
                    raise _Cut()
                if own:
                    acopy(QT[:, :, ti * 128:(ti + 1) * 128], psbf(4)[:, 0:512].rearrange("p (j t) -> p j t", j=4),
                          [PSB[4]], [QTB[ti]])
                if cut == 72:
                    raise _Cut()
                acopy(KT[:, ti * 128:(ti + 1) * 128], psbf(4)[:, 512:640], [PSB[4]], [KTB[ti]])
                if cut == 7:
                    raise _Cut()
            if own or halo:
                ntok = 32 if halo else 512
                for c in range(4):
                    for part, bank in ((0, 5), (1, 6)):
                        col = 768 + part * 512 + c * 128
                        for dc in range(8):
                            mm(PS[bank][:, 0:ntok], w1b[:, dc, col:col + 128], xT[gs][:, dc, 0:ntok],
                               dc == 0, dc == 7, [XT[gs], W1B], [PSB[bank]])
                    act(sg[:, 0:ntok], PS[6][:, 0:ntok], AF.Sigmoid, [PSB[6]], [SG])
                    if halo:
                        tt("dve", HT[:, c, 0:15], PS[5][:, 0:15], sg[:, 0:15], ALU.mult, [PSB[5], SG], [HTB[0]])
                        tt("dve", HT[:, c, NOWN + 15:NOWN + 30], PS[5][:, 16:31], sg[:, 16:31], ALU.mult,
                           [PSB[5], SG], [HTB[NT_OWN // 4]])
                    else:
                        tt("dve", HT[:, c, 15 + grp * 512:15 + (grp + 1) * 512], PS[5][:, :], sg[:, :], ALU.mult,
                           [PSB[5], SG], [HTB[grp]])
    except _Cut:
        pass
    barrier()

    if debug:
        o = dout("d_KT", [128, SEQ], BF16)
        dma("sp", o, KT[:], "dbg", [], [])
        o = dout("d_QT", [128, 4, NOWN], BF16)
        dma("sp", o, QT[:], "dbg", [], [])
        o = dout("d_VA", [128, NT_ALL, 2, 128], BF16)
        dma("sp", o, VA[:], "dbg", [], [])
        o = dout("d_HT", [128, 4, NOWN + 32], BF16)
        dma("sp", o, HT[:], "dbg", [], [])
        barrier()
    if stop_after == "A":
        P.emit()
        return nc, dbg

    new_scope()
    CT = sb([128, 4, NOWN], BF16, "CT", persistent=True)
    CTB = [Buf(f"ct{i}") for i in range(8)]
    dg = sb([128, 4, CONV_W, 128], BF16, "dg")
    DG = Buf("dg")
    cw = sb([128, 4, CONV_W], F32, "cw")
    cwr = sb([CONV_W, 512], F32, "cwr")
    CW = Buf("cw")
    dma("sp", cwr[:], conv_dw, "cw", [], [CW])
    for c in range(4):
        tr(PS[0][:, c * 32:c * 32 + CONV_W], cwr[:, c * 128:(c + 1) * 128], ident_f[0:CONV_W, 0:CONV_W],
           [CW, CONST], [PSB[0]])
    vcopy("dve", cw[:], PS[0][:, 0:128].rearrange("p (c j) -> p c j", c=4)[:, :, 0:CONV_W], [PSB[0]], [CW])
    for c in range(4):
        for j in range(CONV_W):
            eng = "dve" if (j % 2 == 0) else "pool"
            ts(eng, dg[:, c, j, :], ident_b[:], cw[:, c, j:j + 1], None, ALU.mult, None, [CW, CONST], [DG])
    ybuf = sb([128, 4, 512], F32, "ybuf")
    ysq = sb([128, 4, 512], F32, "ysq")
    YB = [Buf(f"yb{c}") for c in range(4)]
    YS = [Buf(f"ys{c}") for c in range(4)]
    m2 = sb([128, 512], F32, "m2")
    M2 = Buf("m2")
    rstd_c = sb([128, 512], F32, "rstd_c")
    RSC = Buf("rstd_c")
    tmpc = sb([128, 512], F32, "tmpc")
    TMPC = Buf("tmpc")
    for q in range(8):
        hreads = [HTB[q]] + ([HTB[q + 1]] if q + 1 <= 8 else []) + ([HTB[q - 1]] if q > 0 else [])
        for c in range(4):
            bank = c % 2
            for j in range(CONV_W):
                mm(PS[bank][:, :], dg[:, c, j, :], HT[:, c, q * 512 + j:q * 512 + j + 512],
                   j == 0, j == CONV_W - 1, hreads + [DG], [PSB[bank]])
            act(ybuf[:, c, :], PS[bank][:, :], AF.Identity, [PSB[bank], CONST], [YB[c]], bias=cb[:, c:c + 1])
            act(ysq[:, c, :], ybuf[:, c, :], AF.Square, [YB[c]], [YS[c]])
        for c in range(4):
            mm(PS[2][:, :], ones_f[:], ybuf[:, c, :], c == 0, c == 3, [YB[c], CONST], [PSB[2]])
        for c in range(4):
            mm(PS[3][:, :], ones_f[:], ysq[:, c, :], c == 0, c == 3, [YS[c], CONST], [PSB[3]])
        act(m2[:], PS[2][:, :], AF.Square, [PSB[2]], [M2])
        tt("dve", m2[:], PS[3][:, :], m2[:], ALU.subtract, [PSB[3], M2], [M2])
        act(m2[:], m2[:], AF.Sqrt, [M2], [M2], bias=EPS, scale=1.0)
        recip(rstd_c[:], m2[:], [M2], [RSC])
        for c in range(4):
            tt("dve", tmpc[:], ybuf[:, c, :], PS[2][:, :], ALU.subtract, [YB[c], PSB[2]], [TMPC])
            tt("dve", tmpc[:], tmpc[:], rstd_c[:], ALU.mult, [TMPC, RSC], [TMPC])
            act(CT[:, c, q * 512:(q + 1) * 512], tmpc[:], AF.Silu, [TMPC, CONST], [CTB[q]],
                bias=lb[:, c:c + 1], scale=lg[:, c:c + 1])
    barrier()
    if debug:
        o = dout("d_CT", [128, 4, NOWN], BF16)
        dma("sp", o, CT[:], "dbg", [], [])
        barrier()
    if stop_after == "B":
        P.emit()
        return nc, dbg

    new_scope()
    htstack.close()
    wob_a = sb([64, 8, D_MODEL], BF16, "wob_a")
    wob_c = sb([128, 4, D_MODEL], BF16, "wob_c")
    WOB = Buf("wob")
    xr = [sb([128, D_MODEL], F32, f"xr{i}") for i in range(2)]
    XR = [Buf(f"xr{i}") for i in range(2)]
    for hh in range(8):
        s = hh % 2
        dma("sp", xr[s][0:64, :], w_out[hh * 64:(hh + 1) * 64, :], f"xr{s}", [], [XR[s]])
        vcopy("dve", wob_a[:, hh, :], xr[s][0:64, :], [XR[s]], [WOB])
    for c in range(4):
        s = c % 2
        dma("sp", xr[s][:, :], w_out[512 + c * 128:512 + (c + 1) * 128, :], f"xr{s}", [], [XR[s]])
        vcopy("dve", wob_c[:, c, :], xr[s][:, :], [XR[s]], [WOB])
    pT = [sb([128, 1024], BF16, f"pT{i}") for i in range(3)]
    PT = [Buf(f"pT{i}") for i in range(3)]
    den = sb([64, 512], F32, "den")
    DEN = Buf("den")
    OTn = sb([64, 8, 512], BF16, "OTn")
    OTB = [Buf(f"otn{h}") for h in range(8)]
    x2t = [sb([128, D_MODEL], F32, f"x2t{i}") for i in range(2)]
    X2T = [Buf(f"x2t{i}") for i in range(2)]
    X2D = [Buf(f"x2d{i}") for i in range(NT_OWN)]
    NKP = NT_ALL // 2
    its = [(qt, g, j, kp) for qt in range(8) for g in range(2) for j in range(4) for kp in range(NKP)]
    LOOK = 1

    QTz = [sb([128, 8, 512], BF16, f"QTz{i}") for i in range(2)]
    QTZ = [Buf(f"qtz{i}") for i in range(2)]
    for zz in range(2):
        memset("pool", QTz[zz][:].rearrange("p a b -> p (a b)"), 0.0, [QTZ[zz]])
    qtz_done = set()

    def fill_qtz(qt):
        if qt in qtz_done:
            return
        qtz_done.add(qt)
        for g in range(2):
            for j in range(4):
                vcopy("pool", QTz[qt % 2][g * 64:(g + 1) * 64, g * 4 + j, :],
                      QT[g * 64:(g + 1) * 64, j, qt * 512:(qt + 1) * 512],
                      [QTB[qt * 4 + ii] for ii in range(4)], [QTZ[qt % 2]])

    def issue_qk(i):
        qt, g, j, kp = its[i]
        fill_qtz(qt)
        sp = i % 2
        for u in range(2):
            kt = kp * 2 + u
            mm(PSD[sp][:, u * 512:(u + 1) * 512], KT[:, kt * 128:(kt + 1) * 128],
               QTz[qt % 2][:, g * 4 + j, :], True, True,
               [KTB[kt], QTZ[qt % 2]], [PSB[2 * sp], PSB[2 * sp + 1]])

    for i in range(LOOK):
        issue_qk(i)
    for i, (qt, g, j, kp) in enumerate(its):
        if i + LOOK < len(its):
            issue_qk(i + LOOK)
        h = g * 4 + j
        obank = 4 + (h % 2)
        sp = i % 2
        slot = i % 3
        act(pT[slot][:], PSD[sp][:, :], AF.Exp, [PSB[2 * sp], PSB[2 * sp + 1]], [PT[slot]])
        for u in range(2):
            kt = kp * 2 + u
            mm(PS[obank][:, :], VA[:, kt, g, :], pT[slot][:, u * 512:(u + 1) * 512], kt == 0, kt == NT_ALL - 1,
               [VAB[kt], PT[slot]], [PSB[obank]])
        if kp != NKP - 1:
            continue
        acopy(den[:], PS[obank][64:128, :], [PSB[obank]], [DEN])
        recip(den[:], den[:], [DEN], [DEN])
        tt("dve", OTn[:, h, :], PS[obank][0:64, :], den[:], ALU.mult, [PSB[obank], DEN], [OTB[h]])
        if h != 7:
            continue
        for sub in range(4):
            ti = qt * 4 + sub
            s = ti % 2
            tok = ti * 128
            dma("sp", xr[s][:], x_own[tok:tok + 128, :], f"xr{s}", [], [XR[s]])
            for half in range(2):
                bank = 6 + half
                n0 = half * 512
                for h in range(8):
                    mm(PS[bank][:, :], OTn[:, h, sub * 128:(sub + 1) * 128], wob_a[:, h, n0:n0 + 512],
                       h == 0, False, [OTB[h], WOB], [PSB[bank]])
                for c in range(4):
                    mm(PS[bank][:, :], CT[:, c, tok:tok + 128], wob_c[:, c, n0:n0 + 512],
                       False, c == 3, [CTB[qt], WOB], [PSB[bank]])
                tt("dve", x2t[s][:, n0:n0 + 512], PS[bank][:, :], xr[s][:, n0:n0 + 512], ALU.add,
                   [PSB[bank], XR[s]], [X2T[s]])
            dma("sp", x2_d[tok:tok + 128, :], x2t[s][:], f"x2o{s}", [X2T[s]], [X2D[ti]])
    barrier()
    if debug:
        o = dout("d_x2", [NOWN, D_MODEL], F32)
        for ti in range(NT_OWN):
            s = ti % 2
            dma("sp", xr[s][:], x2_d[ti * 128:(ti + 1) * 128, :], f"xr{s}", [X2D[ti]], [XR[s]])
            dma("sp", o[ti * 128:(ti + 1) * 128, :], xr[s][:], f"x2o{s}", [XR[s]], [])
        barrier()
    if stop_after == "C":
        P.emit()
        return nc, dbg

    new_scope()
    pers.close()
    wqb = sb([128, 8, 2048], BF16, "wqb")
    WQB = Buf("wqb")
    keysT = sb([128, 2, 128], BF16, "keysT")
    g2bc = sb([128, D_MODEL], F32, "g2bc")
    gfbc = sb([128, D_MODEL], F32, "gfbc")
    x2s = [sb([128, D_MODEL], F32, f"x2s{i}") for i in range(2)]
    X2S = [Buf(f"x2s{i}") for i in range(2)]
    dma("sp", g2bc[:], norm2_g.partition_broadcast(128), "c0", [], [CONST])
    dma("sp", gfbc[:], final_g.partition_broadcast(128), "c0", [], [CONST])
    for hf in range(2):
        dma("sp", x2s[hf][:, 0:128], peer_keys[hf, :, :], f"x2s{hf}", [], [X2S[hf]])
        tr(PS[6][:, hf * 128:(hf + 1) * 128], x2s[hf][:, 0:128], ident_f[:], [X2S[hf], CONST], [PSB[6]])
    acopy(keysT[:], PS[6][:, 0:256].rearrange("p (a n) -> p a n", a=2), [PSB[6]], [CONST])
    for dc in range(8):
        for hf in range(2):
            s = (dc * 2 + hf) % 2
            dma("sp", x2s[s][:], peer_wq[dc * 128:(dc + 1) * 128, hf * 1024:(hf + 1) * 1024], f"x2s{s}",
                [], [X2S[s]])
            if hf == 0:
                vcopy("dve", wqb[:, dc, 0:1024], x2s[s][:], [X2S[s]], [WQB])
            else:
                acopy(wqb[:, dc, 1024:2048], x2s[s][:], [X2S[s]], [WQB])

    junk = sb([128, D_MODEL], BF16, "junkD")
    JUNK = Buf("junkD")
    st = sb([128, 8], F32, "stD")
    ST = Buf("stD")
    hn = sb([128, D_MODEL], F32, "hn")
    HN = Buf("hn")
    hnb = sb([128, D_MODEL], BF16, "hnb")
    HNB = Buf("hnb")
    hnT = sb([128, 8, 128], BF16, "hnT")
    HNT = Buf("hnT")
    qTs = sb([128, 16, 128], BF16, "qTs")
    QTS = Buf("qTs")
    S = sb([128, 16, 128], F32, "S")
    SB_ = Buf("S")
    wk = sb([128, 256], F32, "wk")
    tv = sb([128, 16, 16], F32, "tv")
    tiu = sb([128, 16, 16], U32, "tiu")
    tif = sb([128, 16, 16], F32, "tif")
    TK = Buf("topk1")
    cand = sb([128, 8, 16, 16], F32, "cand")
    CAND = Buf("cand")
    tops = sb([128, 8, 16], F32, "tops")
    posu = sb([128, 8, 16], U32, "posu")
    piu = sb([128, 8, 16], U32, "piu")
    pju = sb([128, 8, 16], U32, "pju")
    pif = sb([128, 8, 16], F32, "pif")
    pjf = sb([128, 8, 16], F32, "pjf")
    TK2 = Buf("topk2")
    oh = sb([128, 8, 16, 16], F32, "oh")
    OH = Buf("oh")
    i1s = sb([128, 8, 16], F32, "i1s")
    i2s = sb([128, 8, 16], F32, "i2s")
    ef = sb([128, 128], F32, "ef")
    gw = sb([128, 8, 16], F32, "gw")
    ssum = sb([128, 8], F32, "ssum")
    EG = Buf("eg")
    eTu = sb([128, 128], U32, "eTu")
    ET = Buf("eTu")
    gT = sb([128, 128], F32, "gT")
    GT = Buf("gT")
    hT = sb([128, 128], F32, "hT")
    HTD = Buf("hT")
    gl = sb([128, 128], F32, "gl")
    gl2 = sb([128, 128], F32, "gl2")
    ghT = sb([128, 128], F32, "ghT")
    GH = Buf("ghT")
    GHB = [Buf(f"ghb{i}") for i in range(4)]
    GHK = [Buf(f"ghk{i}") for i in range(4)]
    NSL = 16
    SUBB = 4
    NG = NSL // SUBB
    gsl = [sb([128, 2 * D_MODEL], BF16, f"gs{i}") for i in range(NSL)]
    GS = [Buf(f"gs{i}") for i in range(NSL)]
    ghb = sb([128, 128], BF16, "ghb")
    bsb = [sb([128, D_MODEL], BF16, f"bsb{i}") for i in range(2)]
    BSB = [Buf(f"bsb{i}") for i in range(2)]
    UVD = Buf("uvd")
    for a in range(128):
        s = a % 2
        stgf = gsl[2 * s][:].bitcast(F32)
        stgv = gsl[2 * s + 1][:].bitcast(F32)
        stgb = gsl[4 + s]
        dma("sp", stgf, peer_u[a * 128:(a + 1) * 128, :], f"pu{s}", [], [GS[2 * s]])
        dma("sp", stgv, peer_v[a * 128:(a + 1) * 128, :], f"pv{s}", [], [GS[2 * s + 1]])
        vcopy("pool", stgb[:, 0:1024], stgf, [GS[2 * s]], [GS[4 + s]])
        acopy(stgb[:, 1024:2048], stgv, [GS[2 * s + 1]], [GS[4 + s]])
        dma("sp", uv_d[a * 128:(a + 1) * 128, :], stgb[:], f"po{s}", [GS[4 + s]], [UVD])
    barrier()
    oT = sb([128, 8, 128], F32, "oT")
    OT = Buf("oT")
    yb = sb([128, D_MODEL], F32, "yb")
    YBD = Buf("yb")
    yo = [sb([128, D_MODEL], F32, f"yo{i}") for i in range(2)]
    YO = [Buf(f"yo{i}") for i in range(2)]
    psO = PSD[2][:, :].rearrange("p (c t) -> p c t", c=8)
    gcnt = 0
    GELU_C = 2.0 * 0.7978845608028654
    n_tiles_d = NT_OWN if ntile_d is None else ntile_d
    hnb2 = [hnb, sb([128, D_MODEL], BF16, "hnb1")]
    HNB2 = [HNB, Buf("hnb1")]
    eTu2 = [eTu, sb([128, 128], U32, "eTu1")]
    ET2 = [ET, Buf("eTu1")]
    gT2 = [gT, sb([128, 128], F32, "gT1")]
    GT2 = [GT, Buf("gT1")]
    st_e = sb([128, 8], F32, "st_e")
    STE = Buf("st_e")
    junk_e = sb([128, D_MODEL], BF16, "junk_e")
    JUNKE = Buf("junk_e")
    junk_d = sb([128, D_MODEL], BF16, "junk_d")
    HTDk = [Buf(f"hTk{i}") for i in range(4)]

    def top16(src_ap, n, tv_ap, ti_ap, R, W):
        P.op("dve", lambda h: h.max(out=tv_ap[:, 0:8], in_=src_ap), R, W)
        P.op("dve", lambda h: h.max_index(out=ti_ap[:, 0:8], in_max=tv_ap[:, 0:8], in_values=src_ap), R, W)
        P.op("dve", lambda h: h.match_replace(out=wk[:, 0:n], in_to_replace=tv_ap[:, 0:8], in_values=src_ap,
                                              imm_value=-1e30), R, W)
        P.op("dve", lambda h: h.max(out=tv_ap[:, 8:16], in_=wk[:, 0:n]), R, W)
        P.op("dve", lambda h: h.max_index(out=ti_ap[:, 8:16], in_max=tv_ap[:, 8:16], in_values=wk[:, 0:n]), R, W)

    def retrieval(ti):
        tok = ti * 128
        s = ti % 2
        hb, HB = hnb2[s], HNB2[s]
        dma("sp", x2s[s][:], x2_d[tok:tok + 128, :], f"x2s{s}", [X2D[ti]], [X2S[s]])
        P.op("dve", (lambda a, c: (lambda h: h.scalar_tensor_tensor(
            out=junk[:], in0=a, scalar=1.0, in1=a, op0=ALU.mult, op1=ALU.mult, accum_out=c)))(
            x2s[s][:], st[:, 0:1]), [X2S[s]], [JUNK, ST])
        act(st[:, 1:2], st[:, 0:1], AF.Ln, [ST], [ST], bias=epsc[:, 0:1], scale=1.0 / D_MODEL)
        act(st[:, 2:3], st[:, 1:2], AF.Exp, [ST], [ST], scale=-0.5)
        stt(hn[:], x2s[s][:], st[:, 2:3], g2bc[:], ALU.mult, ALU.mult, [X2S[s], ST, CONST], [HN])
        acopy(hb[:], hn[:], [HN], [HB])
        yield
        for dc in range(8):
            tr(psbf(7)[:, dc * 128:(dc + 1) * 128], hb[:, dc * 128:(dc + 1) * 128], ident_b[:],
               [HB, CONST], [PSB[7]])
        acopy(hnT[:], psbf(7).rearrange("p (c t) -> p c t", c=8), [PSB[7]], [HNT])
        yield
        for r in range(4):
            b = 6 + (r % 2)
            for a in range(4):
                jj = 4 * r + a
                for dc in range(8):
                    mm(PS[b][:, a * 128:(a + 1) * 128], wqb[:, dc, jj * 128:(jj + 1) * 128],
                       hnT[:, dc, :], dc == 0, dc == 7, [WQB, HNT], [PSB[b]])
            acopy(qTs[:, 4 * r:4 * r + 4, :], PS[b].rearrange("p (a t) -> p a t", a=4), [PSB[b]], [QTS])
            yield
        for r in range(4):
            b = 6 + (r % 2)
            for a in range(4):
                jj = 4 * r + a
                mm(PS[b][:, a * 128:(a + 1) * 128], qTs[:, jj, :], keysT[:, jj % 2, :], True, True,
                   [QTS, CONST], [PSB[b]])
            acopy(S[:, 4 * r:4 * r + 4, :], PS[b].rearrange("p (a t) -> p a t", a=4), [PSB[b]], [SB_])
            yield
        for jj in range(16):
            top16(S[:, jj, :], 128, tv[:, jj, :], tiu[:, jj, :], [SB_], [TK])
            yield
        vcopy("dve", tif[:], tiu[:], [TK], [TK])
        tvv = tv[:].rearrange("p (h a) k -> p h a k", a=2)
        tifv = tif[:].rearrange("p (h a) k -> p h a k", a=2)
        tt("dve", cand[:], tvv[:, :, 0, :].unsqueeze(3).to_broadcast([128, 8, 16, 16]),
           tvv[:, :, 1, :].unsqueeze(2).to_broadcast([128, 8, 16, 16]), ALU.add, [TK], [CAND])
        yield
        for hh in range(8):
            top16(cand[:, hh, :, :].rearrange("p a b -> p (a b)"), 256, tops[:, hh, :], posu[:, hh, :],
                  [CAND], [TK2])
            yield
        ts("dve", piu[:], posu[:], 4, None, ALU.logical_shift_right, None, [TK2], [TK2])
        ts("dve", pju[:], posu[:], 15, None, ALU.bitwise_and, None, [TK2], [TK2])
        vcopy("dve", pif[:], piu[:], [TK2], [TK2])
        vcopy("dve", pjf[:], pju[:], [TK2], [TK2])
        yield
        io16 = iota16[:].unsqueeze(1).unsqueeze(1).to_broadcast([128, 8, 16, 16])
        for (pf, col, dst) in ((pif, 0, i1s), (pjf, 1, i2s)):
            tt("dve", oh[:], io16, pf[:].unsqueeze(3).to_broadcast([128, 8, 16, 16]), ALU.is_equal,
               [TK2, CONST], [OH])
            tt("pool", oh[:], oh[:], tifv[:, :, col, :].unsqueeze(2).to_broadcast([128, 8, 16, 16]), ALU.mult,
               [OH, TK], [OH])
            reduce_add(dst[:], oh[:], [OH], [EG])
            yield
        stt(ef[:], i1s[:].rearrange("p h k -> p (h k)"), 128.0, i2s[:].rearrange("p h k -> p (h k)"),
            ALU.mult, ALU.add, [EG], [EG])
        tt("dve", gw[:], tops[:], tops[:, :, 0:1].to_broadcast([128, 8, 16]), ALU.subtract, [TK2], [EG])
        act(gw[:], gw[:], AF.Exp, [EG], [EG])
        yield
        reduce_add(ssum[:], gw[:], [EG], [EG])
        recip(ssum[:], ssum[:], [EG], [EG])
        tt("dve", gw[:], gw[:], ssum[:].unsqueeze(2).to_broadcast([128, 8, 16]), ALU.mult, [EG], [EG])
        tr(PS[7][:, 0:128], ef[:], ident_f[:], [EG, CONST], [PSB[7]])
        tr(PS[7][:, 128:256], gw[:].rearrange("p h k -> p (h k)"), ident_f[:], [EG, CONST], [PSB[7]])
        yield
        vcopy("dve", eTu2[s][:], PS[7][:, 0:128], [PSB[7]], [ET2[s]])
        acopy(gT2[s][:], PS[7][:, 128:256], [PSB[7]], [GT2[s]])
        if debug and ti == 0:
            o = dout("d_e", [128, 128], U32)
            dma("sp", o, eTu2[s][:], "dbg", [ET2[s]], [])
            o = dout("d_g", [128, 128], F32)
            dma("sp", o, gT2[s][:], "dbg", [GT2[s]], [])
        yield

    def drain(gen):
        if gen is None:
            return
        for _ in gen:
            pass

    def step(gen):
        if gen is None:
            return None
        try:
            next(gen)
            return gen
        except StopIteration:
            return None

    nsb = 128 // SUBB
    gen = retrieval(0)
    drain(gen)
    for ti in range(n_tiles_d):
        tok = ti * 128
        s = ti % 2
        hb, HB, eT_, ETB, gT_, GTB = hnb2[s], HNB2[s], eTu2[s], ET2[s], gT2[s], GT2[s]
        gen = retrieval(ti + 1) if ti + 1 < n_tiles_d else None

        def stageA(k):
            nonlocal gcnt
            for t in range(k * SUBB, (k + 1) * SUBB):
                sl = (k % NG) * SUBB + (t % SUBB)
                pb = (gcnt % 2) * 2
                gcnt += 1
                P.dma("pool", (lambda o, ia: (lambda h: h.indirect_dma_start(
                    out=o, out_offset=None, in_=uv_d[:, :],
                    in_offset=bass.IndirectOffsetOnAxis(ap=ia, axis=0))))(gsl[sl][:], eT_[:, t:t + 1]),
                    f"g{sl}", [ETB, UVD], [GS[sl]])
                lh = ident_b[:, t:t + 1].to_broadcast([128, 128])
                mm(PS[pb][:, :], lh, hb[:, 0:512], True, True, [HB, CONST], [PSB[pb], PSB[pb + 1]])
                mm(PS[pb + 1][:, :], lh, hb[:, 512:1024], True, True, [HB, CONST], [PSB[pb], PSB[pb + 1]])
                W = [HTDk[k % 4]] if (t % SUBB) in (0, SUBB - 1) else []
                P.op("dve", (lambda a, b_, c: (lambda h: h.scalar_tensor_tensor(
                    out=junk_d[:], in0=a, scalar=1.0, in1=b_, op0=ALU.mult, op1=ALU.mult, accum_out=c)))(
                    gsl[sl][:, 0:1024], PSD[pb // 2][:, :], hT[:, t:t + 1]), [GS[sl], PSB[pb], PSB[pb + 1]], W)

        def stageG1(k):
            c0, c1 = k * SUBB, (k + 1) * SUBB
            HK = HTDk[k % 4]
            G = GHK[k % 4]
            tt("dve", gl[:, c0:c1], hT[:, c0:c1], hT[:, c0:c1], ALU.mult, [HK], [G])
            ts("dve", gl[:, c0:c1], gl[:, c0:c1], 0.044715, 1.0, ALU.mult, ALU.add, [G], [G])
            tt("dve", gl[:, c0:c1], gl[:, c0:c1], hT[:, c0:c1], ALU.mult, [G, HK], [G])
            act(gl2[:, c0:c1], gl[:, c0:c1], AF.Exp, [G], [G], scale=-GELU_C)

        def stageG2(k):
            c0, c1 = k * SUBB, (k + 1) * SUBB
            HK = HTDk[k % 4]
            G = GHK[k % 4]
            ts("dve", gl2[:, c0:c1], gl2[:, c0:c1], 1.0, None, ALU.add, None, [G], [G])
            recip(gl2[:, c0:c1], gl2[:, c0:c1], [G], [G])
            tt("dve", gl2[:, c0:c1], gl2[:, c0:c1], hT[:, c0:c1], ALU.mult, [G, HK], [G])
            tt("dve", ghb[:, c0:c1], gl2[:, c0:c1], gT_[:, c0:c1], ALU.mult, [G, GTB], [GHB[k % 4]])

        def stageS(k):
            for t in range(k * SUBB, (k + 1) * SUBB):
                sl = (k % NG) * SUBB + (t % SUBB)
                for c in range(8):
                    mm(psO[:, c, t:t + 1], gsl[sl][:, D_MODEL + c * 128:D_MODEL + (c + 1) * 128], ghb[:, t:t + 1],
                       True, True, [GS[sl], GHB[k % 4]], [PSB[4], PSB[5]])

        for k in range(nsb + 3):
            if 0 <= k - 3 < nsb:
                stageS(k - 3)
            if k < nsb:
                stageA(k)
            if 0 <= k - 1 < nsb:
                stageG1(k - 1)
            if 0 <= k - 2 < nsb:
                stageG2(k - 2)
            gen = step(gen)
            gen = step(gen)
        drain(gen)
        acopy(oT[:], psO, [PSB[4], PSB[5]], [OT])
        for c in range(8):
            tr(PSD[3][:, c * 128:(c + 1) * 128], oT[:, c, :], ident_f[:], [OT, CONST], [PSB[6], PSB[7]])
        tt("dve", yb[:], PSD[3][:, :], x2s[s][:], ALU.add, [PSB[6], PSB[7], X2S[s]], [YBD])
        P.op("dve", (lambda a, c: (lambda h: h.scalar_tensor_tensor(
            out=junk_e[:], in0=a, scalar=1.0, in1=a, op0=ALU.mult, op1=ALU.mult, accum_out=c)))(
            yb[:], st_e[:, 4:5]), [YBD], [JUNKE, STE])
        act(st_e[:, 5:6], st_e[:, 4:5], AF.Ln, [STE], [STE], bias=epsc[:, 0:1], scale=1.0 / D_MODEL)
        act(st_e[:, 6:7], st_e[:, 5:6], AF.Exp, [STE], [STE], scale=-0.5)
        stt(yo[s][:], yb[:], st_e[:, 6:7], gfbc[:], ALU.mult, ALU.mult, [YBD, STE, CONST], [YO[s]])
        dma("sp", out_d[tok:tok + 128, :], yo[s][:], f"yo{s}", [YO[s]], [])
    barrier()
    P.emit()
    return nc, dbg


def make_in_maps(inputs):
    x = np.ascontiguousarray(np.asarray(inputs["x"], dtype=np.float32))
    shared = {}
    for k in ("norm1_g", "w_in", "q_norm_g", "k_norm_g", "conv_b", "conv_ln_g", "conv_ln_b", "w_out",
              "norm2_g", "peer_wq", "peer_keys", "peer_u", "peer_v", "final_g"):
        shared[k] = np.ascontiguousarray(np.asarray(inputs[k], dtype=np.float32))
    shared["conv_dw"] = np.ascontiguousarray(np.asarray(inputs["conv_dw"], dtype=np.float32).reshape(CONV_W, 512))
    maps = []
    for c in range(8):
        b, hf = c // 2, c % 2
        own0 = hf * NOWN
        oth0 = (1 - hf) * NOWN
        m = dict(shared)
        m["x_own"] = x[b, own0:own0 + NOWN]
        m["x_oth"] = x[b, oth0:oth0 + NOWN]
        halo = np.zeros((32, D_MODEL), np.float32)
        if hf == 1:
            halo[0:15] = x[b, own0 - 15:own0]
        else:
            halo[16:31] = x[b, own0 + NOWN:own0 + NOWN + 15]
        m["x_halo"] = halo
        pos = np.concatenate([np.arange(own0, own0 + NOWN), np.arange(oth0, oth0 + NOWN)])
        pos = pos.reshape(NT_ALL, 128).T
        rc = np.stack([pos // 64, pos % 64], axis=-1).astype(np.float32)
        m["rowcol"] = np.ascontiguousarray(rc)
        maps.append(m)
    return maps


_NC_CACHE = {}


def kernel(**inputs):
    if "nc" not in _NC_CACHE:
        _NC_CACHE["nc"] = build_program("D", False)[0]
    nc = _NC_CACHE["nc"]
    maps = make_in_maps(inputs)
    res = run_bass_kernel_spmd(nc, maps, core_ids=list(range(8)))
    out = np.empty((4, SEQ, D_MODEL), np.float32)
    for c in range(8):
        b, hf = c // 2, c % 2
        out[b, hf * NOWN:(hf + 1) * NOWN] = res.results[c]["out"]
    return out
```

```python
import bisect
from contextlib import ExitStack
import numpy as np
import concourse.bass as bass
import concourse.mybir as mybir
from concourse.bass_utils import run_bass_kernel_spmd

F32 = mybir.dt.float32
F32R = mybir.dt.float32r
BF16 = mybir.dt.bfloat16
U32 = mybir.dt.uint32
I32 = mybir.dt.int32
ALU = mybir.AluOpType
AF = mybir.ActivationFunctionType
AX = mybir.AxisListType

SEM_LIMIT = 30000


class _Cut(Exception):
    pass


class Buf:
    def __init__(self, name, excl=False):
        self.name = name
        self.last_w = None
        self.readers = []
        self.excl = excl


class DSem:
    def __init__(self, prog, name):
        self.prog = prog
        self.name = name
        self.sem = prog.nc.alloc_semaphore(name=name)
        self.total = 0
        self.group_ends = []
        self.open = False

    def need(self, v):
        i = bisect.bisect_left(self.group_ends, v)
        if i < len(self.group_ends):
            return self.group_ends[i]
        self.group_ends.append(self.total)
        self.open = False
        return self.total


class Prog:
    ENG = ("pe", "dve", "act", "pool", "sp")

    def __init__(self, nc):
        self.nc = nc
        self.handles = {"pe": nc.tensor, "dve": nc.vector, "act": nc.scalar,
                        "pool": nc.gpsimd, "sp": nc.sync}
        self.lists = {e: [] for e in self.ENG}
        self.cnt = {e: 0 for e in self.ENG}
        self.gen = {e: 0 for e in self.ENG}
        self.sems = {e: [nc.alloc_semaphore(name=f"s_{e}_0")] for e in self.ENG}
        self.waited = {}
        self.same_engine_sync = {"pe": False, "dve": True, "act": True,
                                 "pool": True, "sp": False}
        self.dsems = {}
        self.old_dsems = []
        self.n_inst = 0

    def _wait(self, eng, ev):
        kind, key, val = ev
        if kind == "e":
            e2, g = key
            if e2 == eng and not self.same_engine_sync[eng]:
                return
            sem = self.sems[e2][g]
            wkey = (eng, "e", e2, g)
            need = val
        else:
            ds = key
            need = ds.need(val)
            sem = ds.sem
            wkey = (eng, "d", ds.name)
        if self.waited.get(wkey, 0) >= need:
            return
        self.waited[wkey] = need
        self.lists[eng].append(("wait", sem, need))

    def _deps(self, eng, reads, writes):
        evs = []
        for b in reads:
            if b.last_w is not None:
                evs.append(b.last_w)
            if b.excl:
                for ev in b.readers:
                    if ev[0] == "e" and ev[1][0] != eng:
                        evs.append(ev)
        for b in writes:
            if b.last_w is not None:
                evs.append(b.last_w)
            evs.extend(b.readers)
        best = {}
        for ev in evs:
            k = (ev[0], ev[1] if ev[0] == "e" else id(ev[1]))
            if k not in best or ev[2] > best[k][2]:
                best[k] = ev
        for ev in best.values():
            self._wait(eng, ev)

    def _commit(self, ev, reads, writes):
        for b in writes:
            b.last_w = ev
            b.readers = []
        for b in reads:
            if b not in writes:
                b.readers.append(ev)
                if len(b.readers) > 64:
                    b.readers = b.readers[-64:]

    def op(self, eng, fn, reads=(), writes=()):
        reads = list(reads)
        writes = list(writes)
        self._deps(eng, reads, writes)
        if self.cnt[eng] >= SEM_LIMIT:
            self.gen[eng] += 1
            self.cnt[eng] = 0
            self.sems[eng].append(self.nc.alloc_semaphore(name=f"s_{eng}_{self.gen[eng]}"))
        self.cnt[eng] += 1
        g = self.gen[eng]
        self.lists[eng].append(("op", fn, self.sems[eng][g]))
        ev = ("e", (eng, g), self.cnt[eng])
        self._commit(ev, reads, writes)
        self.n_inst += 1
        return ev

    def dsem(self, name):
        if name not in self.dsems:
            self.dsems[name] = DSem(self, "d_" + name)
        return self.dsems[name]

    def dma(self, queue, fn, dsem, reads=(), writes=()):
        ds = self.dsem(dsem) if isinstance(dsem, str) else dsem
        if ds.total >= SEM_LIMIT and not ds.open and isinstance(dsem, str):
            self._wait(queue, ("d", ds, ds.total))
            self._dgen = getattr(self, "_dgen", 0) + 1
            ds = DSem(self, f"d_{dsem}_{self._dgen}")
            self.dsems[dsem] = ds
            self.old_dsems.append(ds)
        reads = list(reads)
        writes = list(writes)
        self._deps(queue, reads, writes)
        if (not ds.open) and ds.total > 0:
            self._wait(queue, ("d", ds, ds.total))
        ds.total += 16
        ds.open = True
        self.lists[queue].append(("dma", fn, ds.sem))
        ev = ("d", ds, ds.total)
        self._commit(ev, reads, writes)
        self.n_inst += 1
        return ev

    def wait_all(self, eng, bufs):
        for b in bufs:
            if b.last_w is not None:
                self._wait(eng, b.last_w)

    def emit(self):
        nc = self.nc
        with nc.Block() as block:
            def mk(ename):
                items = self.lists[ename]

                def body(h):
                    for it in items:
                        if it[0] == "wait":
                            h.wait_ge(it[1], it[2])
                        elif it[0] == "op":
                            it[1](h).then_inc(it[2], 1)
                        else:
                            it[1](h).then_inc(it[2], 16)
                return body
            block.tensor(mk("pe"))
            block.vector(mk("dve"))
            block.scalar(mk("act"))
            block.gpsimd(mk("pool"))
            block.sync(mk("sp"))


D_MODEL = 1024
SEQ = 8192
NOWN = 4096
HD = 64
CONV_W = 31
IN_W = 1792
EPS = 1e-6
NT_OWN = NOWN // 128
NT_ALL = SEQ // 128
TWO_PI = 2.0 * np.pi


def build_program(stop_after="D", debug=False, grp_list=None, pool_eng="pool", cut=None, ntile_d=None):
    nc = bass.Bass("TRN2", target_bir_lowering=False)
    P = Prog(nc)
    dbg = {}

    def din(name, shape, dt=F32):
        return nc.dram_tensor(name, list(shape), dt, kind="ExternalInput").ap()

    x_own = din("x_own", [NOWN, D_MODEL])
    x_oth = din("x_oth", [NOWN, D_MODEL])
    x_halo = din("x_halo", [32, D_MODEL])
    rowcol = din("rowcol", [128, NT_ALL, 2])
    norm1_g = din("norm1_g", [D_MODEL])
    w_in = din("w_in", [D_MODEL, IN_W])
    q_norm_g = din("q_norm_g", [HD])
    k_norm_g = din("k_norm_g", [HD])
    conv_dw = din("conv_dw", [CONV_W, 512])
    conv_b = din("conv_b", [512])
    conv_ln_g = din("conv_ln_g", [512])
    conv_ln_b = din("conv_ln_b", [512])
    w_out = din("w_out", [D_MODEL, D_MODEL])
    norm2_g = din("norm2_g", [D_MODEL])
    peer_wq = din("peer_wq", [D_MODEL, 2048])
    peer_keys = din("peer_keys", [2, 128, 128])
    peer_u = din("peer_u", [16384, D_MODEL])
    peer_v = din("peer_v", [16384, D_MODEL])
    final_g = din("final_g", [D_MODEL])
    out_d = nc.dram_tensor("out", [NOWN, D_MODEL], F32, kind="ExternalOutput").ap()
    x2_d = nc.dram_tensor("x2_scratch", [NOWN, D_MODEL], F32, kind="Internal").ap()
    uv_d = nc.dram_tensor("uv_scratch", [16384, 2 * D_MODEL], BF16, kind="Internal").ap()

    def dout(name, shape, dt=F32):
        dbg[name] = nc.dram_tensor(name, list(shape), dt, kind="ExternalOutput").ap()
        return dbg[name]

    _n = [0]
    cst = ExitStack()
    pers = ExitStack()
    scope = [ExitStack()]

    def sb(shape, dt=F32, name=None, persistent=False):
        _n[0] += 1
        nm = name or f"sb{_n[0]}"
        if persistent == "c":
            return cst.enter_context(nc.sbuf_tensor(nm, list(shape), dt, side="right"))
        if persistent:
            return pers.enter_context(nc.sbuf_tensor(nm, list(shape), dt, side="right"))
        return scope[0].enter_context(nc.sbuf_tensor(nm, list(shape), dt, side="left"))

    def new_scope():
        barrier()
        scope[0].close()
        scope[0] = ExitStack()

    def mm(out, lhsT, rhs, start, stop, R, W):
        P.op("pe", lambda h: h.matmul(out, lhsT=lhsT, rhs=rhs, start=start, stop=stop), R, W)

    def tr(out, in_, ident, R, W):
        P.op("pe", lambda h: h.transpose(out=out, in_=in_, identity=ident), R, W)

    def act(out, in_, func, R, W, bias=None, scale=None, accum=None):
        kw = {}
        if bias is not None:
            kw["bias"] = bias
        if scale is not None:
            kw["scale"] = scale
        if accum is not None:
            kw["accum_out"] = accum
        P.op("act", lambda h: h.activation(out=out, in_=in_, func=func, **kw), R, W)

    def acopy(out, in_, R, W):
        P.op("act", lambda h: h.copy(out=out, in_=in_), R, W)

    def tt(eng, out, in0, in1, op, R, W):
        P.op(eng, lambda h: h.tensor_tensor(out=out, in0=in0, in1=in1, op=op), R, W)

    def ts(eng, out, in0, s1, s2, op0, op1, R, W):
        if op1 is None:
            P.op(eng, lambda h: h.tensor_scalar(out=out, in0=in0, scalar1=s1, scalar2=None, op0=op0), R, W)
        else:
            P.op(eng, lambda h: h.tensor_scalar(out=out, in0=in0, scalar1=s1, scalar2=s2, op0=op0, op1=op1), R, W)

    def stt(out, in0, scalar, in1, op0, op1, R, W):
        P.op("dve", lambda h: h.scalar_tensor_tensor(out=out, in0=in0, scalar=scalar, in1=in1, op0=op0, op1=op1), R, W)

    def vcopy(eng, out, in_, R, W):
        P.op(eng, lambda h: h.tensor_copy(out=out, in_=in_), R, W)

    def recip(out, in_, R, W):
        P.op("dve", lambda h: h.reciprocal(out=out, in_=in_), R, W)

    def reduce_add(out, in_, R, W):
        P.op("dve", lambda h: h.tensor_reduce(out=out, in_=in_, axis=AX.X, op=ALU.add), R, W)

    def memset(eng, ap, val, W):
        P.op(eng, lambda h: h.memset(ap, val), [], W)

    def dma(q, out, in_, ds, R, W, slow=False):
        if slow:
            P.dma(q, lambda h: h.dma_start(out=out, in_=in_, allow_slow_non_contiguous=True), ds, R, W)
        else:
            P.dma(q, lambda h: h.dma_start(out=out, in_=in_), ds, R, W)

    def barrier():
        evs = []
        for e in P.ENG:
            if P.cnt[e] > 0:
                evs.append(("e", (e, P.gen[e]), P.cnt[e]))
        for ds in list(P.dsems.values()):
            if ds.total > 0:
                evs.append(("d", ds, ds.total))
        for e in P.ENG:
            for ev in evs:
                if ev[0] == "e" and ev[1][0] == e:
                    continue
                P._wait(e, ev)

    PSD = [nc.alloc_psum_tensor(f"pd{i}", [128, 1024], F32) for i in range(4)]
    PS = [PSD[i // 2][:, (i % 2) * 512:(i % 2 + 1) * 512] for i in range(8)]
    PSB = [Buf(f"bank{i}", excl=True) for i in range(8)]

    def psbf(i):
        return PS[i].bitcast(BF16)

    CONST = Buf("const")
    ident_f = sb([128, 128], F32, "ident_f", persistent="c")
    ident_b = sb([128, 128], BF16, "ident_b", persistent="c")
    iot = sb([128, 128], F32, "iot", persistent="c")
    P.op("pool", lambda h: h.iota(iot[:], pattern=[[1, 128]], base=0, channel_multiplier=-1,
                                  allow_small_or_imprecise_dtypes=True), [], [CONST])
    ts("dve", ident_f[:], iot[:], 0.0, None, ALU.is_equal, None, [CONST], [CONST])
    vcopy("dve", ident_b[:], ident_f[:], [CONST], [CONST])
    ones_f = sb([128, 128], F32, "ones_f", persistent="c")
    memset("dve", ones_f[:], 1.0 / 512.0, [CONST])
    iota16 = sb([128, 16], F32, "iota16", persistent="c")
    epsc = sb([128, 1], F32, "epsc", persistent="c")
    memset("dve", epsc[:], EPS, [CONST])
    P.op("pool", lambda h: h.iota(iota16[:], pattern=[[1, 16]], base=0, channel_multiplier=0,
                                  allow_small_or_imprecise_dtypes=True), [], [CONST])

    g1 = sb([128, 8], F32, "g1", persistent="c")
    dma("sp", g1[:], norm1_g.rearrange("(c p) -> p c", p=128), "c0", [], [CONST], slow=True)
    cb = sb([128, 4], F32, "cb", persistent="c")
    lg = sb([128, 4], F32, "lg", persistent="c")
    lb = sb([128, 4], F32, "lb", persistent="c")
    dma("sp", cb[:], conv_b.rearrange("(c p) -> p c", p=128), "c0", [], [CONST], slow=True)
    dma("sp", lg[:], conv_ln_g.rearrange("(c p) -> p c", p=128), "c0", [], [CONST], slow=True)
    dma("sp", lb[:], conv_ln_b.rearrange("(c p) -> p c", p=128), "c0", [], [CONST], slow=True)
    G10 = sb([128, 10, 64], F32, "G10", persistent="c")
    dma("sp", G10[:, 0, :], q_norm_g.partition_broadcast(128), "c0", [], [CONST])
    dma("sp", G10[:, 8, :], k_norm_g.partition_broadcast(128), "c0", [], [CONST])
    ts("dve", G10[:, 0, :], G10[:, 0, :], HD ** -0.5, None, ALU.mult, None, [CONST], [CONST])
    for j in range(1, 8):
        vcopy("dve", G10[:, j, :], G10[:, 0, :], [CONST], [CONST])
    vcopy("dve", G10[:, 9, :], G10[:, 8, :], [CONST], [CONST])
    KT = sb([128, SEQ], BF16, "KT", persistent=True)
    VA = sb([128, NT_ALL, 2, 128], BF16, "VA", persistent=True)
    QT = sb([128, 4, NOWN], BF16, "QT", persistent=True)
    htstack = ExitStack()
    HT = htstack.enter_context(nc.sbuf_tensor("HT", [128, 4, NOWN + 32], BF16, side="left"))

    rc_t = sb([128, NT_ALL, 2], F32, "rc_t")
    dma("sp", rc_t[:], rowcol, "c0", [], [CONST])
    invf = sb([128, 16], F32, "invf")
    act(invf[:], iota16[:], AF.Exp, [CONST], [CONST], scale=-float(np.log(10000.0)) / 16.0)
    NTAB = NT_ALL * 32
    cos_t = sb([128, NT_ALL, 2, 16], F32, "cos_t")
    sin_t = sb([128, NT_ALL, 2, 16], F32, "sin_t")
    w1b = sb([128, 8, IN_W], BF16, "w1b")
    W1B = Buf("w1b")
    with nc.sbuf_tensor("rr_k", [128, NTAB], I32, side="left") as rr_k, \
            nc.sbuf_tensor("rr_f", [128, NTAB], F32, side="left") as rr_f, \
            nc.sbuf_tensor("rr_a", [128, NTAB], F32, side="left") as rr_a, \
            nc.sbuf_tensor("ang", [128, NT_ALL, 2, 16], F32, side="left") as ang:
        tt("dve", ang[:], rc_t[:].unsqueeze(3).to_broadcast([128, NT_ALL, 2, 16]),
           invf[:].unsqueeze(1).unsqueeze(1).to_broadcast([128, NT_ALL, 2, 16]), ALU.mult, [CONST], [CONST])
        angf = ang[:].rearrange("p a b c -> p (a b c)")
        TMPB = Buf("ropetmp")
        for tab, shift in ((sin_t, 0.0), (cos_t, np.pi / 2)):
            tf = tab[:].rearrange("p a b c -> p (a b c)")
            ts("dve", rr_a[:], angf, float(shift), None, ALU.add, None, [CONST], [TMPB])
            ts("dve", rr_k[:], rr_a[:], 1.0 / TWO_PI, 0.5, ALU.mult, ALU.add, [TMPB], [TMPB])
            vcopy("dve", rr_f[:], rr_k[:], [TMPB], [TMPB])
            stt(rr_f[:], rr_f[:], -TWO_PI, rr_a[:], ALU.mult, ALU.add, [TMPB], [TMPB])
            ts("dve", rr_a[:], rr_f[:], -float(np.pi), TWO_PI, ALU.is_lt, ALU.mult, [TMPB], [TMPB])
            tt("dve", rr_f[:], rr_f[:], rr_a[:], ALU.add, [TMPB], [TMPB])
            ts("dve", rr_a[:], rr_f[:], float(np.pi), TWO_PI, ALU.is_gt, ALU.mult, [TMPB], [TMPB])
            tt("dve", rr_a[:], rr_f[:], rr_a[:], ALU.subtract, [TMPB], [TMPB])
            ts("dve", rr_a[:], rr_a[:], float(np.pi), -float(np.pi), ALU.min, ALU.max, [TMPB], [TMPB])
            act(tf, rr_a[:], AF.Sin, [TMPB], [CONST])
        barrier()
    with nc.sbuf_tensor("stg0", [128, IN_W], F32, side="left") as stg0, \
            nc.sbuf_tensor("stg1", [128, IN_W], F32, side="left") as stg1:
        stg = [stg0, stg1]
        STG = [Buf("stg0"), Buf("stg1")]
        for dc in range(8):
            s = dc % 2
            dma("sp", stg[s][:], w_in[dc * 128:(dc + 1) * 128, :], f"stg{s}", [], [STG[s]])
            ts("dve", w1b[:, dc, 0:512].rearrange("p (j g d) -> p j g d", j=4, g=2),
               stg[s][:, 0:512].rearrange("p (g j d) -> p j g d", g=2, j=4),
               g1[:, dc:dc + 1], None, ALU.mult, None, [STG[s], CONST], [W1B])
            ts("pool", w1b[:, dc, 512:IN_W], stg[s][:, 512:IN_W], g1[:, dc:dc + 1], None, ALU.mult, None,
               [STG[s], CONST], [W1B])
        barrier()

    if stop_after == "0":
        o = dout("d_cos", [128, NT_ALL, 2, 16], F32)
        dma("sp", o, cos_t[:], "dbg", [], [])
        o = dout("d_sin", [128, NT_ALL, 2, 16], F32)
        dma("sp", o, sin_t[:], "dbg", [], [])
        o = dout("d_w1b", [128, 8, IN_W], BF16)
        dma("sp", o, w1b[:], "dbg", [], [])
        o = dout("d_G10", [128, 10, 64], F32)
        dma("sp", o, G10[:], "dbg", [], [])
        barrier()
        P.emit()
        return nc, dbg
    KTB = [Buf(f"kt{i}") for i in range(NT_ALL)]
    VAB = [Buf(f"va{i}") for i in range(NT_ALL)]
    QTB = [Buf(f"qt{i}") for i in range(NT_OWN)]
    HTB = [Buf(f"ht{i}") for i in range(NT_OWN // 4 + 1)]
    VINIT = Buf("vinit")
    memset("pool", VA[:, :, :, 64:128].rearrange("p a b c -> p (a b) c"), 1.0, [VINIT])
    for b in VAB:
        b.last_w = VINIT.last_w
    memset("pool", HT[:, :, NOWN + 30:NOWN + 32], 0.0, [HTB[NT_OWN // 4]])

    xin = [sb([128, D_MODEL], F32, f"xin{i}") for i in range(2)]
    XIN = [Buf(f"xin{i}") for i in range(2)]
    junk = sb([128, D_MODEL], BF16, "junk")
    JUNK = Buf("junk")
    xs = [sb([128, D_MODEL], BF16, f"xs{i}") for i in range(2)]
    XS = [Buf(f"xs{i}") for i in range(2)]
    xT = [sb([128, 8, 512], BF16, f"xT{i}") for i in range(2)]
    XT = [Buf(f"xT{i}") for i in range(2)]
    st = sb([128, 8], F32, "st")
    ST = Buf("st")
    sq = sb([128, 640], F32, "sq")
    SQ = Buf("sq")
    ssq = sb([128, 10], F32, "ssq")
    qn = sb([128, 10, 64], F32, "qn")
    QN = Buf("qn")
    t1 = sb([128, 10, 64], F32, "t1")
    t2 = sb([128, 10, 64], F32, "t2")
    T12 = Buf("t12")
    qr = sb([128, 640], BF16, "qr")
    QR = Buf("qr")
    sg = sb([128, 512], F32, "sg")
    SG = Buf("sg")

    def rms_rows(xt_ap, XB, np_, out_bf, OB):
        act(junk[0:np_, :], xt_ap, AF.Square, [XB], [JUNK, ST], accum=st[0:np_, 0:1])
        act(st[0:np_, 1:2], st[0:np_, 0:1], AF.Sqrt, [ST], [ST], bias=EPS, scale=1.0 / D_MODEL)
        recip(st[0:np_, 2:3], st[0:np_, 1:2], [ST], [ST])
        ts("dve", out_bf, xt_ap, st[0:np_, 2:3], None, ALU.mult, None, [XB, ST], [OB])

    try:
        n_groups = NT_ALL // 4
        tile_ctr = 0
        for grp in (grp_list if grp_list is not None else range(n_groups + 1)):
            halo = grp == n_groups
            own = grp < NT_OWN // 4
            gs = grp % 2
            nsub = 1 if halo else 4
            for sub in range(nsub):
                ti = grp * 4 + sub
                s = tile_ctr % 2
                tile_ctr += 1
                np_ = 32 if halo else 128
                if halo:
                    src = x_halo[:, :]
                elif own:
                    src = x_own[ti * 128:(ti + 1) * 128, :]
                else:
                    src = x_oth[(ti - NT_OWN) * 128:(ti - NT_OWN + 1) * 128, :]
                dma("sp", xin[s][0:np_, :], src, f"xin{s}", [], [XIN[s]])
                rms_rows(xin[s][0:np_, :], XIN[s], np_, xs[s][0:np_, :], XS[s])
                if cut == 1:
                    raise _Cut()
                pb = ti % 2
                for dc in range(8):
                    tr(psbf(pb)[:, dc * 128:dc * 128 + np_], xs[s][0:np_, dc * 128:(dc + 1) * 128],
                       ident_b[0:np_, 0:np_], [XS[s], CONST], [PSB[pb]])
                acopy(xT[gs][:, :, sub * 128:sub * 128 + np_],
                      psbf(pb).rearrange("p (c t) -> p c t", c=8)[:, :, 0:np_], [PSB[pb]], [XT[gs]])
                if cut == 2:
                    raise _Cut()
                if halo:
                    continue
                c0 = 0 if own else 512
                ncol = 768 - c0
                for (a, b) in (((0, 512), (512, 768)) if own else ((512, 768),)):
                    bank = 2 if a == 0 else 3
                    for dc in range(8):
                        mm(PS[bank][:, 0:b - a], xT[gs][:, dc, sub * 128:(sub + 1) * 128], w1b[:, dc, a:b],
                           dc == 0, dc == 7, [XT[gs], W1B], [PSB[bank]])
                if cut == 3:
                    raise _Cut()
                h0 = 0 if own else 8
                nh = 10 - h0
                if own:
                    act(sq[:, 0:512], PS[2][:, 0:512], AF.Square, [PSB[2]], [SQ])
                act(sq[:, 512:640], PS[3][:, 0:128], AF.Square, [PSB[3]], [SQ])
                reduce_add(ssq[:, h0:10], sq[:, h0 * 64:640].rearrange("p (h d) -> p h d", d=64), [SQ], [ST])
                act(ssq[:, h0:10], ssq[:, h0:10], AF.Sqrt, [ST], [ST], bias=EPS, scale=1.0 / HD)
                recip(ssq[:, h0:10], ssq[:, h0:10], [ST], [ST])
                if own:
                    tt("dve", qn[:, 0:8, :], PS[2][:, 0:512].rearrange("p (h d) -> p h d", d=64),
                       ssq[:, 0:8].unsqueeze(2).to_broadcast([128, 8, 64]), ALU.mult, [PSB[2], ST], [QN])
                tt("dve", qn[:, 8:10, :], PS[3][:, 0:128].rearrange("p (h d) -> p h d", d=64),
                   ssq[:, 8:10].unsqueeze(2).to_broadcast([128, 2, 64]), ALU.mult, [PSB[3], ST], [QN])
                if cut == 4:
                    raise _Cut()
                acopy(VA[:, ti, :, 0:64], PS[3][:, 128:256].rearrange("p (g d) -> p g d", g=2), [PSB[3]], [VAB[ti]])
                tt(pool_eng, qn[:, h0:10, :], qn[:, h0:10, :], G10[:, h0:10, :], ALU.mult, [QN, CONST], [QN])
                if cut == 5:
                    raise _Cut()
                for rc in range(2):
                    qv = qn[:, h0:10, rc * 32:(rc + 1) * 32].rearrange("p h (a d) -> p h a d", a=2)
                    cosb = cos_t[:, ti, rc, :].unsqueeze(1).unsqueeze(1).to_broadcast([128, nh, 2, 16])
                    sinb = sin_t[:, ti, rc, :].unsqueeze(1).to_broadcast([128, nh, 16])
                    t1v = t1[:, h0:10, rc * 32:(rc + 1) * 32].rearrange("p h (a d) -> p h a d", a=2)
                    t2v = t2[:, h0:10, rc * 32:(rc + 1) * 32].rearrange("p h (a d) -> p h a d", a=2)
                    tt("dve", t1v, qv, cosb, ALU.mult, [QN, CONST], [T12])
                    tt(pool_eng, t2v[:, :, 0, :], qv[:, :, 1, :], sinb, ALU.mult, [QN, CONST], [T12])
                    tt(pool_eng, t2v[:, :, 1, :], qv[:, :, 0, :], sinb, ALU.mult, [QN, CONST], [T12])
                    qrv = qr[:, h0 * 64:640].rearrange("p (h c) -> p h c", c=64)[:, :, rc * 32:(rc + 1) * 32] \
                        .rearrange("p h (a d) -> p h a d", a=2)
                    tt("dve", qrv[:, :, 0, :], t1v[:, :, 0, :], t2v[:, :, 0, :], ALU.subtract, [T12], [QR])
                    tt("dve", qrv[:, :, 1, :], t1v[:, :, 1, :], t2v[:, :, 1, :], ALU.add, [T12], [QR])
                    if cut == 6:
                        raise _Cut()
                if own:
                    for j in range(4):
                        tr(psbf(4)[:, j * 128:(j + 1) * 128], qr[:, j * 128:(j + 1) * 128], ident_b[:],
                           [QR, CONST], [PSB[4]])
                tr(psbf(4)[:, 512:640], qr[:, 512:640], ident_b[:], [QR, CONST], [PSB[4]])
                if cut == 71:
                    raise _Cut()
                if own:
                    acopy(QT[:, :, ti * 128:(ti + 1) * 128], psbf(4)[:, 0:512].rearrange("p (j t) -> p j t", j=4),
                          [PSB[4]], [QTB[ti]])
                if cut == 72:
                    raise _Cut()
                acopy(KT[:, ti * 128:(ti + 1) * 128], psbf(4)[:, 512:640], [PSB[4]], [KTB[ti]])
                if cut == 7:
                    raise _Cut()
            if own or halo:
                ntok = 32 if halo else 512
                for c in range(4):
                    for part, bank in ((0, 5), (1, 6)):
                        col = 768 + part * 512 + c * 128
                        for dc in range(8):
                            mm(PS[bank][:, 0:ntok], w1b[:, dc, col:col + 128], xT[gs][:, dc, 0:ntok],
                               dc == 0, dc == 7, [XT[gs], W1B], [PSB[bank]])
                    act(sg[:, 0:ntok], PS[6][:, 0:ntok], AF.Sigmoid, [PSB[6]], [SG])
                    if halo:
                        tt("dve", HT[:, c, 0:15], PS[5][:, 0:15], sg[:, 0:15], ALU.mult, [PSB[5], SG], [HTB[0]])
                        tt("dve", HT[:, c, NOWN + 15:NOWN + 30], PS[5][:, 16:31], sg[:, 16:31], ALU.mult,
                           [PSB[5], SG], [HTB[NT_OWN // 4]])
                    else:
                        tt("dve", HT[:, c, 15 + grp * 512:15 + (grp + 1) * 512], PS[5][:, :], sg[:, :], ALU.mult,
                           [PSB[5], SG], [HTB[grp]])
    except _Cut:
        pass
    barrier()

    if debug:
        o = dout("d_KT", [128, SEQ], BF16)
        dma("sp", o, KT[:], "dbg", [], [])
        o = dout("d_QT", [128, 4, NOWN], BF16)
        dma("sp", o, QT[:], "dbg", [], [])
        o = dout("d_VA", [128, NT_ALL, 2, 128], BF16)
        dma("sp", o, VA[:], "dbg", [], [])
        o = dout("d_HT", [128, 4, NOWN + 32], BF16)
        dma("sp", o, HT[:], "dbg", [], [])
        barrier()
    if stop_after == "A":
        P.emit()
        return nc, dbg

    new_scope()
    CT = sb([128, 4, NOWN], BF16, "CT", persistent=True)
    CTB = [Buf(f"ct{i}") for i in range(8)]
    dg = sb([128, 4, CONV_W, 128], BF16, "dg")
    DG = Buf("dg")
    cw = sb([128, 4, CONV_W], F32, "cw")
    cwr = sb([CONV_W, 512], F32, "cwr")
    CW = Buf("cw")
    dma("sp", cwr[:], conv_dw, "cw", [], [CW])
    for c in range(4):
        tr(PS[0][:, c * 32:c * 32 + CONV_W], cwr[:, c * 128:(c + 1) * 128], ident_f[0:CONV_W, 0:CONV_W],
           [CW, CONST], [PSB[0]])
    vcopy("dve", cw[:], PS[0][:, 0:128].rearrange("p (c j) -> p c j", c=4)[:, :, 0:CONV_W], [PSB[0]], [CW])
    for c in range(4):
        for j in range(CONV_W):
            eng = "dve" if (j % 2 == 0) else "pool"
            ts(eng, dg[:, c, j, :], ident_b[:], cw[:, c, j:j + 1], None, ALU.mult, None, [CW, CONST], [DG])
    ybuf = sb([128, 4, 512], F32, "ybuf")
    ysq = sb([128, 4, 512], F32, "ysq")
    YB = [Buf(f"yb{c}") for c in range(4)]
    YS = [Buf(f"ys{c}") for c in range(4)]
    m2 = sb([128, 512], F32, "m2")
    M2 = Buf("m2")
    rstd_c = sb([128, 512], F32, "rstd_c")
    RSC = Buf("rstd_c")
    tmpc = sb([128, 512], F32, "tmpc")
    TMPC = Buf("tmpc")
    for q in range(8):
        hreads = [HTB[q]] + ([HTB[q + 1]] if q + 1 <= 8 else []) + ([HTB[q - 1]] if q > 0 else [])
        for c in range(4):
            bank = c % 2
            for j in range(CONV_W):
                mm(PS[bank][:, :], dg[:, c, j, :], HT[:, c, q * 512 + j:q * 512 + j + 512],
                   j == 0, j == CONV_W - 1, hreads + [DG], [PSB[bank]])
            act(ybuf[:, c, :], PS[bank][:, :], AF.Identity, [PSB[bank], CONST], [YB[c]], bias=cb[:, c:c + 1])
            act(ysq[:, c, :], ybuf[:, c, :], AF.Square, [YB[c]], [YS[c]])
        for c in range(4):
            mm(PS[2][:, :], ones_f[:], ybuf[:, c, :], c == 0, c == 3, [YB[c], CONST], [PSB[2]])
        for c in range(4):
            mm(PS[3][:, :], ones_f[:], ysq[:, c, :], c == 0, c == 3, [YS[c], CONST], [PSB[3]])
        act(m2[:], PS[2][:, :], AF.Square, [PSB[2]], [M2])
        tt("dve", m2[:], PS[3][:, :], m2[:], ALU.subtract, [PSB[3], M2], [M2])
        act(m2[:], m2[:], AF.Sqrt, [M2], [M2], bias=EPS, scale=1.0)
        recip(rstd_c[:], m2[:], [M2], [RSC])
        for c in range(4):
            tt("dve", tmpc[:], ybuf[:, c, :], PS[2][:, :], ALU.subtract, [YB[c], PSB[2]], [TMPC])
            tt("dve", tmpc[:], tmpc[:], rstd_c[:], ALU.mult, [TMPC, RSC], [TMPC])
            act(CT[:, c, q * 512:(q + 1) * 512], tmpc[:], AF.Silu, [TMPC, CONST], [CTB[q]],
                bias=lb[:, c:c + 1], scale=lg[:, c:c + 1])
    barrier()
    if debug:
        o = dout("d_CT", [128, 4, NOWN], BF16)
        dma("sp", o, CT[:], "dbg", [], [])
        barrier()
    if stop_after == "B":
        P.emit()
        return nc, dbg

    new_scope()
    htstack.close()
    wob_a = sb([64, 8, D_MODEL], BF16, "wob_a")
    wob_c = sb([128, 4, D_MODEL], BF16, "wob_c")
    WOB = Buf("wob")
    xr = [sb([128, D_MODEL], F32, f"xr{i}") for i in range(2)]
    XR = [Buf(f"xr{i}") for i in range(2)]
    for hh in range(8):
        s = hh % 2
        dma("sp", xr[s][0:64, :], w_out[hh * 64:(hh + 1) * 64, :], f"xr{s}", [], [XR[s]])
        vcopy("dve", wob_a[:, hh, :], xr[s][0:64, :], [XR[s]], [WOB])
    for c in range(4):
        s = c % 2
        dma("sp", xr[s][:, :], w_out[512 + c * 128:512 + (c + 1) * 128, :], f"xr{s}", [], [XR[s]])
        vcopy("dve", wob_c[:, c, :], xr[s][:, :], [XR[s]], [WOB])
    pT = [sb([128, 1024], BF16, f"pT{i}") for i in range(3)]
    PT = [Buf(f"pT{i}") for i in range(3)]
    den = sb([64, 512], F32, "den")
    DEN = Buf("den")
    OTn = sb([64, 8, 512], BF16, "OTn")
    OTB = [Buf(f"otn{h}") for h in range(8)]
    x2t = [sb([128, D_MODEL], F32, f"x2t{i}") for i in range(2)]
    X2T = [Buf(f"x2t{i}") for i in range(2)]
    X2D = [Buf(f"x2d{i}") for i in range(NT_OWN)]
    NKP = NT_ALL // 2
    its = [(qt, g, j, kp) for qt in range(8) for g in range(2) for j in range(4) for kp in range(NKP)]
    LOOK = 1

    QTz = [sb([128, 8, 512], BF16, f"QTz{i}") for i in range(2)]
    QTZ = [Buf(f"qtz{i}") for i in range(2)]
    for zz in range(2):
        memset("pool", QTz[zz][:].rearrange("p a b -> p (a b)"), 0.0, [QTZ[zz]])
    qtz_done = set()

    def acopy_or_dve(out, in_, R, W):
        P.op("dve", lambda h: h.tensor_copy(out=out, in_=in_), R, W)

    def fill_qtz(qt):
        if qt in qtz_done:
            return
        qtz_done.add(qt)
        for g in range(2):
            for j in range(4):
                acopy_or_dve(QTz[qt % 2][g * 64:(g + 1) * 64, g * 4 + j, :],
                      QT[g * 64:(g + 1) * 64, j, qt * 512:(qt + 1) * 512],
                      [QTB[qt * 4 + ii] for ii in range(4)], [QTZ[qt % 2]])

    def issue_qk(i):
        qt, g, j, kp = its[i]
        fill_qtz(qt)
        sp = i % 2
        for u in range(2):
            kt = kp * 2 + u
            mm(PSD[sp][:, u * 512:(u + 1) * 512], KT[:, kt * 128:(kt + 1) * 128],
               QTz[qt % 2][:, g * 4 + j, :], True, True,
               [KTB[kt], QTZ[qt % 2]], [PSB[2 * sp], PSB[2 * sp + 1]])

    UVD_C = Buf("uvd")
    pstg = [sb([128, D_MODEL], F32, f"pstg{i}") for i in range(2)]
    pstb = [sb([128, D_MODEL], BF16, f"pstb{i}") for i in range(2)]
    PSTG = [Buf(f"pstg{i}") for i in range(2)]
    PSTB = [Buf(f"pstb{i}") for i in range(2)]

    def prep_gen():
        steps = [(a, which, tab) for a in range(128) for which, tab in ((0, peer_u), (1, peer_v))]

        def load(n):
            a, which, tab = steps[n]
            z = n % 2
            dma("sp", pstg[z][:], tab[a * 128:(a + 1) * 128, :], f"pi{z}", [], [PSTG[z]])

        load(0)
        for n, (a, which, tab) in enumerate(steps):
            z = n % 2
            if n + 1 < len(steps):
                load(n + 1)
            vcopy("pool", pstb[z][:], pstg[z][:], [PSTG[z]], [PSTB[z]])
            dma("sp", uv_d[a * 128:(a + 1) * 128, which * D_MODEL:(which + 1) * D_MODEL], pstb[z][:],
                f"po{z}", [PSTB[z]], [UVD_C])
            yield

    pgen = prep_gen()
    for i in range(LOOK):
        issue_qk(i)
    for i, (qt, g, j, kp) in enumerate(its):
        if i + LOOK < len(its):
            issue_qk(i + LOOK)
        if i % 4 == 1 and pgen is not None:
            try:
                next(pgen)
            except StopIteration:
                pgen = None
        h = g * 4 + j
        obank = 4 + (h % 2)
        sp = i % 2
        slot = i % 3
        act(pT[slot][:], PSD[sp][:, :], AF.Exp, [PSB[2 * sp], PSB[2 * sp + 1]], [PT[slot]])
        for u in range(2):
            kt = kp * 2 + u
            mm(PS[obank][:, :], VA[:, kt, g, :], pT[slot][:, u * 512:(u + 1) * 512], kt == 0, kt == NT_ALL - 1,
               [VAB[kt], PT[slot]], [PSB[obank]])
        if kp != NKP - 1:
            continue
        acopy(den[:], PS[obank][64:128, :], [PSB[obank]], [DEN])
        recip(den[:], den[:], [DEN], [DEN])
        tt("dve", OTn[:, h, :], PS[obank][0:64, :], den[:], ALU.mult, [PSB[obank], DEN], [OTB[h]])
        if h != 7:
            continue
        for sub in range(4):
            ti = qt * 4 + sub
            s = ti % 2
            tok = ti * 128
            dma("sp", xr[s][:], x_own[tok:tok + 128, :], f"xr{s}", [], [XR[s]])
            for half in range(2):
                bank = 6 + half
                n0 = half * 512
                for h in range(8):
                    mm(PS[bank][:, :], OTn[:, h, sub * 128:(sub + 1) * 128], wob_a[:, h, n0:n0 + 512],
                       h == 0, False, [OTB[h], WOB], [PSB[bank]])
                for c in range(4):
                    mm(PS[bank][:, :], CT[:, c, tok:tok + 128], wob_c[:, c, n0:n0 + 512],
                       False, c == 3, [CTB[qt], WOB], [PSB[bank]])
                tt("dve", x2t[s][:, n0:n0 + 512], PS[bank][:, :], xr[s][:, n0:n0 + 512], ALU.add,
                   [PSB[bank], XR[s]], [X2T[s]])
            dma("sp", x2_d[tok:tok + 128, :], x2t[s][:], f"x2o{s}", [X2T[s]], [X2D[ti]])
    if pgen is not None:
        for _ in pgen:
            pass
    barrier()
    if debug:
        o = dout("d_x2", [NOWN, D_MODEL], F32)
        for ti in range(NT_OWN):
            s = ti % 2
            dma("sp", xr[s][:], x2_d[ti * 128:(ti + 1) * 128, :], f"xr{s}", [X2D[ti]], [XR[s]])
            dma("sp", o[ti * 128:(ti + 1) * 128, :], xr[s][:], f"x2o{s}", [XR[s]], [])
        barrier()
    if stop_after == "C":
        P.emit()
        return nc, dbg

    new_scope()
    pers.close()
    wqb = sb([128, 8, 2048], BF16, "wqb")
    WQB = Buf("wqb")
    keysT = sb([128, 2, 128], BF16, "keysT")
    g2bc = sb([128, D_MODEL], F32, "g2bc")
    gfbc = sb([128, D_MODEL], F32, "gfbc")
    x2s = [sb([128, D_MODEL], F32, f"x2s{i}") for i in range(2)]
    X2S = [Buf(f"x2s{i}") for i in range(2)]
    dma("sp", g2bc[:], norm2_g.partition_broadcast(128), "c0", [], [CONST])
    dma("sp", gfbc[:], final_g.partition_broadcast(128), "c0", [], [CONST])
    for hf in range(2):
        dma("sp", x2s[hf][:, 0:128], peer_keys[hf, :, :], f"x2s{hf}", [], [X2S[hf]])
        tr(PS[6][:, hf * 128:(hf + 1) * 128], x2s[hf][:, 0:128], ident_f[:], [X2S[hf], CONST], [PSB[6]])
    acopy(keysT[:], PS[6][:, 0:256].rearrange("p (a n) -> p a n", a=2), [PSB[6]], [CONST])
    for dc in range(8):
        for hf in range(2):
            s = (dc * 2 + hf) % 2
            dma("sp", x2s[s][:], peer_wq[dc * 128:(dc + 1) * 128, hf * 1024:(hf + 1) * 1024], f"x2s{s}",
                [], [X2S[s]])
            if hf == 0:
                vcopy("dve", wqb[:, dc, 0:1024], x2s[s][:], [X2S[s]], [WQB])
            else:
                acopy(wqb[:, dc, 1024:2048], x2s[s][:], [X2S[s]], [WQB])

    junk = sb([128, D_MODEL], BF16, "junkD")
    JUNK = Buf("junkD")
    st = sb([128, 8], F32, "stD")
    ST = Buf("stD")
    hn = sb([128, D_MODEL], F32, "hn")
    HN = Buf("hn")
    hnb = sb([128, D_MODEL], BF16, "hnb")
    HNB = Buf("hnb")
    hnT = sb([128, 8, 128], BF16, "hnT")
    HNT = Buf("hnT")
    qTs = sb([128, 16, 128], BF16, "qTs")
    QTS = Buf("qTs")
    S = sb([128, 16, 128], F32, "S")
    SB_ = Buf("S")
    wk = sb([128, 256], F32, "wk")
    tv = sb([128, 16, 16], F32, "tv")
    tiu = sb([128, 16, 16], U32, "tiu")
    tif = sb([128, 16, 16], F32, "tif")
    TK = Buf("topk1")
    cand = sb([128, 8, 16, 16], F32, "cand")
    CAND = Buf("cand")
    tops = sb([128, 8, 16], F32, "tops")
    posu = sb([128, 8, 16], U32, "posu")
    piu = sb([128, 8, 16], U32, "piu")
    pju = sb([128, 8, 16], U32, "pju")
    pif = sb([128, 8, 16], F32, "pif")
    pjf = sb([128, 8, 16], F32, "pjf")
    TK2 = Buf("topk2")
    oh = sb([128, 8, 16, 16], F32, "oh")
    OH = Buf("oh")
    i1s = sb([128, 8, 16], F32, "i1s")
    i2s = sb([128, 8, 16], F32, "i2s")
    ef = sb([128, 128], F32, "ef")
    gw = sb([128, 8, 16], F32, "gw")
    ssum = sb([128, 8], F32, "ssum")
    EG = Buf("eg")
    eTu = sb([128, 128], U32, "eTu")
    ET = Buf("eTu")
    gT = sb([128, 128], F32, "gT")
    GT = Buf("gT")
    hT = sb([128, 128], F32, "hT")
    HTD = Buf("hT")
    gl = sb([128, 128], F32, "gl")
    gl2 = sb([128, 128], F32, "gl2")
    ghT = sb([128, 128], F32, "ghT")
    GH = Buf("ghT")
    GHB = [Buf(f"ghb{i}") for i in range(4)]
    GHK = [Buf(f"ghk{i}") for i in range(4)]
    NSL = 16
    SUBB = 4
    NG = NSL // SUBB
    gsl = [sb([128, 2 * D_MODEL], BF16, f"gs{i}") for i in range(NSL)]
    GS = [Buf(f"gs{i}") for i in range(NSL)]
    ghb = sb([128, 128], BF16, "ghb")
    UVD = UVD_C
    oT = sb([128, 8, 128], F32, "oT")
    OT = Buf("oT")
    yb = sb([128, D_MODEL], F32, "yb")
    YBD = Buf("yb")
    yo = [sb([128, D_MODEL], F32, f"yo{i}") for i in range(2)]
    YO = [Buf(f"yo{i}") for i in range(2)]
    psO = PSD[2][:, :].rearrange("p (c t) -> p c t", c=8)
    gcnt = 0
    GELU_C = 2.0 * 0.7978845608028654
    n_tiles_d = NT_OWN if ntile_d is None else ntile_d
    hnb2 = [hnb, sb([128, D_MODEL], BF16, "hnb1")]
    hnT2 = [hnT, sb([128, 8, 128], BF16, "hnT1")]
    HNT2 = [HNT, Buf("hnT1")]
    ugT = [sb([128, 8, 128], BF16, f"ugT{i}") for i in range(2)]
    UGT = [Buf(f"ugT{i}") for i in range(2)]
    HNB2 = [HNB, Buf("hnb1")]
    eTu2 = [eTu, sb([128, 128], U32, "eTu1")]
    ET2 = [ET, Buf("eTu1")]
    gT2 = [gT, sb([128, 128], F32, "gT1")]
    GT2 = [GT, Buf("gT1")]
    st_e = sb([128, 8], F32, "st_e")
    STE = Buf("st_e")
    junk_e = sb([128, D_MODEL], BF16, "junk_e")
    JUNKE = Buf("junk_e")
    junk_d = sb([128, D_MODEL], BF16, "junk_d")
    HTDk = [Buf(f"hTk{i}") for i in range(4)]

    def top16(src_ap, n, tv_ap, ti_ap, R, W):
        P.op("dve", lambda h: h.max(out=tv_ap[:, 0:8], in_=src_ap), R, W)
        P.op("dve", lambda h: h.max_index(out=ti_ap[:, 0:8], in_max=tv_ap[:, 0:8], in_values=src_ap), R, W)
        P.op("dve", lambda h: h.match_replace(out=wk[:, 0:n], in_to_replace=tv_ap[:, 0:8], in_values=src_ap,
                                              imm_value=-1e30), R, W)
        P.op("dve", lambda h: h.max(out=tv_ap[:, 8:16], in_=wk[:, 0:n]), R, W)
        P.op("dve", lambda h: h.max_index(out=ti_ap[:, 8:16], in_max=tv_ap[:, 8:16], in_values=wk[:, 0:n]), R, W)

    def retrieval(ti):
        tok = ti * 128
        s = ti % 2
        hb, HB = hnb2[s], HNB2[s]
        hnT, HNT = hnT2[s], HNT2[s]
        dma("sp", x2s[s][:], x2_d[tok:tok + 128, :], f"x2s{s}", [X2D[ti]], [X2S[s]])
        P.op("dve", (lambda a, c: (lambda h: h.scalar_tensor_tensor(
            out=junk[:], in0=a, scalar=1.0, in1=a, op0=ALU.mult, op1=ALU.mult, accum_out=c)))(
            x2s[s][:], st[:, 0:1]), [X2S[s]], [JUNK, ST])
        act(st[:, 1:2], st[:, 0:1], AF.Ln, [ST], [ST], bias=epsc[:, 0:1], scale=1.0 / D_MODEL)
        act(st[:, 2:3], st[:, 1:2], AF.Exp, [ST], [ST], scale=-0.5)
        stt(hn[:], x2s[s][:], st[:, 2:3], g2bc[:], ALU.mult, ALU.mult, [X2S[s], ST, CONST], [HN])
        acopy(hb[:], hn[:], [HN], [HB])
        yield
        for dc in range(8):
            tr(psbf(7)[:, dc * 128:(dc + 1) * 128], hb[:, dc * 128:(dc + 1) * 128], ident_b[:],
               [HB, CONST], [PSB[7]])
        acopy(hnT[:], psbf(7).rearrange("p (c t) -> p c t", c=8), [PSB[7]], [HNT])
        yield
        for r in range(4):
            b = 6 + (r % 2)
            for a in range(4):
                jj = 4 * r + a
                for dc in range(8):
                    mm(PS[b][:, a * 128:(a + 1) * 128], wqb[:, dc, jj * 128:(jj + 1) * 128],
                       hnT[:, dc, :], dc == 0, dc == 7, [WQB, HNT], [PSB[b]])
            acopy(qTs[:, 4 * r:4 * r + 4, :], PS[b].rearrange("p (a t) -> p a t", a=4), [PSB[b]], [QTS])
            yield
        for r in range(4):
            b = 6 + (r % 2)
            for a in range(4):
                jj = 4 * r + a
                mm(PS[b][:, a * 128:(a + 1) * 128], qTs[:, jj, :], keysT[:, jj % 2, :], True, True,
                   [QTS, CONST], [PSB[b]])
            acopy(S[:, 4 * r:4 * r + 4, :], PS[b].rearrange("p (a t) -> p a t", a=4), [PSB[b]], [SB_])
            yield
        for jj in range(16):
            top16(S[:, jj, :], 128, tv[:, jj, :], tiu[:, jj, :], [SB_], [TK])
            yield
        vcopy("dve", tif[:], tiu[:], [TK], [TK])
        tvv = tv[:].rearrange("p (h a) k -> p h a k", a=2)
        tifv = tif[:].rearrange("p (h a) k -> p h a k", a=2)
        tt("dve", cand[:], tvv[:, :, 0, :].unsqueeze(3).to_broadcast([128, 8, 16, 16]),
           tvv[:, :, 1, :].unsqueeze(2).to_broadcast([128, 8, 16, 16]), ALU.add, [TK], [CAND])
        yield
        for hh in range(8):
            top16(cand[:, hh, :, :].rearrange("p a b -> p (a b)"), 256, tops[:, hh, :], posu[:, hh, :],
                  [CAND], [TK2])
            yield
        ts("dve", piu[:], posu[:], 4, None, ALU.logical_shift_right, None, [TK2], [TK2])
        ts("dve", pju[:], posu[:], 15, None, ALU.bitwise_and, None, [TK2], [TK2])
        vcopy("dve", pif[:], piu[:], [TK2], [TK2])
        vcopy("dve", pjf[:], pju[:], [TK2], [TK2])
        yield
        io16 = iota16[:].unsqueeze(1).unsqueeze(1).to_broadcast([128, 8, 16, 16])
        for (pf, col, dst) in ((pif, 0, i1s), (pjf, 1, i2s)):
            tt("dve", oh[:], io16, pf[:].unsqueeze(3).to_broadcast([128, 8, 16, 16]), ALU.is_equal,
               [TK2, CONST], [OH])
            tt("pool", oh[:], oh[:], tifv[:, :, col, :].unsqueeze(2).to_broadcast([128, 8, 16, 16]), ALU.mult,
               [OH, TK], [OH])
            reduce_add(dst[:], oh[:], [OH], [EG])
            yield
        stt(ef[:], i1s[:].rearrange("p h k -> p (h k)"), 128.0, i2s[:].rearrange("p h k -> p (h k)"),
            ALU.mult, ALU.add, [EG], [EG])
        tt("dve", gw[:], tops[:], tops[:, :, 0:1].to_broadcast([128, 8, 16]), ALU.subtract, [TK2], [EG])
        act(gw[:], gw[:], AF.Exp, [EG], [EG])
        yield
        reduce_add(ssum[:], gw[:], [EG], [EG])
        recip(ssum[:], ssum[:], [EG], [EG])
        tt("dve", gw[:], gw[:], ssum[:].unsqueeze(2).to_broadcast([128, 8, 16]), ALU.mult, [EG], [EG])
        tr(PS[7][:, 0:128], ef[:], ident_f[:], [EG, CONST], [PSB[7]])
        tr(PS[7][:, 128:256], gw[:].rearrange("p h k -> p (h k)"), ident_f[:], [EG, CONST], [PSB[7]])
        yield
        vcopy("dve", eTu2[s][:], PS[7][:, 0:128], [PSB[7]], [ET2[s]])
        acopy(gT2[s][:], PS[7][:, 128:256], [PSB[7]], [GT2[s]])
        if debug and ti == 0:
            o = dout("d_e", [128, 128], U32)
            dma("sp", o, eTu2[s][:], "dbg", [ET2[s]], [])
            o = dout("d_g", [128, 128], F32)
            dma("sp", o, gT2[s][:], "dbg", [GT2[s]], [])
        yield

    def drain(gen):
        if gen is None:
            return
        for _ in gen:
            pass

    def step(gen):
        if gen is None:
            return None
        try:
            next(gen)
            return gen
        except StopIteration:
            return None

    nsb = 128 // SUBB
    gen = retrieval(0)
    drain(gen)
    for ti in range(n_tiles_d):
        tok = ti * 128
        s = ti % 2
        hb, HB, eT_, ETB, gT_, GTB = hnb2[s], HNB2[s], eTu2[s], ET2[s], gT2[s], GT2[s]
        hTc, HTc = hnT2[s], HNT2[s]
        gen = retrieval(ti + 1) if ti + 1 < n_tiles_d else None

        pend = []

        def flushM():
            while pend:
                (t, u, hbk) = pend.pop(0)
                for dc in range(8):
                    mm(PS[hbk][:, t:t + 1], ugT[u][:, dc, :], hTc[:, dc, t:t + 1], dc == 0, dc == 7,
                       [UGT[u], HTc], [PSB[hbk]])

        def stageA(k):
            nonlocal gcnt
            hbk = 2 + (k % 2)
            for t in range(k * SUBB, (k + 1) * SUBB):
                sl = (k % NG) * SUBB + (t % SUBB)
                tb = gcnt % 2
                u = gcnt % 2
                gcnt += 1
                P.dma("pool", (lambda o, ia: (lambda h: h.indirect_dma_start(
                    out=o, out_offset=None, in_=uv_d[:, :],
                    in_offset=bass.IndirectOffsetOnAxis(ap=ia, axis=0))))(gsl[sl][:], eT_[:, t:t + 1]),
                    f"g{sl}", [ETB, UVD], [GS[sl]])
                for dc in range(8):
                    tr(psbf(tb)[:, dc * 128:(dc + 1) * 128], gsl[sl][:, dc * 128:(dc + 1) * 128], ident_b[:],
                       [GS[sl], CONST], [PSB[tb]])
                acopy(ugT[u][:].rearrange("p c h -> p (c h)"), psbf(tb), [PSB[tb]], [UGT[u]])
                flushM()
                pend.append((t, u, hbk))

        def stageG1(k):
            flushM()
            c0, c1 = k * SUBB, (k + 1) * SUBB
            HK = HTDk[k % 4]
            G = GHK[k % 4]
            hbk = 2 + (k % 2)
            acopy(hT[:, c0:c1], PS[hbk][:, c0:c1], [PSB[hbk]], [HK])
            tt("dve", gl[:, c0:c1], hT[:, c0:c1], hT[:, c0:c1], ALU.mult, [HK], [G])
            ts("dve", gl[:, c0:c1], gl[:, c0:c1], 0.044715, 1.0, ALU.mult, ALU.add, [G], [G])
            tt("dve", gl[:, c0:c1], gl[:, c0:c1], hT[:, c0:c1], ALU.mult, [G, HK], [G])
            act(gl2[:, c0:c1], gl[:, c0:c1], AF.Exp, [G], [G], scale=-GELU_C)

        def stageG2(k):
            c0, c1 = k * SUBB, (k + 1) * SUBB
            HK = HTDk[k % 4]
            G = GHK[k % 4]
            ts("dve", gl2[:, c0:c1], gl2[:, c0:c1], 1.0, None, ALU.add, None, [G], [G])
            recip(gl2[:, c0:c1], gl2[:, c0:c1], [G], [G])
            tt("dve", gl2[:, c0:c1], gl2[:, c0:c1], hT[:, c0:c1], ALU.mult, [G, HK], [G])
            tt("dve", ghb[:, c0:c1], gl2[:, c0:c1], gT_[:, c0:c1], ALU.mult, [G, GTB], [GHB[k % 4]])

        def stageS(k):
            for t in range(k * SUBB, (k + 1) * SUBB):
                sl = (k % NG) * SUBB + (t % SUBB)
                for c in range(8):
                    mm(psO[:, c, t:t + 1], gsl[sl][:, D_MODEL + c * 128:D_MODEL + (c + 1) * 128], ghb[:, t:t + 1],
                       True, True, [GS[sl], GHB[k % 4]], [PSB[4], PSB[5]])

        for k in range(nsb + 3):
            if 0 <= k - 3 < nsb:
                stageS(k - 3)
            if k < nsb:
                stageA(k)
            if 0 <= k - 1 < nsb:
                stageG1(k - 1)
            if 0 <= k - 2 < nsb:
                stageG2(k - 2)
            gen = step(gen)
            gen = step(gen)
        drain(gen)
        acopy(oT[:], psO, [PSB[4], PSB[5]], [OT])
        for c in range(8):
            tr(PSD[3][:, c * 128:(c + 1) * 128], oT[:, c, :], ident_f[:], [OT, CONST], [PSB[6], PSB[7]])
        tt("dve", yb[:], PSD[3][:, :], x2s[s][:], ALU.add, [PSB[6], PSB[7], X2S[s]], [YBD])
        P.op("dve", (lambda a, c: (lambda h: h.scalar_tensor_tensor(
            out=junk_e[:], in0=a, scalar=1.0, in1=a, op0=ALU.mult, op1=ALU.mult, accum_out=c)))(
            yb[:], st_e[:, 4:5]), [YBD], [JUNKE, STE])
        act(st_e[:, 5:6], st_e[:, 4:5], AF.Ln, [STE], [STE], bias=epsc[:, 0:1], scale=1.0 / D_MODEL)
        act(st_e[:, 6:7], st_e[:, 5:6], AF.Exp, [STE], [STE], scale=-0.5)
        stt(yo[s][:], yb[:], st_e[:, 6:7], gfbc[:], ALU.mult, ALU.mult, [YBD, STE, CONST], [YO[s]])
        dma("sp", out_d[tok:tok + 128, :], yo[s][:], f"yo{s}", [YO[s]], [])
    barrier()
    P.emit()
    return nc, dbg


def make_in_maps(inputs):
    x = np.ascontiguousarray(np.asarray(inputs["x"], dtype=np.float32))
    shared = {}
    for k in ("norm1_g", "w_in", "q_norm_g", "k_norm_g", "conv_b", "conv_ln_g", "conv_ln_b", "w_out",
              "norm2_g", "peer_wq", "peer_keys", "peer_u", "peer_v", "final_g"):
        shared[k] = np.ascontiguousarray(np.asarray(inputs[k], dtype=np.float32))
    shared["conv_dw"] = np.ascontiguousarray(np.asarray(inputs["conv_dw"], dtype=np.float32).reshape(CONV_W, 512))
    maps = []
    for c in range(8):
        b, hf = c // 2, c % 2
        own0 = hf * NOWN
        oth0 = (1 - hf) * NOWN
        m = dict(shared)
        m["x_own"] = x[b, own0:own0 + NOWN]
        m["x_oth"] = x[b, oth0:oth0 + NOWN]
        halo = np.zeros((32, D_MODEL), np.float32)
        if hf == 1:
            halo[0:15] = x[b, own0 - 15:own0]
        else:
            halo[16:31] = x[b, own0 + NOWN:own0 + NOWN + 15]
        m["x_halo"] = halo
        pos = np.concatenate([np.arange(own0, own0 + NOWN), np.arange(oth0, oth0 + NOWN)])
        pos = pos.reshape(NT_ALL, 128).T
        rc = np.stack([pos // 64, pos % 64], axis=-1).astype(np.float32)
        m["rowcol"] = np.ascontiguousarray(rc)
        maps.append(m)
    return maps


_NC_CACHE = {}


def kernel(**inputs):
    if "nc" not in _NC_CACHE:
        _NC_CACHE["nc"] = build_program("D", False)[0]
    nc = _NC_CACHE["nc"]
    maps = make_in_maps(inputs)
    res = run_bass_kernel_spmd(nc, maps, core_ids=list(range(8)))
    out = np.empty((4, SEQ, D_MODEL), np.float32)
    for c in range(8):
        b, hf = c // 2, c % 2
        out[b, hf * NOWN:(hf + 1) * NOWN] = res.results[c]["out"]
    return out
```

```python
import bisect
from contextlib import ExitStack
import numpy as np
import concourse.bass as bass
import concourse.mybir as mybir
from concourse.bass_utils import run_bass_kernel_spmd

F32 = mybir.dt.float32
F32R = mybir.dt.float32r
BF16 = mybir.dt.bfloat16
U32 = mybir.dt.uint32
I32 = mybir.dt.int32
ALU = mybir.AluOpType
AF = mybir.ActivationFunctionType
AX = mybir.AxisListType

SEM_LIMIT = 30000


class _Cut(Exception):
    pass


class Buf:
    def __init__(self, name, excl=False):
        self.name = name
        self.last_w = None
        self.readers = []
        self.excl = excl


class DSem:
    def __init__(self, prog, name):
        self.prog = prog
        self.name = name
        self.sem = prog.nc.alloc_semaphore(name=name)
        self.total = 0
        self.group_ends = []
        self.open = False

    def need(self, v):
        i = bisect.bisect_left(self.group_ends, v)
        if i < len(self.group_ends):
            return self.group_ends[i]
        self.group_ends.append(self.total)
        self.open = False
        return self.total


class Prog:
    ENG = ("pe", "dve", "act", "pool", "sp")

    def __init__(self, nc):
        self.nc = nc
        self.handles = {"pe": nc.tensor, "dve": nc.vector, "act": nc.scalar,
                        "pool": nc.gpsimd, "sp": nc.sync}
        self.lists = {e: [] for e in self.ENG}
        self.cnt = {e: 0 for e in self.ENG}
        self.gen = {e: 0 for e in self.ENG}
        self.sems = {e: [nc.alloc_semaphore(name=f"s_{e}_0")] for e in self.ENG}
        self.waited = {}
        self.same_engine_sync = {"pe": False, "dve": True, "act": True,
                                 "pool": True, "sp": False}
        self.dsems = {}
        self.old_dsems = []
        self.n_inst = 0

    def _wait(self, eng, ev):
        kind, key, val = ev
        if kind == "e":
            e2, g = key
            if e2 == eng and not self.same_engine_sync[eng]:
                return
            sem = self.sems[e2][g]
            wkey = (eng, "e", e2, g)
            need = val
        else:
            ds = key
            need = ds.need(val)
            sem = ds.sem
            wkey = (eng, "d", ds.name)
        if self.waited.get(wkey, 0) >= need:
            return
        self.waited[wkey] = need
        self.lists[eng].append(("wait", sem, need))

    def _deps(self, eng, reads, writes):
        evs = []
        for b in reads:
            if b.last_w is not None:
                evs.append(b.last_w)
            if b.excl:
                for ev in b.readers:
                    if ev[0] == "e" and ev[1][0] != eng:
                        evs.append(ev)
        for b in writes:
            if b.last_w is not None:
                evs.append(b.last_w)
            evs.extend(b.readers)
        best = {}
        for ev in evs:
            k = (ev[0], ev[1] if ev[0] == "e" else id(ev[1]))
            if k not in best or ev[2] > best[k][2]:
                best[k] = ev
        for ev in best.values():
            self._wait(eng, ev)

    def _commit(self, ev, reads, writes):
        for b in writes:
            b.last_w = ev
            b.readers = []
        for b in reads:
            if b not in writes:
                b.readers.append(ev)
                if len(b.readers) > 64:
                    b.readers = b.readers[-64:]

    def op(self, eng, fn, reads=(), writes=()):
        reads = list(reads)
        writes = list(writes)
        self._deps(eng, reads, writes)
        if self.cnt[eng] >= SEM_LIMIT:
            self.gen[eng] += 1
            self.cnt[eng] = 0
            self.sems[eng].append(self.nc.alloc_semaphore(name=f"s_{eng}_{self.gen[eng]}"))
        self.cnt[eng] += 1
        g = self.gen[eng]
        self.lists[eng].append(("op", fn, self.sems[eng][g]))
        ev = ("e", (eng, g), self.cnt[eng])
        self._commit(ev, reads, writes)
        self.n_inst += 1
        return ev

    def dsem(self, name):
        if name not in self.dsems:
            self.dsems[name] = DSem(self, "d_" + name)
        return self.dsems[name]

    def dma(self, queue, fn, dsem, reads=(), writes=()):
        ds = self.dsem(dsem) if isinstance(dsem, str) else dsem
        if ds.total >= SEM_LIMIT and not ds.open and isinstance(dsem, str):
            self._wait(queue, ("d", ds, ds.total))
            self._dgen = getattr(self, "_dgen", 0) + 1
            ds = DSem(self, f"d_{dsem}_{self._dgen}")
            self.dsems[dsem] = ds
            self.old_dsems.append(ds)
        reads = list(reads)
        writes = list(writes)
        self._deps(queue, reads, writes)
        if (not ds.open) and ds.total > 0:
            self._wait(queue, ("d", ds, ds.total))
        ds.total += 16
        ds.open = True
        self.lists[queue].append(("dma", fn, ds.sem))
        ev = ("d", ds, ds.total)
        self._commit(ev, reads, writes)
        self.n_inst += 1
        return ev

    def wait_all(self, eng, bufs):
        for b in bufs:
            if b.last_w is not None:
                self._wait(eng, b.last_w)

    def emit(self):
        nc = self.nc
        with nc.Block() as block:
            def mk(ename):
                items = self.lists[ename]

                def body(h):
                    for it in items:
                        if it[0] == "wait":
                            h.wait_ge(it[1], it[2])
                        elif it[0] == "op":
                            it[1](h).then_inc(it[2], 1)
                        else:
                            it[1](h).then_inc(it[2], 16)
                return body
            block.tensor(mk("pe"))
            block.vector(mk("dve"))
            block.scalar(mk("act"))
            block.gpsimd(mk("pool"))
            block.sync(mk("sp"))


D_MODEL = 1024
SEQ = 8192
NOWN = 4096
HD = 64
CONV_W = 31
IN_W = 1792
EPS = 1e-6
NT_OWN = NOWN // 128
NT_ALL = SEQ // 128
TWO_PI = 2.0 * np.pi
NSWDGE = 1


def build_program(stop_after="D", debug=False, grp_list=None, pool_eng="pool", cut=None, ntile_d=None):
    nc = bass.Bass("TRN2", target_bir_lowering=False, num_swdge_queues=NSWDGE)
    P = Prog(nc)
    dbg = {}

    def din(name, shape, dt=F32):
        return nc.dram_tensor(name, list(shape), dt, kind="ExternalInput").ap()

    x_own = din("x_own", [NOWN, D_MODEL])
    x_oth = din("x_oth", [NOWN, D_MODEL])
    x_halo = din("x_halo", [32, D_MODEL])
    rowcol = din("rowcol", [128, NT_ALL, 2])
    norm1_g = din("norm1_g", [D_MODEL])
    w_in = din("w_in", [D_MODEL, IN_W])
    q_norm_g = din("q_norm_g", [HD])
    k_norm_g = din("k_norm_g", [HD])
    conv_dw = din("conv_dw", [CONV_W, 512])
    conv_b = din("conv_b", [512])
    conv_ln_g = din("conv_ln_g", [512])
    conv_ln_b = din("conv_ln_b", [512])
    w_out = din("w_out", [D_MODEL, D_MODEL])
    norm2_g = din("norm2_g", [D_MODEL])
    peer_wq = din("peer_wq", [D_MODEL, 2048])
    peer_keys = din("peer_keys", [2, 128, 128])
    peer_u = din("peer_u", [16384, D_MODEL])
    peer_v = din("peer_v", [16384, D_MODEL])
    final_g = din("final_g", [D_MODEL])
    out_d = nc.dram_tensor("out", [NOWN, D_MODEL], F32, kind="ExternalOutput").ap()
    x2_d = nc.dram_tensor("x2_scratch", [NOWN, D_MODEL], F32, kind="Internal").ap()
    uv_d = nc.dram_tensor("uv_scratch", [16384, 2 * D_MODEL], BF16, kind="Internal").ap()

    def dout(name, shape, dt=F32):
        dbg[name] = nc.dram_tensor(name, list(shape), dt, kind="ExternalOutput").ap()
        return dbg[name]

    _n = [0]
    cst = ExitStack()
    pers = ExitStack()
    scope = [ExitStack()]

    def sb(shape, dt=F32, name=None, persistent=False):
        _n[0] += 1
        nm = name or f"sb{_n[0]}"
        if persistent == "c":
            return cst.enter_context(nc.sbuf_tensor(nm, list(shape), dt, side="right"))
        if persistent:
            return pers.enter_context(nc.sbuf_tensor(nm, list(shape), dt, side="right"))
        return scope[0].enter_context(nc.sbuf_tensor(nm, list(shape), dt, side="left"))

    def new_scope():
        barrier()
        scope[0].close()
        scope[0] = ExitStack()

    def mm(out, lhsT, rhs, start, stop, R, W):
        P.op("pe", lambda h: h.matmul(out, lhsT=lhsT, rhs=rhs, start=start, stop=stop), R, W)

    def tr(out, in_, ident, R, W):
        P.op("pe", lambda h: h.transpose(out=out, in_=in_, identity=ident), R, W)

    def act(out, in_, func, R, W, bias=None, scale=None, accum=None):
        kw = {}
        if bias is not None:
            kw["bias"] = bias
        if scale is not None:
            kw["scale"] = scale
        if accum is not None:
            kw["accum_out"] = accum
        P.op("act", lambda h: h.activation(out=out, in_=in_, func=func, **kw), R, W)

    def acopy(out, in_, R, W):
        P.op("act", lambda h: h.copy(out=out, in_=in_), R, W)

    def tt(eng, out, in0, in1, op, R, W):
        P.op(eng, lambda h: h.tensor_tensor(out=out, in0=in0, in1=in1, op=op), R, W)

    def ts(eng, out, in0, s1, s2, op0, op1, R, W):
        if op1 is None:
            P.op(eng, lambda h: h.tensor_scalar(out=out, in0=in0, scalar1=s1, scalar2=None, op0=op0), R, W)
        else:
            P.op(eng, lambda h: h.tensor_scalar(out=out, in0=in0, scalar1=s1, scalar2=s2, op0=op0, op1=op1), R, W)

    def stt(out, in0, scalar, in1, op0, op1, R, W):
        P.op("dve", lambda h: h.scalar_tensor_tensor(out=out, in0=in0, scalar=scalar, in1=in1, op0=op0, op1=op1), R, W)

    def vcopy(eng, out, in_, R, W):
        P.op(eng, lambda h: h.tensor_copy(out=out, in_=in_), R, W)

    def recip(out, in_, R, W):
        P.op("dve", lambda h: h.reciprocal(out=out, in_=in_), R, W)

    def reduce_add(out, in_, R, W):
        P.op("dve", lambda h: h.tensor_reduce(out=out, in_=in_, axis=AX.X, op=ALU.add), R, W)

    def memset(eng, ap, val, W):
        P.op(eng, lambda h: h.memset(ap, val), [], W)

    def dma(q, out, in_, ds, R, W, slow=False):
        if slow:
            P.dma(q, lambda h: h.dma_start(out=out, in_=in_, allow_slow_non_contiguous=True), ds, R, W)
        else:
            P.dma(q, lambda h: h.dma_start(out=out, in_=in_), ds, R, W)

    def barrier():
        evs = []
        for e in P.ENG:
            if P.cnt[e] > 0:
                evs.append(("e", (e, P.gen[e]), P.cnt[e]))
        for ds in list(P.dsems.values()):
            if ds.total > 0:
                evs.append(("d", ds, ds.total))
        for e in P.ENG:
            for ev in evs:
                if ev[0] == "e" and ev[1][0] == e:
                    continue
                P._wait(e, ev)

    PSD = [nc.alloc_psum_tensor(f"pd{i}", [128, 1024], F32) for i in range(4)]
    PS = [PSD[i // 2][:, (i % 2) * 512:(i % 2 + 1) * 512] for i in range(8)]
    PSB = [Buf(f"bank{i}", excl=True) for i in range(8)]

    def psbf(i):
        return PS[i].bitcast(BF16)

    CONST = Buf("const")
    ident_f = sb([128, 128], F32, "ident_f", persistent="c")
    ident_b = sb([128, 128], BF16, "ident_b", persistent="c")
    iot = sb([128, 128], F32, "iot", persistent="c")
    P.op("pool", lambda h: h.iota(iot[:], pattern=[[1, 128]], base=0, channel_multiplier=-1,
                                  allow_small_or_imprecise_dtypes=True), [], [CONST])
    ts("dve", ident_f[:], iot[:], 0.0, None, ALU.is_equal, None, [CONST], [CONST])
    vcopy("dve", ident_b[:], ident_f[:], [CONST], [CONST])
    ones_f = sb([128, 128], F32, "ones_f", persistent="c")
    memset("dve", ones_f[:], 1.0 / 512.0, [CONST])
    iota16 = sb([128, 16], F32, "iota16", persistent="c")
    epsc = sb([128, 1], F32, "epsc", persistent="c")
    memset("dve", epsc[:], EPS, [CONST])
    P.op("pool", lambda h: h.iota(iota16[:], pattern=[[1, 16]], base=0, channel_multiplier=0,
                                  allow_small_or_imprecise_dtypes=True), [], [CONST])

    g1 = sb([128, 8], F32, "g1", persistent="c")
    dma("sp", g1[:], norm1_g.rearrange("(c p) -> p c", p=128), "c0", [], [CONST], slow=True)
    cb = sb([128, 4], F32, "cb", persistent="c")
    lg = sb([128, 4], F32, "lg", persistent="c")
    lb = sb([128, 4], F32, "lb", persistent="c")
    dma("sp", cb[:], conv_b.rearrange("(c p) -> p c", p=128), "c0", [], [CONST], slow=True)
    dma("sp", lg[:], conv_ln_g.rearrange("(c p) -> p c", p=128), "c0", [], [CONST], slow=True)
    dma("sp", lb[:], conv_ln_b.rearrange("(c p) -> p c", p=128), "c0", [], [CONST], slow=True)
    G10 = sb([128, 10, 64], F32, "G10", persistent="c")
    dma("sp", G10[:, 0, :], q_norm_g.partition_broadcast(128), "c0", [], [CONST])
    dma("sp", G10[:, 8, :], k_norm_g.partition_broadcast(128), "c0", [], [CONST])
    ts("dve", G10[:, 0, :], G10[:, 0, :], HD ** -0.5, None, ALU.mult, None, [CONST], [CONST])
    for j in range(1, 8):
        vcopy("dve", G10[:, j, :], G10[:, 0, :], [CONST], [CONST])
    vcopy("dve", G10[:, 9, :], G10[:, 8, :], [CONST], [CONST])
    KT = sb([128, SEQ], BF16, "KT", persistent=True)
    VA = sb([128, NT_ALL, 2, 128], BF16, "VA", persistent=True)
    QT = sb([128, 4, NOWN], BF16, "QT", persistent=True)
    htstack = ExitStack()
    HT = htstack.enter_context(nc.sbuf_tensor("HT", [128, 4, NOWN + 32], BF16, side="left"))

    rc_t = sb([128, NT_ALL, 2], F32, "rc_t")
    dma("sp", rc_t[:], rowcol, "c0", [], [CONST])
    invf = sb([128, 16], F32, "invf")
    act(invf[:], iota16[:], AF.Exp, [CONST], [CONST], scale=-float(np.log(10000.0)) / 16.0)
    NTAB = NT_ALL * 32
    cos_t = sb([128, NT_ALL, 2, 16], F32, "cos_t")
    sin_t = sb([128, NT_ALL, 2, 16], F32, "sin_t")
    w1b = sb([128, 8, IN_W], BF16, "w1b")
    W1B = Buf("w1b")
    with nc.sbuf_tensor("rr_k", [128, NTAB], I32, side="left") as rr_k, \
            nc.sbuf_tensor("rr_f", [128, NTAB], F32, side="left") as rr_f, \
            nc.sbuf_tensor("rr_a", [128, NTAB], F32, side="left") as rr_a, \
            nc.sbuf_tensor("ang", [128, NT_ALL, 2, 16], F32, side="left") as ang:
        tt("dve", ang[:], rc_t[:].unsqueeze(3).to_broadcast([128, NT_ALL, 2, 16]),
           invf[:].unsqueeze(1).unsqueeze(1).to_broadcast([128, NT_ALL, 2, 16]), ALU.mult, [CONST], [CONST])
        angf = ang[:].rearrange("p a b c -> p (a b c)")
        TMPB = Buf("ropetmp")
        for tab, shift in ((sin_t, 0.0), (cos_t, np.pi / 2)):
            tf = tab[:].rearrange("p a b c -> p (a b c)")
            ts("dve", rr_a[:], angf, float(shift), None, ALU.add, None, [CONST], [TMPB])
            ts("dve", rr_k[:], rr_a[:], 1.0 / TWO_PI, 0.5, ALU.mult, ALU.add, [TMPB], [TMPB])
            vcopy("dve", rr_f[:], rr_k[:], [TMPB], [TMPB])
            stt(rr_f[:], rr_f[:], -TWO_PI, rr_a[:], ALU.mult, ALU.add, [TMPB], [TMPB])
            ts("dve", rr_a[:], rr_f[:], -float(np.pi), TWO_PI, ALU.is_lt, ALU.mult, [TMPB], [TMPB])
            tt("dve", rr_f[:], rr_f[:], rr_a[:], ALU.add, [TMPB], [TMPB])
            ts("dve", rr_a[:], rr_f[:], float(np.pi), TWO_PI, ALU.is_gt, ALU.mult, [TMPB], [TMPB])
            tt("dve", rr_a[:], rr_f[:], rr_a[:], ALU.subtract, [TMPB], [TMPB])
            ts("dve", rr_a[:], rr_a[:], float(np.pi), -float(np.pi), ALU.min, ALU.max, [TMPB], [TMPB])
            act(tf, rr_a[:], AF.Sin, [TMPB], [CONST])
        barrier()
    with nc.sbuf_tensor("stg0", [128, IN_W], F32, side="left") as stg0, \
            nc.sbuf_tensor("stg1", [128, IN_W], F32, side="left") as stg1:
        stg = [stg0, stg1]
        STG = [Buf("stg0"), Buf("stg1")]
        for dc in range(8):
            s = dc % 2
            dma("sp", stg[s][:], w_in[dc * 128:(dc + 1) * 128, :], f"stg{s}", [], [STG[s]])
            ts("dve", w1b[:, dc, 0:512].rearrange("p (j g d) -> p j g d", j=4, g=2),
               stg[s][:, 0:512].rearrange("p (g j d) -> p j g d", g=2, j=4),
               g1[:, dc:dc + 1], None, ALU.mult, None, [STG[s], CONST], [W1B])
            ts("pool", w1b[:, dc, 512:IN_W], stg[s][:, 512:IN_W], g1[:, dc:dc + 1], None, ALU.mult, None,
               [STG[s], CONST], [W1B])
        barrier()

    if stop_after == "0":
        o = dout("d_cos", [128, NT_ALL, 2, 16], F32)
        dma("sp", o, cos_t[:], "dbg", [], [])
        o = dout("d_sin", [128, NT_ALL, 2, 16], F32)
        dma("sp", o, sin_t[:], "dbg", [], [])
        o = dout("d_w1b", [128, 8, IN_W], BF16)
        dma("sp", o, w1b[:], "dbg", [], [])
        o = dout("d_G10", [128, 10, 64], F32)
        dma("sp", o, G10[:], "dbg", [], [])
        barrier()
        P.emit()
        return nc, dbg
    KTB = [Buf(f"kt{i}") for i in range(NT_ALL)]
    VAB = [Buf(f"va{i}") for i in range(NT_ALL)]
    QTB = [Buf(f"qt{i}") for i in range(NT_OWN)]
    HTB = [Buf(f"ht{i}") for i in range(NT_OWN // 4 + 1)]
    VINIT = Buf("vinit")
    memset("pool", VA[:, :, :, 64:128].rearrange("p a b c -> p (a b) c"), 1.0, [VINIT])
    for b in VAB:
        b.last_w = VINIT.last_w
    memset("pool", HT[:, :, NOWN + 30:NOWN + 32], 0.0, [HTB[NT_OWN // 4]])

    xin = [sb([128, D_MODEL], F32, f"xin{i}") for i in range(2)]
    XIN = [Buf(f"xin{i}") for i in range(2)]
    junk = sb([128, D_MODEL], BF16, "junk")
    JUNK = Buf("junk")
    xs = [sb([128, D_MODEL], BF16, f"xs{i}") for i in range(2)]
    XS = [Buf(f"xs{i}") for i in range(2)]
    xT = [sb([128, 8, 512], BF16, f"xT{i}") for i in range(2)]
    XT = [Buf(f"xT{i}") for i in range(2)]
    st = sb([128, 8], F32, "st")
    ST = Buf("st")
    sq = sb([128, 640], F32, "sq")
    SQ = Buf("sq")
    ssq = sb([128, 10], F32, "ssq")
    qn = sb([128, 10, 64], F32, "qn")
    QN = Buf("qn")
    t1 = sb([128, 10, 64], F32, "t1")
    t2 = sb([128, 10, 64], F32, "t2")
    T12 = Buf("t12")
    qr = sb([128, 640], BF16, "qr")
    QR = Buf("qr")
    sg = sb([128, 512], F32, "sg")
    SG = Buf("sg")

    def rms_rows(xt_ap, XB, np_, out_bf, OB):
        act(junk[0:np_, :], xt_ap, AF.Square, [XB], [JUNK, ST], accum=st[0:np_, 0:1])
        act(st[0:np_, 1:2], st[0:np_, 0:1], AF.Sqrt, [ST], [ST], bias=EPS, scale=1.0 / D_MODEL)
        recip(st[0:np_, 2:3], st[0:np_, 1:2], [ST], [ST])
        ts("dve", out_bf, xt_ap, st[0:np_, 2:3], None, ALU.mult, None, [XB, ST], [OB])

    try:
        n_groups = NT_ALL // 4
        tile_ctr = 0
        for grp in (grp_list if grp_list is not None else range(n_groups + 1)):
            halo = grp == n_groups
            own = grp < NT_OWN // 4
            gs = grp % 2
            nsub = 1 if halo else 4
            for sub in range(nsub):
                ti = grp * 4 + sub
                s = tile_ctr % 2
                tile_ctr += 1
                np_ = 32 if halo else 128
                if halo:
                    src = x_halo[:, :]
                elif own:
                    src = x_own[ti * 128:(ti + 1) * 128, :]
                else:
                    src = x_oth[(ti - NT_OWN) * 128:(ti - NT_OWN + 1) * 128, :]
                dma("sp", xin[s][0:np_, :], src, f"xin{s}", [], [XIN[s]])
                rms_rows(xin[s][0:np_, :], XIN[s], np_, xs[s][0:np_, :], XS[s])
                if cut == 1:
                    raise _Cut()
                pb = ti % 2
                for dc in range(8):
                    tr(psbf(pb)[:, dc * 128:dc * 128 + np_], xs[s][0:np_, dc * 128:(dc + 1) * 128],
                       ident_b[0:np_, 0:np_], [XS[s], CONST], [PSB[pb]])
                acopy(xT[gs][:, :, sub * 128:sub * 128 + np_],
                      psbf(pb).rearrange("p (c t) -> p c t", c=8)[:, :, 0:np_], [PSB[pb]], [XT[gs]])
                if cut == 2:
                    raise _Cut()
                if halo:
                    continue
                c0 = 0 if own else 512
                ncol = 768 - c0
                for (a, b) in (((0, 512), (512, 768)) if own else ((512, 768),)):
                    bank = 2 if a == 0 else 3
                    for dc in range(8):
                        mm(PS[bank][:, 0:b - a], xT[gs][:, dc, sub * 128:(sub + 1) * 128], w1b[:, dc, a:b],
                           dc == 0, dc == 7, [XT[gs], W1B], [PSB[bank]])
                if cut == 3:
                    raise _Cut()
                h0 = 0 if own else 8
                nh = 10 - h0
                if own:
                    act(sq[:, 0:512], PS[2][:, 0:512], AF.Square, [PSB[2]], [SQ])
                act(sq[:, 512:640], PS[3][:, 0:128], AF.Square, [PSB[3]], [SQ])
                reduce_add(ssq[:, h0:10], sq[:, h0 * 64:640].rearrange("p (h d) -> p h d", d=64), [SQ], [ST])
                act(ssq[:, h0:10], ssq[:, h0:10], AF.Sqrt, [ST], [ST], bias=EPS, scale=1.0 / HD)
                recip(ssq[:, h0:10], ssq[:, h0:10], [ST], [ST])
                if own:
                    tt("dve", qn[:, 0:8, :], PS[2][:, 0:512].rearrange("p (h d) -> p h d", d=64),
                       ssq[:, 0:8].unsqueeze(2).to_broadcast([128, 8, 64]), ALU.mult, [PSB[2], ST], [QN])
                tt("dve", qn[:, 8:10, :], PS[3][:, 0:128].rearrange("p (h d) -> p h d", d=64),
                   ssq[:, 8:10].unsqueeze(2).to_broadcast([128, 2, 64]), ALU.mult, [PSB[3], ST], [QN])
                if cut == 4:
                    raise _Cut()
                acopy(VA[:, ti, :, 0:64], PS[3][:, 128:256].rearrange("p (g d) -> p g d", g=2), [PSB[3]], [VAB[ti]])
                tt(pool_eng, qn[:, h0:10, :], qn[:, h0:10, :], G10[:, h0:10, :], ALU.mult, [QN, CONST], [QN])
                if cut == 5:
                    raise _Cut()
                for rc in range(2):
                    qv = qn[:, h0:10, rc * 32:(rc + 1) * 32].rearrange("p h (a d) -> p h a d", a=2)
                    cosb = cos_t[:, ti, rc, :].unsqueeze(1).unsqueeze(1).to_broadcast([128, nh, 2, 16])
                    sinb = sin_t[:, ti, rc, :].unsqueeze(1).to_broadcast([128, nh, 16])
                    t1v = t1[:, h0:10, rc * 32:(rc + 1) * 32].rearrange("p h (a d) -> p h a d", a=2)
                    t2v = t2[:, h0:10, rc * 32:(rc + 1) * 32].rearrange("p h (a d) -> p h a d", a=2)
                    tt("dve", t1v, qv, cosb, ALU.mult, [QN, CONST], [T12])
                    tt(pool_eng, t2v[:, :, 0, :], qv[:, :, 1, :], sinb, ALU.mult, [QN, CONST], [T12])
                    tt(pool_eng, t2v[:, :, 1, :], qv[:, :, 0, :], sinb, ALU.mult, [QN, CONST], [T12])
                    qrv = qr[:, h0 * 64:640].rearrange("p (h c) -> p h c", c=64)[:, :, rc * 32:(rc + 1) * 32] \
                        .rearrange("p h (a d) -> p h a d", a=2)
                    tt("dve", qrv[:, :, 0, :], t1v[:, :, 0, :], t2v[:, :, 0, :], ALU.subtract, [T12], [QR])
                    tt("dve", qrv[:, :, 1, :], t1v[:, :, 1, :], t2v[:, :, 1, :], ALU.add, [T12], [QR])
                    if cut == 6:
                        raise _Cut()
                if own:
                    for j in range(4):
                        tr(psbf(4)[:, j * 128:(j + 1) * 128], qr[:, j * 128:(j + 1) * 128], ident_b[:],
                           [QR, CONST], [PSB[4]])
                tr(psbf(4)[:, 512:640], qr[:, 512:640], ident_b[:], [QR, CONST], [PSB[4]])
                if cut == 71:
                    raise _Cut()
                if own:
                    acopy(QT[:, :, ti * 128:(ti + 1) * 128], psbf(4)[:, 0:512].rearrange("p (j t) -> p j t", j=4),
                          [PSB[4]], [QTB[ti]])
                if cut == 72:
                    raise _Cut()
                acopy(KT[:, ti * 128:(ti + 1) * 128], psbf(4)[:, 512:640], [PSB[4]], [KTB[ti]])
                if cut == 7:
                    raise _Cut()
            if own or halo:
                ntok = 32 if halo else 512
                for c in range(4):
                    for part, bank in ((0, 5), (1, 6)):
                        col = 768 + part * 512 + c * 128
                        for dc in range(8):
                            mm(PS[bank][:, 0:ntok], w1b[:, dc, col:col + 128], xT[gs][:, dc, 0:ntok],
                               dc == 0, dc == 7, [XT[gs], W1B], [PSB[bank]])
                    act(sg[:, 0:ntok], PS[6][:, 0:ntok], AF.Sigmoid, [PSB[6]], [SG])
                    if halo:
                        tt("dve", HT[:, c, 0:15], PS[5][:, 0:15], sg[:, 0:15], ALU.mult, [PSB[5], SG], [HTB[0]])
                        tt("dve", HT[:, c, NOWN + 15:NOWN + 30], PS[5][:, 16:31], sg[:, 16:31], ALU.mult,
                           [PSB[5], SG], [HTB[NT_OWN // 4]])
                    else:
                        tt("dve", HT[:, c, 15 + grp * 512:15 + (grp + 1) * 512], PS[5][:, :], sg[:, :], ALU.mult,
                           [PSB[5], SG], [HTB[grp]])
    except _Cut:
        pass
    barrier()

    if debug:
        o = dout("d_KT", [128, SEQ], BF16)
        dma("sp", o, KT[:], "dbg", [], [])
        o = dout("d_QT", [128, 4, NOWN], BF16)
        dma("sp", o, QT[:], "dbg", [], [])
        o = dout("d_VA", [128, NT_ALL, 2, 128], BF16)
        dma("sp", o, VA[:], "dbg", [], [])
        o = dout("d_HT", [128, 4, NOWN + 32], BF16)
        dma("sp", o, HT[:], "dbg", [], [])
        barrier()
    if stop_after == "A":
        P.emit()
        return nc, dbg

    new_scope()
    CT = sb([128, 4, NOWN], BF16, "CT", persistent=True)
    CTB = [Buf(f"ct{i}") for i in range(8)]
    dg = sb([128, 4, CONV_W, 128], BF16, "dg")
    DG = Buf("dg")
    cw = sb([128, 4, CONV_W], F32, "cw")
    cwr = sb([CONV_W, 512], F32, "cwr")
    CW = Buf("cw")
    dma("sp", cwr[:], conv_dw, "cw", [], [CW])
    for c in range(4):
        tr(PS[0][:, c * 32:c * 32 + CONV_W], cwr[:, c * 128:(c + 1) * 128], ident_f[0:CONV_W, 0:CONV_W],
           [CW, CONST], [PSB[0]])
    vcopy("dve", cw[:], PS[0][:, 0:128].rearrange("p (c j) -> p c j", c=4)[:, :, 0:CONV_W], [PSB[0]], [CW])
    for c in range(4):
        for j in range(CONV_W):
            eng = "dve" if (j % 2 == 0) else "pool"
            ts(eng, dg[:, c, j, :], ident_b[:], cw[:, c, j:j + 1], None, ALU.mult, None, [CW, CONST], [DG])
    ybuf = sb([128, 4, 512], F32, "ybuf")
    ysq = sb([128, 4, 512], F32, "ysq")
    YB = [Buf(f"yb{c}") for c in range(4)]
    YS = [Buf(f"ys{c}") for c in range(4)]
    m2 = sb([128, 512], F32, "m2")
    M2 = Buf("m2")
    rstd_c = sb([128, 512], F32, "rstd_c")
    RSC = Buf("rstd_c")
    tmpc = sb([128, 512], F32, "tmpc")
    TMPC = Buf("tmpc")
    for q in range(8):
        hreads = [HTB[q]] + ([HTB[q + 1]] if q + 1 <= 8 else []) + ([HTB[q - 1]] if q > 0 else [])
        for c in range(4):
            bank = c % 2
            for j in range(CONV_W):
                mm(PS[bank][:, :], dg[:, c, j, :], HT[:, c, q * 512 + j:q * 512 + j + 512],
                   j == 0, j == CONV_W - 1, hreads + [DG], [PSB[bank]])
            act(ybuf[:, c, :], PS[bank][:, :], AF.Identity, [PSB[bank], CONST], [YB[c]], bias=cb[:, c:c + 1])
            act(ysq[:, c, :], ybuf[:, c, :], AF.Square, [YB[c]], [YS[c]])
        for c in range(4):
            mm(PS[2][:, :], ones_f[:], ybuf[:, c, :], c == 0, c == 3, [YB[c], CONST], [PSB[2]])
        for c in range(4):
            mm(PS[3][:, :], ones_f[:], ysq[:, c, :], c == 0, c == 3, [YS[c], CONST], [PSB[3]])
        act(m2[:], PS[2][:, :], AF.Square, [PSB[2]], [M2])
        tt("dve", m2[:], PS[3][:, :], m2[:], ALU.subtract, [PSB[3], M2], [M2])
        act(m2[:], m2[:], AF.Sqrt, [M2], [M2], bias=EPS, scale=1.0)
        recip(rstd_c[:], m2[:], [M2], [RSC])
        for c in range(4):
            tt("dve", tmpc[:], ybuf[:, c, :], PS[2][:, :], ALU.subtract, [YB[c], PSB[2]], [TMPC])
            tt("dve", tmpc[:], tmpc[:], rstd_c[:], ALU.mult, [TMPC, RSC], [TMPC])
            act(CT[:, c, q * 512:(q + 1) * 512], tmpc[:], AF.Silu, [TMPC, CONST], [CTB[q]],
                bias=lb[:, c:c + 1], scale=lg[:, c:c + 1])
    barrier()
    if debug:
        o = dout("d_CT", [128, 4, NOWN], BF16)
        dma("sp", o, CT[:], "dbg", [], [])
        barrier()
    if stop_after == "B":
        P.emit()
        return nc, dbg

    new_scope()
    htstack.close()
    wob_a = sb([64, 8, D_MODEL], BF16, "wob_a")
    wob_c = sb([128, 4, D_MODEL], BF16, "wob_c")
    WOB = Buf("wob")
    xr = [sb([128, D_MODEL], F32, f"xr{i}") for i in range(2)]
    XR = [Buf(f"xr{i}") for i in range(2)]
    for hh in range(8):
        s = hh % 2
        dma("sp", xr[s][0:64, :], w_out[hh * 64:(hh + 1) * 64, :], f"xr{s}", [], [XR[s]])
        vcopy("dve", wob_a[:, hh, :], xr[s][0:64, :], [XR[s]], [WOB])
    for c in range(4):
        s = c % 2
        dma("sp", xr[s][:, :], w_out[512 + c * 128:512 + (c + 1) * 128, :], f"xr{s}", [], [XR[s]])
        vcopy("dve", wob_c[:, c, :], xr[s][:, :], [XR[s]], [WOB])
    pT = [sb([128, 1024], BF16, f"pT{i}") for i in range(3)]
    PT = [Buf(f"pT{i}") for i in range(3)]
    den = sb([64, 512], F32, "den")
    DEN = Buf("den")
    OTn = sb([64, 8, 512], BF16, "OTn")
    OTB = [Buf(f"otn{h}") for h in range(8)]
    x2t = [sb([128, D_MODEL], F32, f"x2t{i}") for i in range(2)]
    X2T = [Buf(f"x2t{i}") for i in range(2)]
    X2D = [Buf(f"x2d{i}") for i in range(NT_OWN)]
    NKP = NT_ALL // 2
    its = [(qt, g, j, kp) for qt in range(8) for g in range(2) for j in range(4) for kp in range(NKP)]
    LOOK = 1

    QTz = [sb([128, 8, 512], BF16, f"QTz{i}") for i in range(2)]
    QTZ = [Buf(f"qtz{i}") for i in range(2)]
    for zz in range(2):
        memset("pool", QTz[zz][:].rearrange("p a b -> p (a b)"), 0.0, [QTZ[zz]])
    qtz_done = set()

    def acopy_or_dve(out, in_, R, W):
        P.op("dve", lambda h: h.tensor_copy(out=out, in_=in_), R, W)

    def fill_qtz(qt):
        if qt in qtz_done:
            return
        qtz_done.add(qt)
        for g in range(2):
            for j in range(4):
                acopy_or_dve(QTz[qt % 2][g * 64:(g + 1) * 64, g * 4 + j, :],
                      QT[g * 64:(g + 1) * 64, j, qt * 512:(qt + 1) * 512],
                      [QTB[qt * 4 + ii] for ii in range(4)], [QTZ[qt % 2]])

    def issue_qk(i):
        qt, g, j, kp = its[i]
        fill_qtz(qt)
        sp = i % 2
        for u in range(2):
            kt = kp * 2 + u
            mm(PSD[sp][:, u * 512:(u + 1) * 512], KT[:, kt * 128:(kt + 1) * 128],
               QTz[qt % 2][:, g * 4 + j, :], True, True,
               [KTB[kt], QTZ[qt % 2]], [PSB[2 * sp], PSB[2 * sp + 1]])

    UVD_C = Buf("uvd")
    pstg = [sb([128, D_MODEL], F32, f"pstg{i}") for i in range(2)]
    pstb = [sb([128, D_MODEL], BF16, f"pstb{i}") for i in range(2)]
    PSTG = [Buf(f"pstg{i}") for i in range(2)]
    PSTB = [Buf(f"pstb{i}") for i in range(2)]

    def prep_gen():
        steps = [(a, which, tab) for a in range(128) for which, tab in ((0, peer_u), (1, peer_v))]

        def load(n):
            a, which, tab = steps[n]
            z = n % 2
            dma("sp", pstg[z][:], tab[a * 128:(a + 1) * 128, :], f"pi{z}", [], [PSTG[z]])

        load(0)
        for n, (a, which, tab) in enumerate(steps):
            z = n % 2
            if n + 1 < len(steps):
                load(n + 1)
            vcopy("pool", pstb[z][:], pstg[z][:], [PSTG[z]], [PSTB[z]])
            dma("sp", uv_d[a * 128:(a + 1) * 128, which * D_MODEL:(which + 1) * D_MODEL], pstb[z][:],
                f"po{z}", [PSTB[z]], [UVD_C])
            yield

    pgen = prep_gen()
    for i in range(LOOK):
        issue_qk(i)
    for i, (qt, g, j, kp) in enumerate(its):
        if i + LOOK < len(its):
            issue_qk(i + LOOK)
        if i % 4 == 1 and pgen is not None:
            try:
                next(pgen)
            except StopIteration:
                pgen = None
        h = g * 4 + j
        obank = 4 + (h % 2)
        sp = i % 2
        slot = i % 3
        act(pT[slot][:], PSD[sp][:, :], AF.Exp, [PSB[2 * sp], PSB[2 * sp + 1]], [PT[slot]])
        for u in range(2):
            kt = kp * 2 + u
            mm(PS[obank][:, :], VA[:, kt, g, :], pT[slot][:, u * 512:(u + 1) * 512], kt == 0, kt == NT_ALL - 1,
               [VAB[kt], PT[slot]], [PSB[obank]])
        if kp != NKP - 1:
            continue
        acopy(den[:], PS[obank][64:128, :], [PSB[obank]], [DEN])
        recip(den[:], den[:], [DEN], [DEN])
        tt("dve", OTn[:, h, :], PS[obank][0:64, :], den[:], ALU.mult, [PSB[obank], DEN], [OTB[h]])
        if h != 7:
            continue
        for sub in range(4):
            ti = qt * 4 + sub
            s = ti % 2
            tok = ti * 128
            dma("sp", xr[s][:], x_own[tok:tok + 128, :], f"xr{s}", [], [XR[s]])
            for half in range(2):
                bank = 6 + half
                n0 = half * 512
                for h in range(8):
                    mm(PS[bank][:, :], OTn[:, h, sub * 128:(sub + 1) * 128], wob_a[:, h, n0:n0 + 512],
                       h == 0, False, [OTB[h], WOB], [PSB[bank]])
                for c in range(4):
                    mm(PS[bank][:, :], CT[:, c, tok:tok + 128], wob_c[:, c, n0:n0 + 512],
                       False, c == 3, [CTB[qt], WOB], [PSB[bank]])
                tt("dve", x2t[s][:, n0:n0 + 512], PS[bank][:, :], xr[s][:, n0:n0 + 512], ALU.add,
                   [PSB[bank], XR[s]], [X2T[s]])
            dma("sp", x2_d[tok:tok + 128, :], x2t[s][:], f"x2o{s}", [X2T[s]], [X2D[ti]])
    if pgen is not None:
        for _ in pgen:
            pass
    barrier()
    if debug:
        o = dout("d_x2", [NOWN, D_MODEL], F32)
        for ti in range(NT_OWN):
            s = ti % 2
            dma("sp", xr[s][:], x2_d[ti * 128:(ti + 1) * 128, :], f"xr{s}", [X2D[ti]], [XR[s]])
            dma("sp", o[ti * 128:(ti + 1) * 128, :], xr[s][:], f"x2o{s}", [XR[s]], [])
        barrier()
    if stop_after == "C":
        P.emit()
        return nc, dbg

    new_scope()
    pers.close()
    wqb = sb([128, 8, 2048], BF16, "wqb")
    WQB = Buf("wqb")
    keysT = sb([128, 2, 128], BF16, "keysT")
    g2bc = sb([128, D_MODEL], F32, "g2bc")
    gfbc = sb([128, D_MODEL], F32, "gfbc")
    x2s = [sb([128, D_MODEL], F32, f"x2s{i}") for i in range(2)]
    X2S = [Buf(f"x2s{i}") for i in range(2)]
    dma("sp", g2bc[:], norm2_g.partition_broadcast(128), "c0", [], [CONST])
    dma("sp", gfbc[:], final_g.partition_broadcast(128), "c0", [], [CONST])
    for hf in range(2):
        dma("sp", x2s[hf][:, 0:128], peer_keys[hf, :, :], f"x2s{hf}", [], [X2S[hf]])
        tr(PS[6][:, hf * 128:(hf + 1) * 128], x2s[hf][:, 0:128], ident_f[:], [X2S[hf], CONST], [PSB[6]])
    acopy(keysT[:], PS[6][:, 0:256].rearrange("p (a n) -> p a n", a=2), [PSB[6]], [CONST])
    for dc in range(8):
        for hf in range(2):
            s = (dc * 2 + hf) % 2
            dma("sp", x2s[s][:], peer_wq[dc * 128:(dc + 1) * 128, hf * 1024:(hf + 1) * 1024], f"x2s{s}",
                [], [X2S[s]])
            if hf == 0:
                vcopy("dve", wqb[:, dc, 0:1024], x2s[s][:], [X2S[s]], [WQB])
            else:
                acopy(wqb[:, dc, 1024:2048], x2s[s][:], [X2S[s]], [WQB])

    junk = sb([128, D_MODEL], BF16, "junkD")
    JUNK = Buf("junkD")
    st = sb([128, 8], F32, "stD")
    ST = Buf("stD")
    hn = sb([128, D_MODEL], F32, "hn")
    HN = Buf("hn")
    hnb = sb([128, D_MODEL], BF16, "hnb")
    HNB = Buf("hnb")
    hnT = sb([128, 8, 128], BF16, "hnT")
    HNT = Buf("hnT")
    qTs = sb([128, 16, 128], BF16, "qTs")
    QTS = Buf("qTs")
    S = sb([128, 16, 128], F32, "S")
    SB_ = Buf("S")
    wk = sb([128, 256], F32, "wk")
    tv = sb([128, 16, 16], F32, "tv")
    tiu = sb([128, 16, 16], U32, "tiu")
    tif = sb([128, 16, 16], F32, "tif")
    TK = Buf("topk1")
    cand = sb([128, 8, 16, 16], F32, "cand")
    CAND = Buf("cand")
    tops = sb([128, 8, 16], F32, "tops")
    posu = sb([128, 8, 16], U32, "posu")
    piu = sb([128, 8, 16], U32, "piu")
    pju = sb([128, 8, 16], U32, "pju")
    pif = sb([128, 8, 16], F32, "pif")
    pjf = sb([128, 8, 16], F32, "pjf")
    TK2 = Buf("topk2")
    oh = sb([128, 8, 16, 16], F32, "oh")
    OH = Buf("oh")
    i1s = sb([128, 8, 16], F32, "i1s")
    i2s = sb([128, 8, 16], F32, "i2s")
    ef = sb([128, 128], F32, "ef")
    gw = sb([128, 8, 16], F32, "gw")
    ssum = sb([128, 8], F32, "ssum")
    EG = Buf("eg")
    eTu = sb([128, 128], U32, "eTu")
    ET = Buf("eTu")
    gT = sb([128, 128], F32, "gT")
    GT = Buf("gT")
    hT = sb([128, 128], F32, "hT")
    HTD = Buf("hT")
    gl = sb([128, 128], F32, "gl")
    gl2 = sb([128, 128], F32, "gl2")
    ghT = sb([128, 128], F32, "ghT")
    GH = Buf("ghT")
    GHB = [Buf(f"ghb{i}") for i in range(4)]
    GHK = [Buf(f"ghk{i}") for i in range(4)]
    NSL = 16
    SUBB = 4
    NG = NSL // SUBB
    gsl = [sb([128, 2 * D_MODEL], BF16, f"gs{i}") for i in range(NSL)]
    GS = [Buf(f"gs{i}") for i in range(NSL)]
    ghb = sb([128, 128], BF16, "ghb")
    UVD = UVD_C
    oT = sb([128, 8, 128], F32, "oT")
    OT = Buf("oT")
    yb = sb([128, D_MODEL], F32, "yb")
    YBD = Buf("yb")
    yo = [sb([128, D_MODEL], F32, f"yo{i}") for i in range(2)]
    YO = [Buf(f"yo{i}") for i in range(2)]
    psO = PSD[2][:, :].rearrange("p (c t) -> p c t", c=8)
    gcnt = 0
    GELU_C = 2.0 * 0.7978845608028654
    n_tiles_d = NT_OWN if ntile_d is None else ntile_d
    hnb2 = [hnb, sb([128, D_MODEL], BF16, "hnb1")]
    hnT2 = [hnT, sb([128, 8, 128], BF16, "hnT1")]
    HNT2 = [HNT, Buf("hnT1")]
    ugT = [sb([128, 8, 128], BF16, f"ugT{i}") for i in range(2)]
    UGT = [Buf(f"ugT{i}") for i in range(2)]
    HNB2 = [HNB, Buf("hnb1")]
    eTu2 = [eTu, sb([128, 128], U32, "eTu1")]
    ET2 = [ET, Buf("eTu1")]
    gT2 = [gT, sb([128, 128], F32, "gT1")]
    GT2 = [GT, Buf("gT1")]
    st_e = sb([128, 8], F32, "st_e")
    STE = Buf("st_e")
    junk_e = sb([128, D_MODEL], BF16, "junk_e")
    JUNKE = Buf("junk_e")
    junk_d = sb([128, D_MODEL], BF16, "junk_d")
    HTDk = [Buf(f"hTk{i}") for i in range(4)]

    def top16(src_ap, n, tv_ap, ti_ap, R, W):
        P.op("dve", lambda h: h.max(out=tv_ap[:, 0:8], in_=src_ap), R, W)
        P.op("dve", lambda h: h.max_index(out=ti_ap[:, 0:8], in_max=tv_ap[:, 0:8], in_values=src_ap), R, W)
        P.op("dve", lambda h: h.match_replace(out=wk[:, 0:n], in_to_replace=tv_ap[:, 0:8], in_values=src_ap,
                                              imm_value=-1e30), R, W)
        P.op("dve", lambda h: h.max(out=tv_ap[:, 8:16], in_=wk[:, 0:n]), R, W)
        P.op("dve", lambda h: h.max_index(out=ti_ap[:, 8:16], in_max=tv_ap[:, 8:16], in_values=wk[:, 0:n]), R, W)

    def retrieval(ti):
        tok = ti * 128
        s = ti % 2
        hb, HB = hnb2[s], HNB2[s]
        hnT, HNT = hnT2[s], HNT2[s]
        dma("sp", x2s[s][:], x2_d[tok:tok + 128, :], f"x2s{s}", [X2D[ti]], [X2S[s]])
        P.op("dve", (lambda a, c: (lambda h: h.scalar_tensor_tensor(
            out=junk[:], in0=a, scalar=1.0, in1=a, op0=ALU.mult, op1=ALU.mult, accum_out=c)))(
            x2s[s][:], st[:, 0:1]), [X2S[s]], [JUNK, ST])
        act(st[:, 1:2], st[:, 0:1], AF.Ln, [ST], [ST], bias=epsc[:, 0:1], scale=1.0 / D_MODEL)
        act(st[:, 2:3], st[:, 1:2], AF.Exp, [ST], [ST], scale=-0.5)
        stt(hn[:], x2s[s][:], st[:, 2:3], g2bc[:], ALU.mult, ALU.mult, [X2S[s], ST, CONST], [HN])
        acopy(hb[:], hn[:], [HN], [HB])
        yield
        for dc in range(8):
            tr(psbf(7)[:, dc * 128:(dc + 1) * 128], hb[:, dc * 128:(dc + 1) * 128], ident_b[:],
               [HB, CONST], [PSB[7]])
        acopy(hnT[:], psbf(7).rearrange("p (c t) -> p c t", c=8), [PSB[7]], [HNT])
        yield
        for r in range(4):
            b = 6 + (r % 2)
            for a in range(4):
                jj = 4 * r + a
                for dc in range(8):
                    mm(PS[b][:, a * 128:(a + 1) * 128], wqb[:, dc, jj * 128:(jj + 1) * 128],
                       hnT[:, dc, :], dc == 0, dc == 7, [WQB, HNT], [PSB[b]])
            acopy(qTs[:, 4 * r:4 * r + 4, :], PS[b].rearrange("p (a t) -> p a t", a=4), [PSB[b]], [QTS])
            yield
        for r in range(4):
            b = 6 + (r % 2)
            for a in range(4):
                jj = 4 * r + a
                mm(PS[b][:, a * 128:(a + 1) * 128], qTs[:, jj, :], keysT[:, jj % 2, :], True, True,
                   [QTS, CONST], [PSB[b]])
            acopy(S[:, 4 * r:4 * r + 4, :], PS[b].rearrange("p (a t) -> p a t", a=4), [PSB[b]], [SB_])
            yield
        for jj in range(16):
            top16(S[:, jj, :], 128, tv[:, jj, :], tiu[:, jj, :], [SB_], [TK])
            yield
        vcopy("dve", tif[:], tiu[:], [TK], [TK])
        tvv = tv[:].rearrange("p (h a) k -> p h a k", a=2)
        tifv = tif[:].rearrange("p (h a) k -> p h a k", a=2)
        tt("dve", cand[:], tvv[:, :, 0, :].unsqueeze(3).to_broadcast([128, 8, 16, 16]),
           tvv[:, :, 1, :].unsqueeze(2).to_broadcast([128, 8, 16, 16]), ALU.add, [TK], [CAND])
        yield
        for hh in range(8):
            top16(cand[:, hh, :, :].rearrange("p a b -> p (a b)"), 256, tops[:, hh, :], posu[:, hh, :],
                  [CAND], [TK2])
            yield
        ts("dve", piu[:], posu[:], 4, None, ALU.logical_shift_right, None, [TK2], [TK2])
        ts("dve", pju[:], posu[:], 15, None, ALU.bitwise_and, None, [TK2], [TK2])
        vcopy("dve", pif[:], piu[:], [TK2], [TK2])
        vcopy("dve", pjf[:], pju[:], [TK2], [TK2])
        yield
        io16 = iota16[:].unsqueeze(1).unsqueeze(1).to_broadcast([128, 8, 16, 16])
        for (pf, col, dst) in ((pif, 0, i1s), (pjf, 1, i2s)):
            tt("dve", oh[:], io16, pf[:].unsqueeze(3).to_broadcast([128, 8, 16, 16]), ALU.is_equal,
               [TK2, CONST], [OH])
            tt("pool", oh[:], oh[:], tifv[:, :, col, :].unsqueeze(2).to_broadcast([128, 8, 16, 16]), ALU.mult,
               [OH, TK], [OH])
            reduce_add(dst[:], oh[:], [OH], [EG])
            yield
        stt(ef[:], i1s[:].rearrange("p h k -> p (h k)"), 128.0, i2s[:].rearrange("p h k -> p (h k)"),
            ALU.mult, ALU.add, [EG], [EG])
        tt("dve", gw[:], tops[:], tops[:, :, 0:1].to_broadcast([128, 8, 16]), ALU.subtract, [TK2], [EG])
        act(gw[:], gw[:], AF.Exp, [EG], [EG])
        yield
        reduce_add(ssum[:], gw[:], [EG], [EG])
        recip(ssum[:], ssum[:], [EG], [EG])
        tt("dve", gw[:], gw[:], ssum[:].unsqueeze(2).to_broadcast([128, 8, 16]), ALU.mult, [EG], [EG])
        tr(PS[7][:, 0:128], ef[:], ident_f[:], [EG, CONST], [PSB[7]])
        tr(PS[7][:, 128:256], gw[:].rearrange("p h k -> p (h k)"), ident_f[:], [EG, CONST], [PSB[7]])
        yield
        vcopy("dve", eTu2[s][:], PS[7][:, 0:128], [PSB[7]], [ET2[s]])
        acopy(gT2[s][:], PS[7][:, 128:256], [PSB[7]], [GT2[s]])
        if debug and ti == 0:
            o = dout("d_e", [128, 128], U32)
            dma("sp", o, eTu2[s][:], "dbg", [ET2[s]], [])
            o = dout("d_g", [128, 128], F32)
            dma("sp", o, gT2[s][:], "dbg", [GT2[s]], [])
        yield

    def drain(gen):
        if gen is None:
            return
        for _ in gen:
            pass

    def step(gen):
        if gen is None:
            return None
        try:
            next(gen)
            return gen
        except StopIteration:
            return None

    nsb = 128 // SUBB
    pending_epi = []

    def run_epilogue():
        while pending_epi:
            ti_, s_ = pending_epi.pop(0)
            tok_ = ti_ * 128
            for c in range(8):
                tr(PSD[3][:, c * 128:(c + 1) * 128], oT[:, c, :], ident_f[:], [OT, CONST], [PSB[6], PSB[7]])
            tt("dve", yb[:], PSD[3][:, :], x2s[s_][:], ALU.add, [PSB[6], PSB[7], X2S[s_]], [YBD])
            P.op("dve", (lambda a, c: (lambda h: h.scalar_tensor_tensor(
                out=junk_e[:], in0=a, scalar=1.0, in1=a, op0=ALU.mult, op1=ALU.mult, accum_out=c)))(
                yb[:], st_e[:, 4:5]), [YBD], [JUNKE, STE])
            act(st_e[:, 5:6], st_e[:, 4:5], AF.Ln, [STE], [STE], bias=epsc[:, 0:1], scale=1.0 / D_MODEL)
            act(st_e[:, 6:7], st_e[:, 5:6], AF.Exp, [STE], [STE], scale=-0.5)
            stt(yo[s_][:], yb[:], st_e[:, 6:7], gfbc[:], ALU.mult, ALU.mult, [YBD, STE, CONST], [YO[s_]])
            dma("sp", out_d[tok_:tok_ + 128, :], yo[s_][:], f"yo{s_}", [YO[s_]], [])

    gen = retrieval(0)
    drain(gen)
    for ti in range(n_tiles_d):
        tok = ti * 128
        s = ti % 2
        hb, HB, eT_, ETB, gT_, GTB = hnb2[s], HNB2[s], eTu2[s], ET2[s], gT2[s], GT2[s]
        hTc, HTc = hnT2[s], HNT2[s]
        gen = retrieval(ti + 1) if ti + 1 < n_tiles_d else None

        pend = []

        def flushM():
            while pend:
                (t, u, hbk) = pend.pop(0)
                for dc in range(8):
                    mm(PS[hbk][:, t:t + 1], ugT[u][:, dc, :], hTc[:, dc, t:t + 1], dc == 0, dc == 7,
                       [UGT[u], HTc], [PSB[hbk]])

        def stageA(k, hooks=None):
            nonlocal gcnt
            hbk = 2 + (k % 2)
            if k >= nsb:
                flushM()
                for hk_ in (hooks or []):
                    hk_()
                return
            for t in range(k * SUBB, (k + 1) * SUBB):
                sl = (k % NG) * SUBB + (t % SUBB)
                tb = gcnt % 2
                u = gcnt % 2
                gcnt += 1
                P.dma("pool", (lambda o, ia: (lambda h: h.indirect_dma_start(
                    out=o, out_offset=None, in_=uv_d[:, :],
                    in_offset=bass.IndirectOffsetOnAxis(ap=ia, axis=0))))(gsl[sl][:], eT_[:, t:t + 1]),
                    f"g{sl}", [ETB, UVD], [GS[sl]])
                for dc in range(8):
                    tr(psbf(tb)[:, dc * 128:(dc + 1) * 128], gsl[sl][:, dc * 128:(dc + 1) * 128], ident_b[:],
                       [GS[sl], CONST], [PSB[tb]])
                acopy(ugT[u][:].rearrange("p c h -> p (c h)"), psbf(tb), [PSB[tb]], [UGT[u]])
                flushM()
                pend.append((t, u, hbk))
                if hooks is not None and (t % SUBB) < len(hooks):
                    hooks[t % SUBB]()

        def stageG1a(k):
            c0, c1 = k * SUBB, (k + 1) * SUBB
            HK = HTDk[k % 4]
            G = GHK[k % 4]
            hbk = 2 + (k % 2)
            acopy(hT[:, c0:c1], PS[hbk][:, c0:c1], [PSB[hbk]], [HK])
            tt("dve", gl[:, c0:c1], hT[:, c0:c1], hT[:, c0:c1], ALU.mult, [HK], [G])
            ts("dve", gl[:, c0:c1], gl[:, c0:c1], 0.044715, 1.0, ALU.mult, ALU.add, [G], [G])
            tt("dve", gl[:, c0:c1], gl[:, c0:c1], hT[:, c0:c1], ALU.mult, [G, HK], [G])

        def stageG1b(k):
            c0, c1 = k * SUBB, (k + 1) * SUBB
            G = GHK[k % 4]
            act(gl2[:, c0:c1], gl[:, c0:c1], AF.Exp, [G], [G], scale=-GELU_C)

        def stageG2(k):
            c0, c1 = k * SUBB, (k + 1) * SUBB
            HK = HTDk[k % 4]
            G = GHK[k % 4]
            ts("dve", gl2[:, c0:c1], gl2[:, c0:c1], 1.0, None, ALU.add, None, [G], [G])
            recip(gl2[:, c0:c1], gl2[:, c0:c1], [G], [G])
            tt("dve", gl2[:, c0:c1], gl2[:, c0:c1], hT[:, c0:c1], ALU.mult, [G, HK], [G])
            tt("dve", ghb[:, c0:c1], gl2[:, c0:c1], gT_[:, c0:c1], ALU.mult, [G, GTB], [GHB[k % 4]])

        def stageS(k):
            for t in range(k * SUBB, (k + 1) * SUBB):
                sl = (k % NG) * SUBB + (t % SUBB)
                for c in range(8):
                    mm(psO[:, c, t:t + 1], gsl[sl][:, D_MODEL + c * 128:D_MODEL + (c + 1) * 128], ghb[:, t:t + 1],
                       True, True, [GS[sl], GHB[k % 4]], [PSB[4], PSB[5]])

        for k in range(nsb + 1):
            hooks = None
            if k >= 1:
                kk_ = k - 1
                hooks = [lambda kk_=kk_: stageG1a(kk_), lambda kk_=kk_: stageG1b(kk_),
                         lambda kk_=kk_: stageG2(kk_), lambda kk_=kk_: stageS(kk_)]
            stageA(k, hooks)
            if k == 0:
                run_epilogue()
            if k >= 1:
                gen = step(gen)
                if k % 2 == 0:
                    gen = step(gen)
        drain(gen)
        acopy(oT[:], psO, [PSB[4], PSB[5]], [OT])
        pending_epi.append((ti, s))
    run_epilogue()
    barrier()
    P.emit()
    return nc, dbg


def make_in_maps(inputs):
    x = np.ascontiguousarray(np.asarray(inputs["x"], dtype=np.float32))
    shared = {}
    for k in ("norm1_g", "w_in", "q_norm_g", "k_norm_g", "conv_b", "conv_ln_g", "conv_ln_b", "w_out",
              "norm2_g", "peer_wq", "peer_keys", "peer_u", "peer_v", "final_g"):
        shared[k] = np.ascontiguousarray(np.asarray(inputs[k], dtype=np.float32))
    shared["conv_dw"] = np.ascontiguousarray(np.asarray(inputs["conv_dw"], dtype=np.float32).reshape(CONV_W, 512))
    maps = []
    for c in range(8):
        b, hf = c // 2, c % 2
        own0 = hf * NOWN
        oth0 = (1 - hf) * NOWN
        m = dict(shared)
        m["x_own"] = x[b, own0:own0 + NOWN]
        m["x_oth"] = x[b, oth0:oth0 + NOWN]
        halo = np.zeros((32, D_MODEL), np.float32)
        if hf == 1:
            halo[0:15] = x[b, own0 - 15:own0]
        else:
            halo[16:31] = x[b, own0 + NOWN:own0 + NOWN + 15]
        m["x_halo"] = halo
        pos = np.concatenate([np.arange(own0, own0 + NOWN), np.arange(oth0, oth0 + NOWN)])
        pos = pos.reshape(NT_ALL, 128).T
        rc = np.stack([pos // 64, pos % 64], axis=-1).astype(np.float32)
        m["rowcol"] = np.ascontiguousarray(rc)
        maps.append(m)
    return maps


_NC_CACHE = {}


def kernel(**inputs):
    if "nc" not in _NC_CACHE:
        _NC_CACHE["nc"] = build_program("D", False)[0]
    nc = _NC_CACHE["nc"]
    maps = make_in_maps(inputs)
    res = run_bass_kernel_spmd(nc, maps, core_ids=list(range(8)))
    out = np.empty((4, SEQ, D_MODEL), np.float32)
    for c in range(8):
        b, hf = c // 2, c % 2
        out[b, hf * NOWN:(hf + 1) * NOWN] = res.results[c]["out"]
    return out
```
